# Optimizing a Trainium2 kernel written in Bass

```python
import math
import jax, jax.numpy as jnp
from jax import lax
import numpy as np

D_MODEL = 4096
BATCH = 2
SEQ = 4096
DEPTH = 2

N_META = 16
D_MIX = D_MODEL
RW_HEAD = 64
RW_WIDTH = 3 * D_MIX // 8
RW_HEADS = RW_WIDTH // RW_HEAD
RW_DECAY_LORA = 128
RW_AAA_LORA = 128
RW_GATE_LORA = 480
GN_EPS = 64e-5
AT_HEAD = 128
AT_WIDTH = 3 * D_MIX // 8
AT_HEADS = AT_WIDTH // AT_HEAD
AT_Q_LORA = 512
IDX_HEADS = 8
IDX_HEAD = 128
TOPK_MAX = 256
Q_BLOCK = 64
ROPE_THETA = 10000.0
POOL_WIDTH = D_MIX - RW_WIDTH - AT_WIDTH
POOL_WINDOWS = (2, 4, 8, 16)
POOL_GROUP = POOL_WIDTH // len(POOL_WINDOWS)
N_GROUPS = 8
EXPERTS_PER_GROUP = 8
N_EXPERTS = N_GROUPS * EXPERTS_PER_GROUP
TOP_K = 2
D_EXPERT = 512
MOE_BLOCK = 128
DN_ALPHA = (2 * DEPTH) ** 0.25
DN_BETA = (8 * DEPTH) ** -0.25
LN_EPS = 1e-5
RW_SIZES = (RW_WIDTH, RW_WIDTH, RW_WIDTH, RW_DECAY_LORA, RW_AAA_LORA, RW_GATE_LORA)
OTHER_SIZES = (AT_Q_LORA, AT_WIDTH, AT_WIDTH, IDX_HEAD, IDX_HEADS, POOL_WIDTH)
RW_COLS = sum(RW_SIZES)
D_IN = RW_COLS + sum(OTHER_SIZES)

kernel_name = "hymba_rwkv7_dsa_pool_hmoe_deepnorm"


def _layernorm(x, g, b, eps=LN_EPS):
    xf = x.astype(jnp.float32)
    mu = jnp.mean(xf, axis=-1, keepdims=True)
    var = jnp.mean(jnp.square(xf - mu), axis=-1, keepdims=True)
    return ((xf - mu) * lax.rsqrt(var + eps) * g + b).astype(x.dtype)


def _rmsnorm(x, g, eps=1e-6):
    xf = x.astype(jnp.float32)
    return (xf * lax.rsqrt(jnp.mean(jnp.square(xf), axis=-1, keepdims=True) + eps) * g).astype(x.dtype)


def _rope(x, pos):
    d = x.shape[-1]
    inv = ROPE_THETA ** (-jnp.arange(0, d, 2, dtype=jnp.float32) / d)
    ang = pos.astype(jnp.float32)[:, None] * inv[None, :]
    cos = jnp.cos(ang)[None, :, None, :]
    sin = jnp.sin(ang)[None, :, None, :]
    xf = x.astype(jnp.float32)
    x1, x2 = xf[..., : d // 2], xf[..., d // 2:]
    return jnp.concatenate([x1 * cos - x2 * sin, x2 * cos + x1 * sin], axis=-1).astype(x.dtype)


def _split(h, sizes):
    idx = np.cumsum(sizes)[:-1].tolist()
    return jnp.split(h, idx, axis=-1)


def _rwkv7_step(S, inp):
    r_t, w_t, k_t, v_t, kk_t, a_t = inp
    sa = jnp.einsum('bhij,bhj->bhi', S, -kk_t)
    S = S * w_t[:, :, None, :] + sa[..., None] * (kk_t * a_t)[:, :, None, :] + v_t[..., None] * k_t[:, :, None, :]
    y = jnp.einsum('bhij,bhj->bhi', S, r_t)
    return S, y


def rwkv7_mix(r, k, v, dw, da, dg, w2, w0, a2, a0, g2, k_k, k_a, r_k, gn_g, gn_b):
    f32 = jnp.float32
    bsz, t_len, _ = r.shape
    dt = r.dtype
    r, k, v = r.astype(f32), k.astype(f32), v.astype(f32)
    w = -jax.nn.softplus(-(w0 + jnp.tanh(dw.astype(f32)) @ w2.astype(f32))) - 0.5
    a = jax.nn.sigmoid(a0 + da.astype(f32) @ a2.astype(f32))
    g = jax.nn.sigmoid(dg.astype(f32)) @ g2.astype(f32)
    hs = lambda z: z.reshape(bsz, t_len, RW_HEADS, RW_HEAD)
    kk = hs(k * k_k)
    kk = kk / jnp.maximum(jnp.sqrt(jnp.sum(kk * kk, axis=-1, keepdims=True)), 1e-12)
    k = k * (1.0 + (a - 1.0) * k_a)
    decay = jnp.exp(-jnp.exp(w))
    r_h, k_h, v_h, a_h, d_h = hs(r), hs(k), hs(v), hs(a), hs(decay)
    seq_in = tuple(jnp.moveaxis(z, 1, 0) for z in (r_h, d_h, k_h, v_h, kk, a_h))
    s0 = jnp.zeros((bsz, RW_HEADS, RW_HEAD, RW_HEAD), f32)
    _, y = lax.scan(_rwkv7_step, s0, seq_in)
    y = jnp.moveaxis(y, 0, 1)
    mu = jnp.mean(y, axis=-1, keepdims=True)
    var = jnp.mean(jnp.square(y - mu), axis=-1, keepdims=True)
    yn = ((y - mu) * lax.rsqrt(var + GN_EPS)).reshape(bsz, t_len, RW_WIDTH) * gn_g + gn_b
    bonus = jnp.sum(r_h * k_h * r_k, axis=-1, keepdims=True) * v_h
    out = (yn + bonus.reshape(bsz, t_len, RW_WIDTH)) * g
    return out.astype(dt)


def dsa_attention(cq, k, v, kidx, widx, q_norm_g, w_uq, w_iq, kidx_g, kidx_b, n_sel):
    f32 = jnp.float32
    bsz, t_len, _ = k.shape
    dt = k.dtype
    pos = jnp.arange(t_len)
    cq = _rmsnorm(cq, q_norm_g)
    q = _rope((cq @ w_uq).reshape(bsz, t_len, AT_HEADS, AT_HEAD), pos)
    qi = _rope((cq @ w_iq).reshape(bsz, t_len, IDX_HEADS, IDX_HEAD), pos)
    k = _rope(k.reshape(bsz, t_len, AT_HEADS, AT_HEAD), pos)
    v = v.reshape(bsz, t_len, AT_HEADS, AT_HEAD)
    ki = _rope(_layernorm(kidx, kidx_g, kidx_b)[:, :, None, :], pos)[:, :, 0]
    wq = widx.astype(f32) * (IDX_HEADS ** -0.5) * (IDX_HEAD ** -0.5)
    scale = AT_HEAD ** -0.5
    nb = -(-t_len // Q_BLOCK)
    t_pad = nb * Q_BLOCK
    padq = lambda z: jnp.pad(z, ((0, 0), (0, t_pad - t_len)) + ((0, 0),) * (z.ndim - 2))
    blk = lambda z: jnp.moveaxis(padq(z).reshape((bsz, nb, Q_BLOCK) + z.shape[2:]), 1, 0)
    q_b, qi_b, w_b = blk(q), blk(qi), blk(wq)
    pos_b = jnp.arange(t_pad).reshape(nb, Q_BLOCK)
    gather = jax.vmap(lambda kb, ib: kb[ib])

    def one_block(args):
        qq, qqi, ww, tq = args
        sc = jax.nn.relu(jnp.einsum('bqhd,bsd->bqhs', qqi, ki).astype(f32))
        sc = jnp.einsum('bqhs,bqh->bqs', sc, ww)
        causal = pos[None, :] <= tq[:, None]
        sc = jnp.where(causal[None], sc, -jnp.inf)
        _, sel = lax.top_k(sc, n_sel)
        valid = sel <= tq[None, :, None]
        k_sel = gather(k, sel)
        v_sel = gather(v, sel)
        lg = jnp.einsum('bqhd,bqkhd->bqhk', qq, k_sel).astype(f32) * scale
        lg = jnp.where(valid[:, :, None, :], lg, -jnp.inf)
        p = jax.nn.softmax(lg, axis=-1)
        return jnp.einsum('bqhk,bqkhd->bqhd', p.astype(dt), v_sel)

    o = lax.map(one_block, (q_b, qi_b, w_b, pos_b))
    o = jnp.moveaxis(o, 0, 1).reshape(bsz, t_pad, AT_WIDTH)[:, :t_len]
    return o.astype(dt)


def pool_mix(p, w_pool, pool_scale):
    f32 = jnp.float32
    bsz, t_len, _ = p.shape
    pf = p.astype(f32)
    c = jnp.pad(jnp.cumsum(pf, axis=1), ((0, 0), (1, 0), (0, 0)))
    t = jnp.arange(t_len)
    outs = []
    for gi, win in enumerate(POOL_WINDOWS):
        cg = c[..., gi * POOL_GROUP:(gi + 1) * POOL_GROUP]
        lo = jnp.maximum(t + 1 - win, 0)
        cnt = (t + 1 - lo).astype(f32)
        outs.append((cg[:, 1:] - cg[:, lo]) / cnt[None, :, None])
    pooled = (jnp.concatenate(outs, axis=-1) - pf).reshape(bsz, t_len, len(POOL_WINDOWS), POOL_GROUP)
    y = jnp.einsum('btgc,gcd->btgd', pooled, w_pool.astype(f32)).reshape(bsz, t_len, POOL_WIDTH)
    return (y * pool_scale).astype(p.dtype)


def hier_moe(x2, wg, bg, we, be, w1, w3, w2):
    f32 = jnp.float32
    n, d = x2.shape
    g_prob = jax.nn.softmax((x2 @ wg).astype(f32) + bg, axis=-1)
    g_val, g_idx = lax.top_k(g_prob, 1)
    e_logits = ((x2 @ we).astype(f32) + be).reshape(n, N_GROUPS, EXPERTS_PER_GROUP)
    e_logits = e_logits[jnp.arange(n), g_idx[:, 0]]
    e_val, e_idx = lax.top_k(jax.nn.softmax(e_logits, axis=-1), TOP_K)
    gate = g_val * e_val / jnp.sum(e_val, axis=-1, keepdims=True)
    expert = g_idx * EXPERTS_PER_GROUP + e_idx
    a_tot = n * TOP_K
    e_flat = expert.reshape(-1)
    tok_flat = jnp.repeat(jnp.arange(n), TOP_K)
    order = jnp.argsort(e_flat)
    se, stok, sgate = e_flat[order], tok_flat[order], gate.reshape(-1)[order]
    counts = jnp.bincount(e_flat, length=N_EXPERTS)
    starts = jnp.cumsum(counts) - counts
    pcounts = (counts + MOE_BLOCK - 1) // MOE_BLOCK * MOE_BLOCK
    pends = jnp.cumsum(pcounts)
    pstarts = pends - pcounts
    dest = pstarts[se] + (jnp.arange(a_tot) - starts[se])
    p_rows = -(-(a_tot + N_EXPERTS * (MOE_BLOCK - 1)) // MOE_BLOCK) * MOE_BLOCK
    nb = p_rows // MOE_BLOCK
    row_tok = jnp.full((p_rows,), n, jnp.int32).at[dest].set(stok.astype(jnp.int32))
    row_gate = jnp.zeros((p_rows,), f32).at[dest].set(sgate)
    blk_e = jnp.minimum(jnp.searchsorted(pends, jnp.arange(nb) * MOE_BLOCK, side='right'), N_EXPERTS - 1)
    xpad = jnp.concatenate([x2, jnp.zeros((1, d), x2.dtype)], axis=0)

    def expert_block(args):
        rows, e = args
        xb = xpad[rows]
        h = jax.nn.silu(xb @ w1[e]) * (xb @ w3[e])
        return h @ w2[e]

    yb = lax.map(expert_block, (row_tok.reshape(nb, MOE_BLOCK), blk_e)).reshape(p_rows, d)
    out = jnp.zeros((n + 1, d), x2.dtype).at[row_tok].add(yb * row_gate[:, None].astype(x2.dtype))
    return out[:n]


def setup_inputs(seed: int = 0) -> dict:
    key = jax.random.key(seed)
    ks = jax.random.split(key, 40)
    f32 = jnp.float32
    L = DEPTH
    nrm = lambda k, shape, s: jax.random.normal(k, shape, f32) * s
    return {
        "x": nrm(ks[0], (BATCH, SEQ, D_MODEL), 1.0),
        "meta": nrm(ks[1], (N_META, D_MODEL), 1.0),
        "w_in": nrm(ks[2], (L, D_MODEL, D_IN), D_MODEL ** -0.5),
        "rw_mu": jax.random.uniform(ks[3], (L, RW_COLS), f32),
        "rw_w2": nrm(ks[4], (L, RW_DECAY_LORA, RW_WIDTH), 0.5 * RW_DECAY_LORA ** -0.5),
        "rw_w0": jax.random.uniform(ks[5], (L, RW_WIDTH), f32, -5.0, 1.0),
        "rw_a2": nrm(ks[6], (L, RW_AAA_LORA, RW_WIDTH), 0.5 * RW_AAA_LORA ** -0.5),
        "rw_a0": nrm(ks[7], (L, RW_WIDTH), 0.3),
        "rw_g2": nrm(ks[8], (L, RW_GATE_LORA, RW_WIDTH), RW_GATE_LORA ** -0.5),
        "rw_kk": 0.85 + nrm(ks[9], (L, RW_WIDTH), 0.05),
        "rw_ka": 1.0 + nrm(ks[10], (L, RW_WIDTH), 0.05),
        "rw_rk": nrm(ks[11], (L, RW_HEADS, RW_HEAD), 0.1),
        "rw_gn_g": 1.0 + nrm(ks[12], (L, RW_WIDTH), 0.02),
        "rw_gn_b": nrm(ks[13], (L, RW_WIDTH), 0.02),
        "at_qnorm_g": 1.0 + nrm(ks[14], (L, AT_Q_LORA), 0.02),
        "at_w_uq": nrm(ks[15], (L, AT_Q_LORA, AT_WIDTH), AT_Q_LORA ** -0.5),
        "at_w_iq": nrm(ks[16], (L, AT_Q_LORA, IDX_HEADS * IDX_HEAD), AT_Q_LORA ** -0.5),
        "at_kidx_g": 1.0 + nrm(ks[17], (L, IDX_HEAD), 0.02),
        "at_kidx_b": nrm(ks[18], (L, IDX_HEAD), 0.02),
        "pool_w": nrm(ks[19], (L, len(POOL_WINDOWS), POOL_GROUP, POOL_GROUP), POOL_GROUP ** -0.5),
        "pool_scale": 1.0 + nrm(ks[20], (L, POOL_WIDTH), 0.02),
        "w_out": nrm(ks[21], (L, D_MIX, D_MODEL), DN_BETA * D_MIX ** -0.5),
        "ln1_g": 1.0 + nrm(ks[22], (L, D_MODEL), 0.02),
        "ln1_b": nrm(ks[23], (L, D_MODEL), 0.02),
        "router_g_w": nrm(ks[24], (L, D_MODEL, N_GROUPS), D_MODEL ** -0.5),
        "router_g_b": nrm(ks[25], (L, N_GROUPS), 0.01),
        "router_e_w": nrm(ks[26], (L, D_MODEL, N_EXPERTS), D_MODEL ** -0.5),
        "router_e_b": nrm(ks[27], (L, N_EXPERTS), 0.01),
        "exp_w1": nrm(ks[28], (L, N_EXPERTS, D_MODEL, D_EXPERT), D_MODEL ** -0.5),
        "exp_w3": nrm(ks[29], (L, N_EXPERTS, D_MODEL, D_EXPERT), D_MODEL ** -0.5),
        "exp_w2": nrm(ks[30], (L, N_EXPERTS, D_EXPERT, D_MODEL), DN_BETA * D_EXPERT ** -0.5),
        "ln2_g": 1.0 + nrm(ks[31], (L, D_MODEL), 0.02),
        "ln2_b": nrm(ks[32], (L, D_MODEL), 0.02),
    }


def reference(x, meta, w_in, rw_mu, rw_w2, rw_w0, rw_a2, rw_a0, rw_g2, rw_kk, rw_ka, rw_rk,
              rw_gn_g, rw_gn_b, at_qnorm_g, at_w_uq, at_w_iq, at_kidx_g, at_kidx_b, pool_w,
              pool_scale, w_out, ln1_g, ln1_b, router_g_w, router_g_b, router_e_w, router_e_b,
              exp_w1, exp_w3, exp_w2, ln2_g, ln2_b):
    bsz, seq_len, d = x.shape
    n_sel = min(TOPK_MAX, seq_len // 4)
    h = jnp.concatenate([jnp.broadcast_to(meta.astype(x.dtype)[None], (bsz, N_META, d)), x], axis=1)
    t_len = h.shape[1]
    for l in range(DEPTH):
        proj = h @ w_in[l]
        p_rw, p_rest = proj[..., :RW_COLS], proj[..., RW_COLS:]
        prev = jnp.pad(p_rw, ((0, 0), (1, 0), (0, 0)))[:, :-1]
        p_rw = p_rw + (prev - p_rw) * rw_mu[l]
        r, k, v, dw, da, dg = _split(p_rw, RW_SIZES)
        cq, ka, va, kidx, widx, pin = _split(p_rest, OTHER_SIZES)
        y_rw = rwkv7_mix(r, k, v, dw, da, dg, rw_w2[l], rw_w0[l], rw_a2[l], rw_a0[l], rw_g2[l],
                         rw_kk[l], rw_ka[l], rw_rk[l], rw_gn_g[l], rw_gn_b[l])
        y_at = dsa_attention(cq, ka, va, kidx, widx, at_qnorm_g[l], at_w_uq[l], at_w_iq[l],
                             at_kidx_g[l], at_kidx_b[l], n_sel)
        y_pl = pool_mix(pin, pool_w[l], pool_scale[l])
        mix = jnp.concatenate([y_rw, y_at, y_pl], axis=-1) @ w_out[l]
        h = _layernorm(DN_ALPHA * h + mix, ln1_g[l], ln1_b[l])
        ff = hier_moe(h.reshape(bsz * t_len, d), router_g_w[l], router_g_b[l], router_e_w[l],
                      router_e_b[l], exp_w1[l], exp_w3[l], exp_w2[l]).reshape(bsz, t_len, d)
        h = _layernorm(DN_ALPHA * h + ff, ln2_g[l], ln2_b[l])
    return h[:, N_META:]
```

```python
import time, math
import contextlib
import numpy as np
import concourse.bass as bass
import concourse.mybir as mybir
from concourse.bass_utils import run_bass_kernel_spmd

F32 = mybir.dt.float32
BF16 = mybir.dt.bfloat16
I32 = mybir.dt.int32
U32 = mybir.dt.uint32
ALU = mybir.AluOpType
AF = mybir.ActivationFunctionType
AX = mybir.AxisListType

SEM_ROLL = 20000
import threading


class Coop(threading.Thread):
    def __init__(self, fn):
        super().__init__(daemon=True)
        self.fn = fn
        self.tickets = threading.Semaphore(0)
        self.consumed = threading.Semaphore(0)
        self.finished = False
        self.err = None
        self.start()

    def run(self):
        try:
            self.fn()
        except BaseException as e:
            self.err = e
        self.finished = True
        self.consumed.release()

    def advance(self, k=1):
        for _ in range(k):
            if self.finished:
                break
            self.tickets.release()
            self.consumed.acquire()
        if self.err is not None:
            raise self.err

    def finish(self):
        while not self.finished:
            self.advance(1)
        if self.err is not None:
            raise self.err


class Sched:
    def __init__(self, nc, es, ndsem=6, same_eng_sync=True):
        self.nc = nc
        self.es = es
        self.eng = dict(pe=nc.tensor, dve=nc.vector, act=nc.scalar, pool=nc.gpsimd, sp=nc.sync)
        self.same = same_eng_sync
        self.nsem = 0
        self.csem = {}
        self.ccnt = {}
        self.cep = {}
        for k in ("pe", "dve", "act", "pool"):
            self._new_csem(k, 0)
        self.dsem = {}
        self.dcnt = {}
        self.drr = {}
        for q in ("sp", "act", "pool"):
            self.dsem[q] = [self._sem(f"d_{q}_{i}") for i in range(ndsem)]
            self.dcnt[q] = [0] * ndsem
            self.drr[q] = 0
        self.seen = {k: {} for k in self.eng}
        self.lastw = {}
        self.readers = {}
        self.ninst = 0

    def _sem(self, name):
        self.nsem += 1
        return self.es.enter_context(self.nc.semaphore(name))

    def _new_csem(self, k, ep):
        self.cep[k] = ep
        self.csem[k] = self._sem(f"c_{k}_{ep}")
        self.ccnt[k] = 0

    def _wait(self, e, tok):
        sem, val, key, src = tok
        if src == e and src in ("pe",):
            return
        if src == e and not self.same and src in ("dve", "act", "pool") and key[0] == "c":
            return
        if self.seen[e].get(key, 0) >= val:
            return
        self.eng[e].wait_ge(sem, val)
        self.seen[e][key] = val
        self.ninst += 1

    def _deps(self, e, r, w):
        for k in r:
            t = self.lastw.get(k)
            if t is not None:
                self._wait(e, t)
        for k in w:
            t = self.lastw.get(k)
            if t is not None:
                self._wait(e, t)
            for t in self.readers.get(k, ()):
                self._wait(e, t)

    def _record(self, tok, r, w):
        for k in r:
            self.readers.setdefault(k, []).append(tok)
        for k in w:
            self.lastw[k] = tok
            self.readers[k] = []

    def op(self, e, fn, r=(), w=()):
        cw = threading.current_thread()
        if isinstance(cw, Coop):
            cw.tickets.acquire()
            try:
                return self._op(e, fn, r, w)
            finally:
                cw.consumed.release()
        return self._op(e, fn, r, w)

    def _op(self, e, fn, r=(), w=()):
        self._deps(e, r, w)
        inst = fn(self.eng[e])
        if self.ccnt[e] >= SEM_ROLL:
            self._new_csem(e, self.cep[e] + 1)
        self.ccnt[e] += 1
        inst.then_inc(self.csem[e], 1)
        tok = (self.csem[e], self.ccnt[e], ("c", e, self.cep[e]), e)
        self._record(tok, r, w)
        self.ninst += 1
        return tok

    def dma(self, q, out, in_, r=(), w=(), fn=None, **kw):
        cw = threading.current_thread()
        if isinstance(cw, Coop):
            cw.tickets.acquire()
            try:
                return self._dma(q, out, in_, r, w, fn, **kw)
            finally:
                cw.consumed.release()
        return self._dma(q, out, in_, r, w, fn, **kw)

    def _dma(self, q, out, in_, r=(), w=(), fn=None, **kw):
        i = self.drr[q]
        self.drr[q] = (i + 1) % len(self.dsem[q])
        sem = self.dsem[q][i]
        key = ("d", q, i)
        if self.dcnt[q][i] > 0:
            self._wait(q, (sem, 16 * self.dcnt[q][i], key, "dma"))
        self._deps(q, r, w)
        if fn is None:
            inst = self.eng[q].dma_start(out=out, in_=in_, **kw)
        else:
            inst = fn(self.eng[q])
        self.dcnt[q][i] += 1
        inst.then_inc(sem, 16)
        tok = (sem, 16 * self.dcnt[q][i], key, "dma")
        self._record(tok, r, w)
        self.ninst += 1
        return tok

    def barrier(self):
        toks = []
        for k in ("pe", "dve", "act", "pool"):
            if self.ccnt[k] > 0:
                toks.append((self.csem[k], self.ccnt[k], ("c", k, self.cep[k]), "bar"))
        for q in self.dsem:
            for i, sem in enumerate(self.dsem[q]):
                if self.dcnt[q][i] > 0:
                    toks.append((sem, 16 * self.dcnt[q][i], ("d", q, i), "bar"))
        for e in ("pe", "dve", "act", "pool", "sp"):
            for t in toks:
                self._wait(e, t)

    def finish(self, toks, e="sp"):
        for t in toks:
            self._wait(e, t)


def new_nc():
    return bass.Bass("TRN2", target_bir_lowering=False)


def run(nc, in_maps):
    res = run_bass_kernel_spmd(nc, in_maps, core_ids=list(range(len(in_maps))))
    return res.results


def mm_phase(nc, S, es, xT, w, dst, ntok, K, N, ps, cb=256, dst_key=None):
    KC = K // 128
    sbt = lambda n, s, d: es.enter_context(nc.sbuf_tensor(n, s, d))
    xb = sbt("mm_xb", [128, KC, ntok], BF16)
    xs = sbt("mm_xs", [128, KC // 4, ntok], F32)
    ws = sbt("mm_ws", [128, KC, cb], F32)
    wb = [sbt(f"mm_wb{i}", [128, KC, cb], BF16) for i in range(2)]
    ot = [sbt(f"mm_ot{i}", [128, cb], F32) for i in range(4)]
    xv = xT.rearrange("(kc p) t -> p kc t", p=128)
    wv = w.rearrange("(kc p) n -> p kc n", p=128)
    q = KC // 4
    for i in range(4):
        S.dma("sp" if i % 2 == 0 else "act", xs[:, :, :], xv[:, i * q:(i + 1) * q, :], w=["mm_xs"])
        h2 = q // 2
        if h2 > 0:
            S.op("dve", lambda e: e.tensor_copy(out=xb[:, i * q:i * q + h2, :], in_=xs[:, :h2, :]), r=["mm_xs"], w=[("mm_xb", i)])
        S.op("act", lambda e: e.copy(out=xb[:, i * q + h2:(i + 1) * q, :], in_=xs[:, h2:, :]), r=["mm_xs"], w=[("mm_xb", i)])
    toks = [(t0, min(128, ntok - t0)) for t0 in range(0, ntok, 128)]
    ncb = (N + cb - 1) // cb
    it = 0
    outs = []
    h = KC // 2
    for c in range(ncb):
        c0 = c * cb
        cn = min(cb, N - c0)
        wbb = wb[c % 2]
        S.dma("sp", ws[:, :h, :cn], wv[:, :h, c0:c0 + cn], w=[("mm_ws", 0)])
        S.dma("act", ws[:, h:, :cn], wv[:, h:, c0:c0 + cn], w=[("mm_ws", 1)])
        S.op("dve", lambda e: e.tensor_copy(out=wbb[:, :h, :cn], in_=ws[:, :h, :cn]), r=[("mm_ws", 0)], w=[("mm_wb", c % 2, 0)])
        S.op("act", lambda e: e.copy(out=wbb[:, h:, :cn], in_=ws[:, h:, :cn]), r=[("mm_ws", 1)], w=[("mm_wb", c % 2, 1)])
        for (t0, m) in toks:
            p = ps[it % 4]
            o = ot[it % 4]
            for kc in range(KC):
                S.op("pe", lambda e: e.matmul(p[:m, :cn], lhsT=xb[:, kc, t0:t0 + m], rhs=wbb[:, kc, :cn], start=(kc == 0), stop=(kc == KC - 1)),
                     r=[("mm_xb", kc // q), ("mm_wb", c % 2, 0 if kc < h else 1)], w=[("mm_ps", it % 4)])
            if it % 2 == 0:
                S.op("dve", lambda e: e.tensor_copy(out=o[:m, :cn], in_=p[:m, :cn]), r=[("mm_ps", it % 4)], w=[("mm_ot", it % 4)])
            else:
                S.op("act", lambda e: e.copy(out=o[:m, :cn], in_=p[:m, :cn]), r=[("mm_ps", it % 4)], w=[("mm_ot", it % 4)])
            outs.append(S.dma("pool", dst[t0:t0 + m, c0:c0 + cn], o[:m, :cn], r=[("mm_ot", it % 4)], w=([(dst_key, t0)] if dst_key else [])))
            it += 1
    return outs


def build_mm(ntok, K, N, cb=256, name="mm"):
    nc = new_nc()
    xT = nc.dram_tensor("xT", [K, ntok], F32, kind="ExternalInput").ap()
    w = nc.dram_tensor("w", [K, N], F32, kind="ExternalInput").ap()
    out = nc.dram_tensor("out", [ntok, N], F32, kind="ExternalOutput").ap()
    es = contextlib.ExitStack()
    with es:
        S = Sched(nc, es)
        ps = [es.enter_context(nc.psum_tensor(f"ps{i}", [128, 512], F32)) for i in range(4)]
        outs = mm_phase(nc, S, es, xT, w, out, ntok, K, N, ps, cb)
        S.finish(outs, "pool")
        print("ninst", S.ninst, "nsem", S.nsem)
    return nc


import math

GN_EPS = 64e-5
DEBUG = False
NV = 10
C_MUR, C_MUK, C_MUV, C_W0, C_A0, C_KK, C_KA, C_RK, C_GG, C_GB = range(10)

def build_rwkv(T, TC=256, TCB=32, same=False):
    nc = new_nc()
    G = 3
    di = lambda n, s: nc.dram_tensor(n, s, F32, kind="ExternalInput").ap()
    prT = di("prT", [384, T + 1]); pkT = di("pkT", [384, T + 1]); pvT = di("pvT", [384, T + 1])
    pdwT = di("pdwT", [128, T + 1]); pdaT = di("pdaT", [128, T + 1]); pdgT = di("pdgT", [480, T + 1])
    pv_tm = di("pv_tm", [T + 1, 384])
    vec = di("vec", [384, NV]); vlo = di("vlo", [128, 2]); vdg = di("vdg", [480, 1]); muvb = di("muvb", [128, 384])
    w2s = di("w2s", [128, 384]); a2s = di("a2s", [128, 384]); g2s = di("g2s", [480, 384])
    cM = di("cM", [128, 128]); cSel = di("cSel", [64, 2, 128])
    yT = nc.dram_tensor("yT", [384, T], F32, kind="ExternalOutput").ap()
    vscr = nc.dram_tensor("vscr", [T, 384], F32, kind="Internal").ap()
    dbg = nc.dram_tensor("dbg", [12, 128, 3, TC], F32, kind="ExternalOutput").ap() if DEBUG else None
    es = contextlib.ExitStack()
    with es:
        S = Sched(nc, es, same_eng_sync=same)
        sb = lambda n, s, d=F32: es.enter_context(nc.sbuf_tensor(n, s, d))
        pt = lambda n, s: es.enter_context(nc.psum_tensor(n, s, F32))
        vec_t = sb("vec_t", [128, G, NV]); vlo_t = sb("vlo_t", [128, 2]); vdg_t = sb("vdg_t", [128, 4, 1]); muvb_t = sb("muvb_t", [128, 384])
        w2_t = sb("w2_t", [128, 384]); a2_t = sb("a2_t", [128, 384]); g2_t = sb("g2_t", [128, 4, 384])
        M_t = sb("M_t", [128, 128]); Mavg_t = sb("Mavg_t", [128, 128]); Sel_t = sb("Sel_t", [64, 2, 128]); Mrk_t = sb("Mrk_t", [128, G, 128])
        S.dma("sp", vec_t[:], vec.rearrange("(g p) n -> p g n", p=128), w=["vec"])
        S.dma("sp", vlo_t[:], vlo, w=["vlo"])
        for kc in range(4):
            n = 128 if kc < 3 else 96
            S.dma("sp", vdg_t[:n, kc, :], vdg[kc * 128:kc * 128 + n, :], w=["vdg"])
            S.dma("act", g2_t[:n, kc, :], g2s[kc * 128:kc * 128 + n, :], w=["g2"])
        S.dma("sp", muvb_t[:], muvb, w=["muvb"])
        S.dma("act", w2_t[:], w2s, w=["w2"]); S.dma("act", a2_t[:], a2s, w=["a2"])
        S.dma("sp", M_t[:], cM, w=["M"]); S.dma("sp", Sel_t[:], cSel, w=["Sel"])
        S.op("dve", lambda e: e.tensor_scalar(out=Mavg_t[:], in0=M_t[:], scalar1=1.0 / 64, scalar2=None, op0=ALU.mult), r=["M"], w=["Mavg"])
        for g in range(G):
            S.op("dve", lambda e: e.tensor_scalar(out=Mrk_t[:, g, :], in0=M_t[:], scalar1=vec_t[:, g, C_RK:C_RK + 1], scalar2=None, op0=ALU.mult), r=["M", "vec"], w=["Mrk"])
        W1 = TC + 1
        ld_r = sb("ld_r", [128, G, W1]); ld_k = sb("ld_k", [128, G, W1]); ld_v = sb("ld_v", [128, G, W1])
        ld_dw = sb("ld_dw", [128, W1]); ld_da = sb("ld_da", [128, W1]); ld_dg = sb("ld_dg", [128, 4, W1])
        tmA = sb("tmA", [128, 384]); tmB = sb("tmB", [128, 384]); tmD = sb("tmD", [128, 384])
        rs = sb("rs", [128, G, TC]); ks = sb("ks", [128, G, TC]); vs = sb("vs", [128, G, TC])
        dws = sb("dws", [128, TC]); das = sb("das", [128, TC]); dgs = sb("dgs", [128, 4, TC])
        aa = sb("aa", [128, G, TC])
        dec2 = [sb(f"dec{q}", [128, G, TC]) for q in range(2)]; gg2 = [sb(f"gg{q}", [128, G, TC]) for q in range(2)]
        nkk2 = [sb(f"nkk{q}", [128, G, TC]) for q in range(2)]; bb2 = [sb(f"bb{q}", [128, G, TC]) for q in range(2)]
        kmod2 = [sb(f"kmod{q}", [128, G, TC]) for q in range(2)]
        Rm2 = [sb(f"Rm{q}", [128, G, TC, 2]) for q in range(2)]; bonv2 = [sb(f"bonv{q}", [128, G, TC]) for q in range(2)]
        t1 = sb("t1", [128, TC]); t2 = sb("t2", [128, TC]); t3 = sb("t3", [128, TC])
        u1 = sb("u1", [128, TC]); u2 = sb("u2", [128, TC])
        ysb = sb("ysb", [64, G, TC * 2]); yfm = sb("yfm", [128, TC]); yo = sb("yo", [128, G, TC])
        SD = [[sb(f"SD{q}_{g}", [128, 64]) for g in range(G)] for q in range(2)]
        S1 = [sb(f"S1_{g}", [128, 64]) for g in range(G)]
        S2 = [sb(f"S2_{g}", [128, 64]) for g in range(G)]
        Zall = sb("Zall", [128, G * 64], mybir.dt.float32r)
        M_r = sb("M_r", [128, 128], mybir.dt.float32r)
        NL = 4
        Lr = [[sb(f"L{g}_{i}", [128, 128]) for i in range(NL)] for g in range(G)]
        vb = [[sb(f"vb{g}_{i}", [128, TCB, 64]) for i in range(2)] for g in range(G)]
        ps1 = [pt(f"ps1_{g}", [128, 512]) for g in range(G)]
        psy = [pt(f"psy_{g}", [128, 512]) for g in range(G)]
        psA = pt("psA", [128, 512]); psB = pt("psB", [128, 512])
        for g in range(G):
            for q in range(2):
                S.op("pool", lambda e: e.memset(SD[q][g][:], 0.0), w=[("SD", q, g)])
        S.op("dve", lambda e: e.tensor_copy(out=M_r[:], in_=M_t[:]), r=["M"], w=["M_r"])
        out_toks = []
        nvb = 0
        lcnt = [0] * G
        def pre(c0, par):
            n = min(TC, T - c0)
            dec, gg, nkk, bb, kmod, Rm, bonv = dec2[par], gg2[par], nkk2[par], bb2[par], kmod2[par], Rm2[par], bonv2[par]
            for r0 in range(c0, c0 + n, 128):
                m = min(128, c0 + n - r0)
                S.dma("sp", tmA[:m, :], pv_tm[r0 + 1:r0 + 1 + m, :], w=["tmA"])
                S.dma("act", tmB[:m, :], pv_tm[r0:r0 + m, :], w=["tmB"])
                S.op("dve", lambda e: e.tensor_tensor(out=tmD[:m, :], in0=tmB[:m, :], in1=tmA[:m, :], op=ALU.subtract), r=["tmA", "tmB"], w=["tmD"])
                S.op("dve", lambda e: e.tensor_tensor(out=tmD[:m, :], in0=tmD[:m, :], in1=muvb_t[:m, :], op=ALU.mult), r=["tmD", "muvb"], w=["tmD"])
                S.op("dve", lambda e: e.tensor_tensor(out=tmD[:m, :], in0=tmD[:m, :], in1=tmA[:m, :], op=ALU.add), r=["tmD", "tmA"], w=["tmD"])
                S.dma("sp", vscr[r0:r0 + m, :], tmD[:m, :], r=["tmD"], w=[("vscr", r0 // TCB + i) for i in range((m + TCB - 1) // TCB)])
            S.dma("sp", ld_r[:, :, :n + 1], prT[:, c0:c0 + n + 1].rearrange("(g p) t -> p g t", p=128), w=["ld_r"])
            S.dma("act", ld_k[:, :, :n + 1], pkT[:, c0:c0 + n + 1].rearrange("(g p) t -> p g t", p=128), w=["ld_k"])
            S.dma("sp", ld_v[:, :, :n + 1], pvT[:, c0:c0 + n + 1].rearrange("(g p) t -> p g t", p=128), w=["ld_v"])
            S.dma("act", ld_dw[:, :n + 1], pdwT[:, c0:c0 + n + 1], w=["ld_dw"])
            S.dma("sp", ld_da[:, :n + 1], pdaT[:, c0:c0 + n + 1], w=["ld_da"])
            for kc in range(4):
                kn = 128 if kc < 3 else 96
                S.dma("act", ld_dg[:kn, kc, :n + 1], pdgT[kc * 128:kc * 128 + kn, c0:c0 + n + 1], w=[("ld_dg", kc)])

            def shift(dst, src, mu, P, rk, wk):
                S.op("dve", lambda e: e.tensor_tensor(out=t1[:P, :n], in0=src[:P, 0:n], in1=src[:P, 1:n + 1], op=ALU.subtract), r=[rk], w=["t1"])
                S.op("dve", lambda e: e.scalar_tensor_tensor(out=dst[:P, :n], in0=t1[:P, :n], scalar=mu, in1=src[:P, 1:n + 1], op0=ALU.mult, op1=ALU.add), r=["t1", rk, "vec", "vlo", "vdg"], w=[wk])
            for g in range(G):
                shift(rs[:, g, :], ld_r[:, g, :], vec_t[:, g, C_MUR:C_MUR + 1], 128, "ld_r", ("rs", g))
                shift(ks[:, g, :], ld_k[:, g, :], vec_t[:, g, C_MUK:C_MUK + 1], 128, "ld_k", ("ks", g))
                shift(vs[:, g, :], ld_v[:, g, :], vec_t[:, g, C_MUV:C_MUV + 1], 128, "ld_v", ("vs", g))
            shift(dws, ld_dw, vlo_t[:, 0:1], 128, "ld_dw", "dws")
            shift(das, ld_da, vlo_t[:, 1:2], 128, "ld_da", "das")
            for kc in range(4):
                kn = 128 if kc < 3 else 96
                shift(dgs[:, kc, :], ld_dg[:, kc, :], vdg_t[:kn, kc, :], kn, ("ld_dg", kc), ("dgs", kc))
            S.op("act", lambda e: e.activation(out=dws[:, :n], in_=dws[:, :n], func=AF.Tanh), r=["dws"], w=["dws"])
            for kc in range(4):
                kn = 128 if kc < 3 else 96
                S.op("act", lambda e: e.activation(out=dgs[:kn, kc, :n], in_=dgs[:kn, kc, :n], func=AF.Sigmoid), r=[("dgs", kc)], w=[("dgs", kc)])
            for g in range(G):
                gs = slice(g * 128, (g + 1) * 128)
                S.op("pe", lambda e: e.matmul(psA[:, :n], lhsT=w2_t[:, gs], rhs=dws[:, :n], start=True, stop=True), r=["w2", "dws"], w=["psA"])
                S.op("act", lambda e: e.activation(out=t2[:, :n], in_=psA[:, :n], func=AF.Sigmoid, bias=vec_t[:, g, C_W0:C_W0 + 1]), r=["psA", "vec"], w=["t2"])
                S.op("act", lambda e: e.activation(out=dec[:, g, :n], in_=t2[:, :n], func=AF.Exp, scale=-math.exp(-0.5)), r=["t2"], w=[("dec", par, g)])
                S.op("pe", lambda e: e.matmul(psB[:, :n], lhsT=a2_t[:, gs], rhs=das[:, :n], start=True, stop=True), r=["a2", "das"], w=["psB"])
                S.op("act", lambda e: e.activation(out=aa[:, g, :n], in_=psB[:, :n], func=AF.Sigmoid, bias=vec_t[:, g, C_A0:C_A0 + 1]), r=["psB", "vec"], w=[("aa", g)])
                for kc in range(4):
                    kn = 128 if kc < 3 else 96
                    S.op("pe", lambda e: e.matmul(psA[:, :n], lhsT=g2_t[:kn, kc, gs], rhs=dgs[:kn, kc, :n], start=(kc == 0), stop=(kc == 3)), r=["g2", ("dgs", kc)], w=["psA"])
                S.op("act", lambda e: e.copy(out=gg[:, g, :n], in_=psA[:, :n]), r=["psA"], w=[("gg", par, g)])
                S.op("dve", lambda e: e.tensor_scalar(out=t1[:, :n], in0=ks[:, g, :n], scalar1=vec_t[:, g, C_KK:C_KK + 1], scalar2=None, op0=ALU.mult), r=[("ks", g), "vec"], w=["t1"])
                S.op("dve", lambda e: e.tensor_tensor(out=t3[:, :n], in0=t1[:, :n], in1=t1[:, :n], op=ALU.mult), r=["t1"], w=["t3"])
                S.op("pe", lambda e: e.matmul(psB[:, :n], lhsT=M_t[:], rhs=t3[:, :n], start=True, stop=True), r=["M", "t3"], w=["psB"])
                S.op("act", lambda e: e.activation(out=t3[:, :n], in_=psB[:, :n], func=AF.Sqrt), r=["psB"], w=["t3"])
                S.op("dve", lambda e: e.tensor_scalar(out=t3[:, :n], in0=t3[:, :n], scalar1=1e-12, scalar2=None, op0=ALU.max), r=["t3"], w=["t3"])
                S.op("dve", lambda e: e.reciprocal(out=t3[:, :n], in_=t3[:, :n]), r=["t3"], w=["t3"])
                S.op("dve", lambda e: e.scalar_tensor_tensor(out=nkk[:, g, :n], in0=t1[:, :n], scalar=-1.0, in1=t3[:, :n], op0=ALU.mult, op1=ALU.mult), r=["t1", "t3"], w=[("nkk", par, g)])
                S.op("dve", lambda e: e.scalar_tensor_tensor(out=bb[:, g, :n], in0=nkk[:, g, :n], scalar=-1.0, in1=aa[:, g, :n], op0=ALU.mult, op1=ALU.mult), r=[("nkk", par, g), ("aa", g)], w=[("bb", par, g)])
                S.op("dve", lambda e: e.tensor_scalar(out=t1[:, :n], in0=aa[:, g, :n], scalar1=-1.0, scalar2=vec_t[:, g, C_KA:C_KA + 1], op0=ALU.add, op1=ALU.mult), r=[("aa", g), "vec"], w=["t1"])
                S.op("dve", lambda e: e.scalar_tensor_tensor(out=kmod[:, g, :n], in0=t1[:, :n], scalar=1.0, in1=ks[:, g, :n], op0=ALU.add, op1=ALU.mult), r=["t1", ("ks", g)], w=[("kmod", par, g)])
                S.op("dve", lambda e: e.tensor_tensor(out=t1[:, :n], in0=rs[:, g, :n], in1=kmod[:, g, :n], op=ALU.mult), r=[("rs", g), ("kmod", par, g)], w=["t1"])
                S.op("pe", lambda e: e.matmul(psB[:, :n], lhsT=Mrk_t[:, g, :], rhs=t1[:, :n], start=True, stop=True), r=["Mrk", "t1"], w=["psB"])
                S.op("dve", lambda e: e.tensor_tensor(out=bonv[:, g, :n], in0=psB[:, :n], in1=vs[:, g, :n], op=ALU.mult), r=["psB", ("vs", g)], w=[("bonv", par, g)])
                S.op("pool", lambda e: e.memset(Rm[:, g, :n, :], 0.0), w=[("Rm", par, g)])
                S.op("pool", lambda e: e.tensor_copy(out=Rm[0:64, g, :n, 0], in_=rs[0:64, g, :n]), r=[("rs", g)], w=[("Rm", par, g)])
                S.op("pool", lambda e: e.tensor_copy(out=Rm[64:128, g, :n, 1], in_=rs[64:128, g, :n]), r=[("rs", g)], w=[("Rm", par, g)])
        def load_block(kb):
            t0b = kb * TCB
            if t0b >= T:
                return
            nb = min(TCB, T - t0b)
            ci_ = t0b // TC; parb = ci_ % 2; s0 = t0b - ci_ * TC; vbi_ = kb % 2
            for g in range(G):
                for h in range(2):
                    src = vscr[t0b:t0b + nb, g * 128 + h * 64:g * 128 + (h + 1) * 64]
                    S.dma("sp" if h == 0 else "act", vb[g][vbi_][h * 64:(h + 1) * 64, :nb, :], src.partition_broadcast(64),
                          r=[("vscr", kb)], w=[("vb", g, vbi_)])
                S.op("dve", lambda e: e.tensor_tensor(out=vb[g][vbi_][:, :nb, :], in0=vb[g][vbi_][:, :nb, :],
                                                      in1=kmod2[parb][:, g, s0:s0 + nb].unsqueeze(2).to_broadcast([128, nb, 64]), op=ALU.mult),
                     r=[("vb", g, vbi_), ("kmod", parb, g)], w=[("vb", g, vbi_)])

        def scan(c0, par, hook):
            nonlocal nvb
            n = min(TC, T - c0)
            dec, nkk, bb, kmod, Rm = dec2[par], nkk2[par], bb2[par], kmod2[par], Rm2[par]
            for s in range(n):
                t = c0 + s
                hook(s)
                if t % TCB == 0:
                    kb = t // TCB
                    if kb == 0:
                        load_block(0)
                    load_block(kb + 1)
                    vbi = kb % 2
                sl = t % TCB
                pr_, pw_ = (t + 1) % 2, t % 2
                for g in range(G):
                    S.op("act", lambda e: e.activation(out=Zall[:, g * 64:(g + 1) * 64], in_=SD[pr_][g][:], func=AF.Copy, scale=nkk[:, g, s:s + 1]), r=[("SD", pr_, g), ("nkk", par, g)], w=[("Z", g)])
                for g in range(G):
                    S.op("dve", lambda e: e.scalar_tensor_tensor(out=S1[g][:], in0=SD[pr_][g][:], scalar=dec[:, g, s:s + 1], in1=vb[g][vbi][:, sl, :], op0=ALU.mult, op1=ALU.add),
                         r=[("SD", pr_, g), ("dec", par, g), ("vb", g, vbi)], w=[("S1", g)])
                for g in range(G):
                    S.op("pe", lambda e: e.matmul(ps1[g][:, 0:64], lhsT=M_r[:], rhs=Zall[:, g * 64:(g + 1) * 64], start=True, stop=True), r=["M_r", ("Z", g)], w=[("ps1", g)])
                if s > 0:
                    for g in range(G):
                        S.op("pe", lambda e: e.matmul(psy[g][0:64, 2 * (s - 1):2 * (s - 1) + 2], lhsT=SD[pr_][g][:], rhs=Rm[:, g, s - 1, :], start=True, stop=True), r=[("SD", pr_, g), ("Rm", par, g)], w=[("psy", g)])
                for g in range(G):
                    S.op("dve", lambda e: e.scalar_tensor_tensor(out=SD[pw_][g][:], in0=ps1[g][:, 0:64], scalar=bb[:, g, s:s + 1], in1=S1[g][:], op0=ALU.mult, op1=ALU.add),
                         r=[("ps1", g), ("bb", par, g), ("S1", g)], w=[("SD", pw_, g)])
            lastp = (c0 + n - 1) % 2
            for g in range(G):
                S.op("pe", lambda e: e.matmul(psy[g][0:64, 2 * (n - 1):2 * (n - 1) + 2], lhsT=SD[lastp][g][:], rhs=Rm[:, g, n - 1, :], start=True, stop=True), r=[("SD", lastp, g), ("Rm", par, g)], w=[("psy", g)])
        def post(c0, par):
            n = min(TC, T - c0)
            gg, bonv = gg2[par], bonv2[par]
            for g in range(G):
                S.op("act", lambda e: e.copy(out=ysb[:, g, :2 * n], in_=psy[g][0:64, :2 * n]), r=[("psy", g)], w=[("ysb", g)])
            for g in range(G):
                yv = ysb[:, g, :2 * n].rearrange("p (t h) -> p t h", h=2)
                for h in range(2):
                    S.op("pe", lambda e: e.matmul(psA[:, :n], lhsT=Sel_t[:, h, :], rhs=yv[:, :, h], start=(h == 0), stop=(h == 1)), r=["Sel", ("ysb", g)], w=["psA"])
                S.op("act", lambda e: e.copy(out=yfm[:, :n], in_=psA[:, :n]), r=["psA"], w=["yfm"])
                S.op("pe", lambda e: e.matmul(psB[:, :n], lhsT=Mavg_t[:], rhs=yfm[:, :n], start=True, stop=True), r=["Mavg", "yfm"], w=["psB"])
                S.op("dve", lambda e: e.tensor_tensor(out=u1[:, :n], in0=yfm[:, :n], in1=psB[:, :n], op=ALU.subtract), r=["yfm", "psB"], w=["u1"])
                S.op("dve", lambda e: e.tensor_tensor(out=u2[:, :n], in0=u1[:, :n], in1=u1[:, :n], op=ALU.mult), r=["u1"], w=["u2"])
                S.op("pe", lambda e: e.matmul(psB[:, :n], lhsT=Mavg_t[:], rhs=u2[:, :n], start=True, stop=True), r=["Mavg", "u2"], w=["psB"])
                S.op("dve", lambda e: e.tensor_scalar(out=u2[:, :n], in0=psB[:, :n], scalar1=GN_EPS, scalar2=None, op0=ALU.add), r=["psB"], w=["u2"])
                S.op("act", lambda e: e.activation(out=u2[:, :n], in_=u2[:, :n], func=AF.Sqrt), r=["u2"], w=["u2"])
                S.op("dve", lambda e: e.reciprocal(out=u2[:, :n], in_=u2[:, :n]), r=["u2"], w=["u2"])
                S.op("dve", lambda e: e.tensor_tensor(out=u1[:, :n], in0=u1[:, :n], in1=u2[:, :n], op=ALU.mult), r=["u1", "u2"], w=["u1"])
                S.op("dve", lambda e: e.tensor_scalar(out=u1[:, :n], in0=u1[:, :n], scalar1=vec_t[:, g, C_GG:C_GG + 1], scalar2=vec_t[:, g, C_GB:C_GB + 1], op0=ALU.mult, op1=ALU.add), r=["u1", "vec"], w=["u1"])
                S.op("dve", lambda e: e.tensor_tensor(out=u1[:, :n], in0=u1[:, :n], in1=bonv[:, g, :n], op=ALU.add), r=["u1", ("bonv", par, g)], w=["u1"])
                S.op("dve", lambda e: e.tensor_tensor(out=yo[:, g, :n], in0=u1[:, :n], in1=gg[:, g, :n], op=ALU.mult), r=["u1", ("gg", par, g)], w=[("yo", g)])
                out_toks.append(S.dma("pool", yT[g * 128:(g + 1) * 128, c0:c0 + n], yo[:, g, :n], r=[("yo", g)]))
        chunks = list(range(0, T, TC))
        NPRE, NPOST = 400, 60
        pre(chunks[0], 0)
        wpost = None
        for ci, c0 in enumerate(chunks):
            par = ci % 2
            n = min(TC, T - c0)
            wpre = Coop(lambda c1=(chunks[ci + 1] if ci + 1 < len(chunks) else None), p1=(ci + 1) % 2: pre(c1, p1)) if ci + 1 < len(chunks) else None
            if wpost is not None:
                wpost.advance(G)
            kpre = (NPRE + n - 1) // n; kpost = (NPOST + n - 1) // n
            def hook(s, wpre=wpre, wpost=wpost):
                if wpost is not None and not wpost.finished:
                    wpost.advance(2)
                elif wpre is not None:
                    wpre.advance(2)
            scan(c0, par, hook)
            if wpost is not None:
                wpost.finish()
            if wpre is not None:
                wpre.finish()
            wpost = Coop(lambda c1=c0, p1=par: post(c1, p1))
        wpost.finish()
        S.finish(out_toks, "pool")
        print("ninst", S.ninst, "nsem", S.nsem)
    return nc


def consts():
    M = np.zeros((128, 128), np.float32); M[:64, :64] = 1; M[64:, 64:] = 1
    Sel = np.zeros((64, 2, 128), np.float32)
    for i in range(64):
        Sel[i, 0, i] = 1; Sel[i, 1, 64 + i] = 1
    return M, Sel


def rwkv_inmap(pr, pk, pv, pdw, pda, pdg, mu_r, mu_k, mu_v, mu_dw, mu_da, mu_dg, w2, w0, a2, a0, g2, k_k, k_a, r_k, gn_g, gn_b):
    M, Sel = consts()
    z = lambda a: np.ascontiguousarray(np.concatenate([np.zeros((a.shape[1], 1), np.float32), a.T], axis=1))
    vec = np.stack([mu_r, mu_k, mu_v, w0, a0, k_k, k_a, r_k, gn_g, gn_b], axis=1).astype(np.float32)
    return dict(prT=z(pr), pkT=z(pk), pvT=z(pv), pdwT=z(pdw), pdaT=z(pda), pdgT=z(pdg),
                pv_tm=np.ascontiguousarray(np.concatenate([np.zeros((1, pv.shape[1]), np.float32), pv], axis=0)),
                vec=np.ascontiguousarray(vec), vlo=np.ascontiguousarray(np.stack([mu_dw, mu_da], axis=1)),
                vdg=np.ascontiguousarray(mu_dg[:, None]), muvb=np.ascontiguousarray(np.broadcast_to(mu_v[None, :], (128, 384))),
                w2s=np.ascontiguousarray(w2), a2s=np.ascontiguousarray(a2), g2s=np.ascontiguousarray(g2), cM=M, cSel=Sel)


import math
import ml_dtypes

NEG = -1.0e30
LN_EPS = 1e-5

def build_dsa(NS=8, NH=12, topk=256):
    T = 512 * NS + 16
    NQ = 128 * NS + 4
    NKT = (T + 127) // 128
    nc = new_nc()
    di = lambda n, s, d=F32: nc.dram_tensor(n, s, d, kind="ExternalInput").ap()
    cq = di("cq", [NQ, 512]); widx = di("widx", [NQ, 8]); qpos = di("qpos", [NQ, 1])
    kT = di("kT", [NH * 128, T]); v = di("v", [T, NH * 128]); kidx = di("kidx", [T, 128])
    w_uq = di("w_uq", [512, NH * 128]); w_iq = di("w_iq", [512, 1024])
    qg = di("qg", [128, 4]); kig = di("kig", [128, 128]); kib = di("kib", [128, 128])
    cos_fm = di("cos_fm", [128, T]); sin_fm = di("sin_fm", [128, T])
    cosq = di("cosq", [128, NQ]); sinq = di("sinq", [128, NQ])
    kpos = di("kpos", [128, T]); cR = di("cR", [128, 128]); cI = di("cI", [128, 128])
    y = nc.dram_tensor("y", [NQ, NH * 128], F32, kind="ExternalOutput").ap()
    slots = [(j * 128, 128, 512 * (j + 1)) for j in range(NS)] + [(128 * NS, 4, T)]
    es = contextlib.ExitStack()
    with es:
        S = Sched(nc, es)
        sb = lambda n, s, d=F32, st=es: st.enter_context(nc.sbuf_tensor(n, s, d))
        pt = lambda n, s, d=F32: es.enter_context(nc.psum_tensor(n, s, d))
        def op(eng, fn, r, w): return S.op(eng, fn, r=r, w=w)
        maskT = sb("maskT", [128, NKT, NQ], BF16)
        cqnT = sb("cqnT", [128, 4, NQ]); cosq_t = sb("cosq_t", [128, NQ]); sinq_t = sb("sinq_t", [128, NQ])
        ctab = [sb(f"ctab{i}", [128, 512]) for i in range(2)]; stab = [sb(f"stab{i}", [128, 512]) for i in range(2)]
        xT = sb("xT", [128, 512])
        R_t = sb("R_t", [128, 128]); I_t = sb("I_t", [128, 128]); Ib_t = sb("Ib_t", [128, 128], BF16)
        tA = sb("tA", [128, 512]); tB = sb("tB", [128, 512])
        psR = [pt(f"psR{i}", [128, 512]) for i in range(2)]
        psM = [pt(f"psM{i}", [128, 512]) for i in range(2)]
        psO = [pt(f"psO{i}", [128, 512]) for i in range(2)]
        psT = [pt(f"psT{i}", [128, 512], BF16) for i in range(2)]
        S.dma("sp", R_t[:], cR, w=["R"]); S.dma("sp", I_t[:], cI, w=["I"])
        S.dma("sp", cosq_t[:], cosq, w=["cosq"]); S.dma("act", sinq_t[:], sinq, w=["sinq"])
        tcnt = [0]
        def rope_k(dst, src, c, n, rk, wk):
            i = tcnt[0] % 2; tcnt[0] += 1
            S.dma("sp", ctab[i][:, :n], cos_fm[:, c:c + n], w=[("ctab", i)])
            S.dma("act", stab[i][:, :n], sin_fm[:, c:c + n], w=[("stab", i)])
            rope_fm(dst, src, n, ctab[i][:, :n], stab[i][:, :n], rk, wk, [("ctab", i), ("stab", i)])
        op("dve", lambda e: e.tensor_copy(out=Ib_t[:], in_=I_t[:]), ["I"], ["Ib"])
        rcnt = [0]
        def rope_fm(dst, src, n, ct, st_, rk, wk, ck):
            i = rcnt[0] % 2; rcnt[0] += 1
            op("pe", lambda e: e.matmul(psR[i][:, :n], lhsT=R_t[:], rhs=src, start=True, stop=True), ["R"] + rk, [("psR", i)])
            op("dve", lambda e: e.tensor_tensor(out=tA[:, :n], in0=psR[i][:, :n], in1=st_, op=ALU.mult), [("psR", i)] + ck, ["tA"])
            op("pool", lambda e: e.tensor_tensor(out=tB[:, :n], in0=src, in1=ct, op=ALU.mult), rk + ck, ["tB"])
            op("dve", lambda e: e.tensor_tensor(out=dst, in0=tA[:, :n], in1=tB[:, :n], op=ALU.add), ["tA", "tB"], wk)

        p1 = contextlib.ExitStack()
        with p1:
            sb1 = lambda n, s, d=F32: sb(n, s, d, p1)
            qiS = sb1("qiS", [128, 8, 128]); kiT = sb1("kiT", [128, T]); wiq_t = sb1("wiq_t", [128, 4, 1024])
            wq_t = sb1("wq_t", [128, NS + 1, 8]); qpos_t = sb1("qpos_t", [128, NS + 1, 1])
            qg_t = sb1("qg_t", [128, 4]); kig_t = sb1("kig_t", [128, 128]); kib_t = sb1("kib_t", [128, 128])
            kpos_t = sb1("kpos_t", [128, 512]); qpc = sb1("qpc", [128, 1])
            xin = sb1("xin", [128, 512]); xsq = sb1("xsq", [128, 512]); st1 = sb1("st1", [128, 4])
            acc = sb1("acc", [128, T]); wrk = sb1("wrk", [128, T]); mk = sb1("mk", [128, T], BF16)
            m8 = sb1("m8", [128, 8]); thr = sb1("thr", [128, 1]); rl = sb1("rl", [128, 512])
            for kc in range(4):
                S.dma("sp" if kc % 2 == 0 else "act", wiq_t[:, kc, :], w_iq[kc * 128:(kc + 1) * 128, :], w=["wiq"])
            S.dma("sp", qg_t[:], qg, w=["qg"]); S.dma("sp", kig_t[:], kig, w=["kig"]); S.dma("act", kib_t[:], kib, w=["kib"])
            S.dma("sp", kpos_t[:], kpos[:, 0:512], w=["kpos"])
            for j, (q0, m, nk) in enumerate(slots):
                S.dma("sp", wq_t[:m, j, :], widx[q0:q0 + m, :], w=["wq"])
                S.dma("act", qpos_t[:m, j, :], qpos[q0:q0 + m, :], w=["qpos"])
            for j, (q0, m, nk) in enumerate(slots):
                S.dma("sp", xin[:m, :], cq[q0:q0 + m, :], w=["xin"])
                op("dve", lambda e: e.tensor_tensor(out=xsq[:m, :], in0=xin[:m, :], in1=xin[:m, :], op=ALU.mult), ["xin"], ["xsq"])
                op("dve", lambda e: e.tensor_reduce(out=st1[:m, 0:1], in_=xsq[:m, :], axis=AX.X, op=ALU.add), ["xsq"], ["st1"])
                op("dve", lambda e: e.tensor_scalar(out=st1[:m, 1:2], in0=st1[:m, 0:1], scalar1=1.0 / 512, scalar2=1e-6, op0=ALU.mult, op1=ALU.add), ["st1"], ["st1"])
                op("act", lambda e: e.activation(out=st1[:m, 2:3], in_=st1[:m, 1:2], func=AF.Sqrt), ["st1"], ["st1"])
                op("dve", lambda e: e.reciprocal(out=st1[:m, 3:4], in_=st1[:m, 2:3]), ["st1"], ["st1"])
                op("dve", lambda e: e.tensor_scalar(out=xsq[:m, :], in0=xin[:m, :], scalar1=st1[:m, 3:4], scalar2=None, op0=ALU.mult), ["xin", "st1"], ["xsq"])
                for kc in range(4):
                    i = kc % 2
                    op("pe", lambda e: e.transpose(psM[i][:, :m], xsq[:m, kc * 128:(kc + 1) * 128], I_t[:m, :m]), ["xsq", "I"], [("psM", i)])
                    op("act", lambda e: e.activation(out=cqnT[:, kc, q0:q0 + m], in_=psM[i][:, :m], func=AF.Copy, scale=qg_t[:, kc:kc + 1]), [("psM", i), "qg"], ["cqnT"])
            for kt in range(NKT):
                t0 = kt * 128; m = min(128, T - t0)
                S.dma("sp", xin[:m, :128], kidx[t0:t0 + m, :], w=["xin"])
                op("dve", lambda e: e.tensor_reduce(out=st1[:m, 0:1], in_=xin[:m, :128], axis=AX.X, op=ALU.add), ["xin"], ["st1"])
                op("dve", lambda e: e.tensor_scalar(out=st1[:m, 1:2], in0=st1[:m, 0:1], scalar1=-1.0 / 128, scalar2=None, op0=ALU.mult), ["st1"], ["st1"])
                op("dve", lambda e: e.tensor_scalar(out=xsq[:m, :128], in0=xin[:m, :128], scalar1=st1[:m, 1:2], scalar2=None, op0=ALU.add), ["xin", "st1"], ["xsq"])
                op("dve", lambda e: e.tensor_tensor(out=xsq[:m, 128:256], in0=xsq[:m, :128], in1=xsq[:m, :128], op=ALU.mult), ["xsq"], ["xsq"])
                op("dve", lambda e: e.tensor_reduce(out=st1[:m, 0:1], in_=xsq[:m, 128:256], axis=AX.X, op=ALU.add), ["xsq"], ["st1"])
                op("dve", lambda e: e.tensor_scalar(out=st1[:m, 1:2], in0=st1[:m, 0:1], scalar1=1.0 / 128, scalar2=LN_EPS, op0=ALU.mult, op1=ALU.add), ["st1"], ["st1"])
                op("act", lambda e: e.activation(out=st1[:m, 2:3], in_=st1[:m, 1:2], func=AF.Sqrt), ["st1"], ["st1"])
                op("dve", lambda e: e.reciprocal(out=st1[:m, 3:4], in_=st1[:m, 2:3]), ["st1"], ["st1"])
                op("dve", lambda e: e.scalar_tensor_tensor(out=xsq[:m, 256:384], in0=xsq[:m, :128], scalar=st1[:m, 3:4], in1=kig_t[:m, :], op0=ALU.mult, op1=ALU.mult), ["xsq", "st1", "kig"], ["xsq"])
                op("dve", lambda e: e.tensor_tensor(out=xsq[:m, 384:512], in0=xsq[:m, 256:384], in1=kib_t[:m, :], op=ALU.add), ["xsq", "kib"], ["xsq"])
                i = kt % 2
                op("pe", lambda e: e.transpose(psM[i][:, :m], xsq[:m, 384:512], I_t[:m, :m]), ["xsq", "I"], [("psM", i)])
                op("act", lambda e: e.copy(out=acc[:, t0:t0 + m], in_=psM[i][:, :m]), [("psM", i)], ["acc"])
            for c in range(0, T, 512):
                n = min(512, T - c)
                rope_k(kiT[:, c:c + n], acc[:, c:c + n], c, n, ["acc"], ["kiT"])
            cscale = (8 ** -0.5) * (128 ** -0.5)
            for j, (q0, m, nk) in enumerate(slots):
                for hh in range(8):
                    i = hh % 2
                    for kc in range(4):
                        op("pe", lambda e: e.matmul(psM[i][:, :m], lhsT=wiq_t[:, kc, hh * 128:(hh + 1) * 128], rhs=cqnT[:, kc, q0:q0 + m], start=(kc == 0), stop=(kc == 3)),
                           ["wiq", "cqnT"], [("psM", i)])
                    op("act", lambda e: e.copy(out=xT[:, :m], in_=psM[i][:, :m]), [("psM", i)], ["xT"])
                    rope_fm(qiS[:, hh, :m], xT[:, :m], m, cosq_t[:, q0:q0 + m], sinq_t[:, q0:q0 + m], ["xT"], ["qiS"], ["cosq", "sinq"])
                for c in range(0, nk, 512):
                    n = min(512, nk - c)
                    for hh in range(8):
                        i = hh % 2
                        op("pe", lambda e: e.matmul(psM[i][:m, :n], lhsT=qiS[:, hh, :m], rhs=kiT[:, c:c + n], start=True, stop=True), ["qiS", "kiT"], [("psM", i)])
                        op("act", lambda e: e.activation(out=rl[:m, :n], in_=psM[i][:m, :n], func=AF.Relu, scale=cscale), [("psM", i)], ["rl"])
                        if hh == 0:
                            op("dve", lambda e: e.tensor_scalar(out=acc[:m, c:c + n], in0=rl[:m, :n], scalar1=wq_t[:m, j, hh:hh + 1], scalar2=None, op0=ALU.mult), ["rl", "wq"], ["acc"])
                        else:
                            op("dve", lambda e: e.scalar_tensor_tensor(out=acc[:m, c:c + n], in0=rl[:m, :n], scalar=wq_t[:m, j, hh:hh + 1], in1=acc[:m, c:c + n], op0=ALU.mult, op1=ALU.add), ["rl", "wq", "acc"], ["acc"])
                for c in range(0, nk, 512):
                    n = min(512, nk - c)
                    op("dve", lambda e: e.tensor_scalar(out=qpc[:m, :], in0=qpos_t[:m, j, :], scalar1=float(-c), scalar2=None, op0=ALU.add), ["qpos"], ["qpc"])
                    op("dve", lambda e: e.tensor_scalar(out=wrk[:m, c:c + n], in0=kpos_t[:m, :n], scalar1=qpc[:m, :], scalar2=NEG, op0=ALU.is_gt, op1=ALU.mult), ["kpos", "qpc"], ["wrk"])
                op("dve", lambda e: e.tensor_tensor(out=acc[:m, :nk], in0=acc[:m, :nk], in1=wrk[:m, :nk], op=ALU.add), ["acc", "wrk"], ["acc"])
                src = acc
                for rnd in range(topk // 8):
                    op("dve", lambda e: e.max(out=m8[:m, :], in_=src[:m, :nk]), ["acc", "wrk"], ["m8"])
                    if rnd < topk // 8 - 1:
                        op("dve", lambda e: e.match_replace(out=wrk[:m, :nk], in_to_replace=m8[:m, :], in_values=src[:m, :nk], imm_value=-3.0e38), ["acc", "wrk", "m8"], ["wrk"])
                    src = wrk
                op("dve", lambda e: e.tensor_scalar(out=thr[:m, :], in0=m8[:m, 7:8], scalar1=-1.0e29, scalar2=None, op0=ALU.max), ["m8"], ["thr"])
                op("dve", lambda e: e.tensor_scalar(out=mk[:m, :nk], in0=acc[:m, :nk], scalar1=thr[:m, :], scalar2=None, op0=ALU.is_ge), ["acc", "thr"], ["mk"])
                for kt in range((nk + 127) // 128):
                    k0 = kt * 128; kn = min(128, nk - k0)
                    i = kt % 2
                    op("pe", lambda e: e.transpose(psT[i][:kn, :m], mk[:m, k0:k0 + kn], Ib_t[:m, :m]), ["mk", "Ib"], [("psT", i)])
                    op("act", lambda e: e.copy(out=maskT[:kn, kt, q0:q0 + m], in_=psT[i][:kn, :m]), [("psT", i)], ["maskT"])
        S.barrier()
        kr = sb("kr", [128, T], BF16); kraw = sb("kraw", [128, T]); va = sb("va", [128, NKT, 132], BF16); va32 = sb("va32", [128, NKT, 128])
        qTh = sb("qTh", [128, NQ], BF16); wuq_t = sb("wuq_t", [128, 4, 128])
        qchunks = [(c, min(512, NQ - c)) for c in range(0, NQ, 512)]
        ex = [sb(f"ex{i}", [128, 512]) for i in range(2)]
        pp = [sb(f"pp{i}", [128, 512], BF16) for i in range(2)]
        ot = [sb(f"ot{i}", [128, 128]) for i in range(2)]
        rs_ = sb("rs_", [128, 1])
        scale = 128 ** -0.5
        outs = []
        op("pool", lambda e: e.memset(va[:, :, 128:129], 1.0), [], ["va1"])
        gi = 0; oi = 0
        for h in range(NH):
            S.dma("sp", kraw[:, :], kT[h * 128:(h + 1) * 128, :], w=["kraw"])
            nfull = T // 128
            S.dma("act", va32[:, :nfull, :], v[0:nfull * 128, h * 128:(h + 1) * 128].rearrange("(kt p) d -> p kt d", p=128), w=["va32"])
            op("act", lambda e: e.copy(out=va[:, :nfull, 0:128], in_=va32[:, :nfull, :]), ["va32"], ["va"])
            if T % 128:
                S.dma("act", va32[:T % 128, nfull, :], v[nfull * 128:T, h * 128:(h + 1) * 128], w=["va32"])
                op("act", lambda e: e.copy(out=va[:T % 128, nfull, 0:128], in_=va32[:T % 128, nfull, :]), ["va32"], ["va"])
            for c in range(0, T, 512):
                n = min(512, T - c)
                rope_k(kr[:, c:c + n], kraw[:, c:c + n], c, n, ["kraw"], ["kr"])
            S.dma("sp", wuq_t[:], w_uq[:, h * 128:(h + 1) * 128].rearrange("(kc p) d -> p kc d", p=128), w=["wuq"])
            for (c, n) in qchunks:
                i = (c // 512) % 2
                for kc in range(4):
                    op("pe", lambda e: e.matmul(psR[i][:, :n], lhsT=wuq_t[:, kc, :], rhs=cqnT[:, kc, c:c + n], start=(kc == 0), stop=(kc == 3)), ["wuq", "cqnT"], [("psR", i)])
                op("act", lambda e: e.copy(out=xT[:, :n], in_=psR[i][:, :n]), [("psR", i)], ["xT"])
                rope_fm(qTh[:, c:c + n], xT[:, :n], n, cosq_t[:, c:c + n], sinq_t[:, c:c + n], ["xT"], ["qTh"], ["cosq", "sinq"])
            for j, (q0, m, nk) in enumerate(slots):
                nkt = (nk + 127) // 128
                po = psO[oi % 2]; pok = ("psO", oi % 2)
                for g0 in range(0, nkt, 4):
                    gn = min(4, nkt - g0)
                    i = gi % 2; gi += 1
                    for kk_ in range(gn):
                        kt = g0 + kk_; k0 = kt * 128; kn = min(128, nk - k0)
                        op("pe", lambda e: e.matmul(psM[i][:kn, kk_ * 128:kk_ * 128 + m], lhsT=kr[:, k0:k0 + kn], rhs=qTh[:, q0:q0 + m], start=True, stop=True), ["kr", "qTh"], [("psM", i)])
                    kn_last = min(128, nk - (g0 + gn - 1) * 128)
                    pv = psM[i][:, :gn * 128].rearrange("p (a q) -> p a q", q=128)
                    ev = ex[i][:, :gn * 128].rearrange("p (a q) -> p a q", q=128)
                    ppv = pp[i][:, :gn * 128].rearrange("p (a q) -> p a q", q=128)
                    if kn_last == 128:
                        op("act", lambda e: e.activation(out=ev[:, :, :m], in_=pv[:, :, :m], func=AF.Exp, scale=scale), [("psM", i)], [("ex", i)])
                        op("dve", lambda e: e.tensor_tensor(out=ppv[:, :, :m], in0=ev[:, :, :m], in1=maskT[:, g0:g0 + gn, q0:q0 + m], op=ALU.mult), [("ex", i), "maskT"], [("pp", i)])
                    else:
                        if gn > 1:
                            op("act", lambda e: e.activation(out=ev[:, :gn - 1, :m], in_=pv[:, :gn - 1, :m], func=AF.Exp, scale=scale), [("psM", i)], [("ex", i)])
                            op("dve", lambda e: e.tensor_tensor(out=ppv[:, :gn - 1, :m], in0=ev[:, :gn - 1, :m], in1=maskT[:, g0:g0 + gn - 1, q0:q0 + m], op=ALU.mult), [("ex", i), "maskT"], [("pp", i)])
                        op("act", lambda e: e.activation(out=ev[:kn_last, gn - 1, :m], in_=pv[:kn_last, gn - 1, :m], func=AF.Exp, scale=scale), [("psM", i)], [("ex", i)])
                        op("dve", lambda e: e.tensor_tensor(out=ppv[:kn_last, gn - 1, :m], in0=ev[:kn_last, gn - 1, :m], in1=maskT[:kn_last, g0 + gn - 1, q0:q0 + m], op=ALU.mult), [("ex", i), "maskT"], [("pp", i)])
                    for kk_ in range(gn):
                        kt = g0 + kk_; kn = min(128, nk - kt * 128)
                        op("pe", lambda e: e.matmul(po[:m, :129], lhsT=ppv[:kn, kk_, :m], rhs=va[:kn, kt, :129], start=(kt == 0), stop=(kt == nkt - 1)), [("pp", i), "va", "va1"], [pok])
                o = ot[oi % 2]
                op("dve", lambda e: e.reciprocal(out=rs_[:m, :], in_=po[:m, 128:129]), [pok], ["rs_"])
                op("dve", lambda e: e.tensor_scalar(out=o[:m, :], in0=po[:m, :128], scalar1=rs_[:m, :], scalar2=None, op0=ALU.mult), [pok, "rs_"], [("ot", oi % 2)])
                outs.append(S.dma("pool", y[q0:q0 + m, h * 128:(h + 1) * 128], o[:m, :], r=[("ot", oi % 2)]))
                oi += 1
        S.finish(outs, "pool")
        print("ninst", S.ninst, "nsem", S.nsem)
    return nc


def rope_tables(pos):
    inv = (10000.0 ** (-np.arange(0, 128, 2, dtype=np.float32) / 128)).astype(np.float32)
    ang = (pos.astype(np.float32)[:, None] * inv[None, :]).astype(np.float32)
    c = np.cos(ang.astype(np.float64)).astype(np.float32); s = np.sin(ang.astype(np.float64)).astype(np.float32)
    cos_fm = np.concatenate([c, c], axis=1).T
    sin_fm = np.concatenate([-s, s], axis=1).T
    return np.ascontiguousarray(cos_fm), np.ascontiguousarray(sin_fm)


def dsa_consts(T):
    R = np.zeros((128, 128), np.float32)
    for i in range(64):
        R[i + 64, i] = 1; R[i, i + 64] = 1
    I = np.eye(128, dtype=np.float32)
    kpos = np.ascontiguousarray(np.broadcast_to(np.arange(T, dtype=np.float32)[None, :], (128, T)))
    return R, I, kpos


def core_qpos(r, NS):
    pos = []
    for j in range(NS):
        pos.extend(range(512 * j + 128 * r, 512 * j + 128 * r + 128))
    pos.extend(range(512 * NS + 4 * r, 512 * NS + 4 * r + 4))
    return np.array(pos)


def dsa_inmap(r, NS, cq, ka, va, kidx, widx, qnorm_g, w_uq, w_iq, kidx_g, kidx_b):
    T = cq.shape[0]
    pos = core_qpos(r, NS)
    R, I, kpos = dsa_consts(T)
    cos_fm, sin_fm = rope_tables(np.arange(T))
    cosq, sinq = rope_tables(pos)
    bc = lambda a: np.ascontiguousarray(np.broadcast_to(a[None, :], (128, a.shape[0])).astype(np.float32))
    return dict(cq=np.ascontiguousarray(cq[pos]), widx=np.ascontiguousarray(widx[pos]), qpos=pos.astype(np.float32)[:, None].copy(),
                kT=np.ascontiguousarray(ka.T), v=np.ascontiguousarray(va), kidx=np.ascontiguousarray(kidx),
                w_uq=np.ascontiguousarray(w_uq), w_iq=np.ascontiguousarray(w_iq),
                qg=np.ascontiguousarray(qnorm_g.reshape(4, 128).T), kig=bc(kidx_g), kib=bc(kidx_b),
                cos_fm=cos_fm, sin_fm=sin_fm, cosq=cosq, sinq=sinq, kpos=kpos, cR=R, cI=I)


WINS = (2, 4, 8, 16)

def build_pool(NT=1028):
    nc = new_nc()
    di = lambda n, s: nc.dram_tensor(n, s, F32, kind="ExternalInput").ap()
    pinT = di("pinT", [1024, NT + 15]); icnt = di("icnt", [4, 128, NT]); wp = di("wp", [4, 256, 256]); psc = di("psc", [128, 8])
    yT = nc.dram_tensor("yT", [1024, NT], F32, kind="ExternalOutput").ap()
    es = contextlib.ExitStack()
    with es:
        S = Sched(nc, es)
        sb = lambda n, s, d=F32: es.enter_context(nc.sbuf_tensor(n, s, d))
        W = NT + 15
        x = sb("x", [128, 8, W]); a = sb("a", [128, W]); b = sb("b", [128, W]); pl = sb("pl", [128, 8, NT])
        ic = sb("ic", [128, 4, NT]); wt = sb("wt", [128, 4, 2, 256]); sc = sb("sc", [128, 8]); o = [sb(f"o{i}", [128, NT]) for i in range(2)]
        ps = [es.enter_context(nc.psum_tensor(f"ps{i}", [128, 512], F32)) for i in range(2)]
        S.dma("sp", x[:], pinT.rearrange("(c p) t -> p c t", p=128), w=["x"])
        S.dma("act", ic[:], icnt.rearrange("w p t -> p w t"), w=["ic"])
        S.dma("act", wt[:], wp.rearrange("g (kc p) d -> p g kc d", p=128), w=["wt"])
        S.dma("sp", sc[:], psc, w=["sc"])
        for c in range(8):
            g = c // 2; win = WINS[g]
            src = x[:, c, :]; sh = 1; cur = None
            bufs = [a, b]; bi = 0
            prev = src
            while sh < win:
                dst = bufs[bi]; bi ^= 1
                op_eng = "dve" if c % 2 == 0 else "pool"
                S.op(op_eng, lambda e: e.tensor_tensor(out=dst[:, sh:W], in0=prev[:, sh:W], in1=prev[:, 0:W - sh], op=ALU.add), r=["x", "a", "b"], w=["a" if dst is a else "b"])
                if sh > 0:
                    S.op(op_eng, lambda e: e.tensor_copy(out=dst[:, 0:sh], in_=prev[:, 0:sh]), r=["x", "a", "b"], w=["a" if dst is a else "b"])
                prev = dst; sh *= 2
            S.op("dve", lambda e: e.tensor_tensor(out=pl[:, c, :], in0=prev[:, 15:W], in1=ic[:, g, :], op=ALU.mult), r=["a", "b", "ic"], w=[("pl", c)])
            S.op("dve", lambda e: e.tensor_tensor(out=pl[:, c, :], in0=pl[:, c, :], in1=x[:, c, 15:W], op=ALU.subtract), r=[("pl", c), "x"], w=[("pl", c)])
        outs = []
        k = 0
        for g in range(4):
            for oc in range(2):
                ot = o[k % 2]
                for t0 in range(0, NT, 512):
                    n = min(512, NT - t0)
                    p = ps[(k + t0 // 512) % 2]; pk = ("ps", (k + t0 // 512) % 2)
                    for kc in range(2):
                        S.op("pe", lambda e: e.matmul(p[:, :n], lhsT=wt[:, g, kc, oc * 128:(oc + 1) * 128], rhs=pl[:, 2 * g + kc, t0:t0 + n], start=(kc == 0), stop=(kc == 1)),
                             r=["wt", ("pl", 2 * g + kc)], w=[pk])
                    S.op("act", lambda e: e.activation(out=ot[:, t0:t0 + n], in_=p[:, :n], func=AF.Copy, scale=sc[:, 2 * g + oc:2 * g + oc + 1]), r=[pk, "sc"], w=[("o", k % 2)])
                outs.append(S.dma("pool", yT[(2 * g + oc) * 128:(2 * g + oc + 1) * 128, :], ot[:], r=[("o", k % 2)]))
                k += 1
        S.finish(outs, "pool")
    return nc

def pool_inmap(pin_b, t0, NT, pool_w, pool_scale):
    T = pin_b.shape[0]
    pad = np.concatenate([np.zeros((15, 1024), np.float32), pin_b], axis=0)
    sl = pad[t0:t0 + NT + 15]
    tt = np.arange(t0, t0 + NT)
    icnt = np.stack([np.broadcast_to((1.0 / np.minimum(tt + 1, w)).astype(np.float32)[None, :], (128, NT)) for w in WINS])
    return dict(pinT=np.ascontiguousarray(sl.T), icnt=np.ascontiguousarray(icnt), wp=np.ascontiguousarray(pool_w),
                psc=np.ascontiguousarray(pool_scale.reshape(8, 128).T))


import math

ALPHA = 2.0 ** 0.5
LN_EPS = 1e-5
NEGB = -1.0e30


def ln_rows(S, u, m, D, gb, bb, out, sq, st, uk, ok):
    S.op("dve", lambda e: e.tensor_reduce(out=st[:m, 0:1], in_=u[:m, :], axis=AX.X, op=ALU.add), r=[uk], w=["ln_st"])
    S.op("dve", lambda e: e.tensor_scalar(out=st[:m, 1:2], in0=st[:m, 0:1], scalar1=-1.0 / D, scalar2=None, op0=ALU.mult), r=["ln_st"], w=["ln_st"])
    S.op("dve", lambda e: e.tensor_scalar(out=u[:m, :], in0=u[:m, :], scalar1=st[:m, 1:2], scalar2=None, op0=ALU.add), r=[uk, "ln_st"], w=[uk])
    S.op("pool", lambda e: e.tensor_tensor(out=sq[:m, :], in0=u[:m, :], in1=u[:m, :], op=ALU.mult), r=[uk], w=["ln_sq"])
    S.op("dve", lambda e: e.tensor_reduce(out=st[:m, 0:1], in_=sq[:m, :], axis=AX.X, op=ALU.add), r=["ln_sq"], w=["ln_st"])
    S.op("dve", lambda e: e.tensor_scalar(out=st[:m, 1:2], in0=st[:m, 0:1], scalar1=1.0 / D, scalar2=LN_EPS, op0=ALU.mult, op1=ALU.add), r=["ln_st"], w=["ln_st"])
    S.op("act", lambda e: e.activation(out=st[:m, 2:3], in_=st[:m, 1:2], func=AF.Sqrt), r=["ln_st"], w=["ln_st"])
    S.op("dve", lambda e: e.reciprocal(out=st[:m, 3:4], in_=st[:m, 2:3]), r=["ln_st"], w=["ln_st"])
    S.op("dve", lambda e: e.scalar_tensor_tensor(out=sq[:m, :], in0=u[:m, :], scalar=st[:m, 3:4], in1=gb[:m, :], op0=ALU.mult, op1=ALU.mult), r=[uk, "ln_st", "lngb"], w=["ln_sq"])
    S.op("pool", lambda e: e.tensor_tensor(out=out[:m, :], in0=sq[:m, :], in1=bb[:m, :], op=ALU.add), r=["ln_sq", "lngb"], w=[ok])


def build_s3(NT=1028, D=4096, CAP=192, cb=256):
    nc = new_nc()
    KC = D // 128
    di = lambda n, s, d=F32: nc.dram_tensor(n, s, d, kind="ExternalInput").ap()
    do = lambda n, s, d=F32: nc.dram_tensor(n, s, d, kind="ExternalOutput").ap()
    mixT = di("mixT", [D, NT]); hrow = di("hrow", [NT, D]); w_out = di("w_out", [D, D])
    lng = di("lng", [128, D]); lnb = di("lnb", [128, D]); wr = di("wr", [D, 72]); rbias = di("rbias", [128, 72])
    cU = di("cU", [128, 128]); cOnes = di("cOnes", [128, 128]); cIota = di("cIota", [128, 8]); cI = di("cI", [128, 128])
    h1 = do("h1", [NT, D]); xg = do("xg", [8 * CAP, D]); gsc = do("gsc", [8 * CAP, 8]); idx = do("idx", [NT, 1], I32)
    z = nc.dram_tensor("z", [NT, D], F32, kind="Internal").ap()
    toks = [(t0, min(128, NT - t0)) for t0 in range(0, NT, 128)]
    es = contextlib.ExitStack()
    with es:
        S = Sched(nc, es)
        sb = lambda n, s, d=F32, st=es: st.enter_context(nc.sbuf_tensor(n, s, d))
        ps = [es.enter_context(nc.psum_tensor(f"ps{i}", [128, 512], F32)) for i in range(4)]
        pa = contextlib.ExitStack()
        with pa:
            mm_phase(nc, S, pa, mixT, w_out, z, NT, D, D, ps, cb, dst_key="z")
        S.barrier()
        gb = sb("gb", [128, D]); bb = sb("bb", [128, D]); zt = sb("zt", [128, D]); ht = sb("ht", [128, D]); sq = sb("sq", [128, D]); h1t = sb("h1t", [128, D])
        h1T = sb("h1T", [128, KC, 128]); wr_t = sb("wr_t", [128, KC, 72]); rb_t = sb("rb_t", [128, 72])
        U_t = sb("U_t", [128, 128]); On_t = sb("On_t", [128, 128]); Io_t = sb("Io_t", [128, 8]); I_t = sb("I_t", [128, 128]); zero = sb("zero", [128, D])
        st = sb("st", [128, 4]); lg = sb("lg", [128, 72]); sm = sb("sm", [128, 16]); goh = sb("goh", [128, 8]); ex8 = sb("ex8", [128, 8])
        esel = sb("esel", [128, 8]); e2 = sb("e2", [128, 8]); oh1 = sb("oh1", [128, 8]); oh2 = sb("oh2", [128, 8]); gvec = sb("gvec", [128, 8]); t8 = sb("t8", [128, 8])
        carry = sb("carry", [128, 8]); pos8 = sb("pos8", [128, 8]); idx_t = sb("idx_t", [128, 1], I32)
        S.dma("sp", gb[:], lng, w=["lngb"]); S.dma("act", bb[:], lnb, w=["lngb"])
        S.dma("sp", wr_t[:], wr.rearrange("(kc p) n -> p kc n", p=128), w=["wr"]); S.dma("act", rb_t[:], rbias, w=["rb"])
        S.dma("sp", U_t[:], cU, w=["U"]); S.dma("act", On_t[:], cOnes, w=["On"]); S.dma("sp", Io_t[:], cIota, w=["Io"]); S.dma("act", I_t[:], cI, w=["I"])
        S.op("pool", lambda e: e.memset(zero[:], 0.0), w=["zero"])
        S.op("pool", lambda e: e.memset(carry[:], 0.0), w=["carry"])
        for r0 in range(0, 8 * CAP, 128):
            S.dma("sp" if (r0 // 128) % 2 == 0 else "act", xg[r0:r0 + 128, :], zero[:], r=["zero"], w=["xg"])
        S.dma("sp", gsc.rearrange("(a p) e -> p a e", p=128), zero[:, :8 * CAP // 128 * 8].rearrange("p (a e) -> p a e", e=8), r=["zero"], w=["gsc"])
        outs = []
        for (t0, m) in toks:
            S.dma("sp", zt[:m, :], z[t0:t0 + m, :], r=[("z", t0)], w=["zt"])
            S.dma("act", ht[:m, :], hrow[t0:t0 + m, :], w=["ht"])
            S.op("dve", lambda e: e.scalar_tensor_tensor(out=zt[:m, :], in0=ht[:m, :], scalar=ALPHA, in1=zt[:m, :], op0=ALU.mult, op1=ALU.add), r=["ht", "zt"], w=["zt"])
            ln_rows(S, zt, m, D, gb, bb, h1t, sq, st, "zt", "h1t")
            outs.append(S.dma("sp", h1[t0:t0 + m, :], h1t[:m, :], r=["h1t"]))
            for k4 in range(0, KC, 4):
                p = ps[(k4 // 4) % 4]; pk = ("ps", (k4 // 4) % 4)
                for kk in range(4):
                    kc = k4 + kk
                    S.op("pe", lambda e: e.transpose(p[:, kk * 128:kk * 128 + m], h1t[:m, kc * 128:(kc + 1) * 128], I_t[:m, :m]), r=["h1t", "I"], w=[pk])
                pv = p[:, :].rearrange("p (a q) -> p a q", q=128)
                S.op("act" if (k4 // 4) % 2 == 0 else "dve", (lambda e: e.copy(out=h1T[:, k4:k4 + 4, :m], in_=pv[:, :, :m])) if (k4 // 4) % 2 == 0 else (lambda e: e.tensor_copy(out=h1T[:, k4:k4 + 4, :m], in_=pv[:, :, :m])), r=[pk], w=["h1T"])
            pl = ps[0]
            for kc in range(KC):
                S.op("pe", lambda e: e.matmul(pl[:m, :72], lhsT=h1T[:, kc, :m], rhs=wr_t[:, kc, :], start=(kc == 0), stop=(kc == KC - 1)), r=["h1T", "wr"], w=[("ps", 0)])
            S.op("dve", lambda e: e.tensor_tensor(out=lg[:m, :], in0=pl[:m, :72], in1=rb_t[:m, :], op=ALU.add), r=[("ps", 0), "rb"], w=["lg"])
            D_ = lambda fn, r, w: S.op("dve", fn, r=r, w=w)
            D_(lambda e: e.tensor_reduce(out=sm[:m, 0:1], in_=lg[:m, 0:8], axis=AX.X, op=ALU.max), ["lg"], ["sm"])
            D_(lambda e: e.tensor_scalar(out=goh[:m, :], in0=lg[:m, 0:8], scalar1=sm[:m, 0:1], scalar2=None, op0=ALU.is_equal), ["lg", "sm"], ["goh"])
            D_(lambda e: e.tensor_scalar(out=sm[:m, 1:2], in0=sm[:m, 0:1], scalar1=-1.0, scalar2=None, op0=ALU.mult), ["sm"], ["sm"])
            S.op("act", lambda e: e.activation(out=ex8[:m, :], in_=lg[:m, 0:8], func=AF.Exp, bias=sm[:m, 1:2]), r=["lg", "sm"], w=["ex8"])
            D_(lambda e: e.tensor_reduce(out=sm[:m, 2:3], in_=ex8[:m, :], axis=AX.X, op=ALU.add), ["ex8"], ["sm"])
            D_(lambda e: e.reciprocal(out=sm[:m, 3:4], in_=sm[:m, 2:3]), ["sm"], ["sm"])
            for g in range(8):
                if g == 0:
                    D_(lambda e: e.tensor_scalar(out=esel[:m, :], in0=lg[:m, 8:16], scalar1=goh[:m, 0:1], scalar2=None, op0=ALU.mult), ["lg", "goh"], ["esel"])
                else:
                    D_(lambda e: e.scalar_tensor_tensor(out=esel[:m, :], in0=lg[:m, 8 + 8 * g:16 + 8 * g], scalar=goh[:m, g:g + 1], in1=esel[:m, :], op0=ALU.mult, op1=ALU.add), ["lg", "goh", "esel"], ["esel"])
            D_(lambda e: e.tensor_reduce(out=sm[:m, 4:5], in_=esel[:m, :], axis=AX.X, op=ALU.max), ["esel"], ["sm"])
            D_(lambda e: e.tensor_scalar(out=oh1[:m, :], in0=esel[:m, :], scalar1=sm[:m, 4:5], scalar2=None, op0=ALU.is_equal), ["esel", "sm"], ["oh1"])
            D_(lambda e: e.scalar_tensor_tensor(out=e2[:m, :], in0=oh1[:m, :], scalar=NEGB, in1=esel[:m, :], op0=ALU.mult, op1=ALU.add), ["oh1", "esel"], ["e2"])
            D_(lambda e: e.tensor_reduce(out=sm[:m, 5:6], in_=e2[:m, :], axis=AX.X, op=ALU.max), ["e2"], ["sm"])
            D_(lambda e: e.tensor_scalar(out=oh2[:m, :], in0=e2[:m, :], scalar1=sm[:m, 5:6], scalar2=None, op0=ALU.is_equal), ["e2", "sm"], ["oh2"])
            D_(lambda e: e.tensor_tensor(out=sm[:m, 6:7], in0=sm[:m, 5:6], in1=sm[:m, 4:5], op=ALU.subtract), ["sm"], ["sm"])
            S.op("act", lambda e: e.activation(out=sm[:m, 7:8], in_=sm[:m, 6:7], func=AF.Exp), r=["sm"], w=["sm"])
            D_(lambda e: e.tensor_scalar(out=sm[:m, 8:9], in0=sm[:m, 7:8], scalar1=1.0, scalar2=None, op0=ALU.add), ["sm"], ["sm"])
            D_(lambda e: e.reciprocal(out=sm[:m, 9:10], in_=sm[:m, 8:9]), ["sm"], ["sm"])
            D_(lambda e: e.tensor_tensor(out=sm[:m, 10:11], in0=sm[:m, 9:10], in1=sm[:m, 3:4], op=ALU.mult), ["sm"], ["sm"])
            D_(lambda e: e.tensor_tensor(out=sm[:m, 11:12], in0=sm[:m, 3:4], in1=sm[:m, 10:11], op=ALU.subtract), ["sm"], ["sm"])
            D_(lambda e: e.tensor_scalar(out=t8[:m, :], in0=oh1[:m, :], scalar1=sm[:m, 10:11], scalar2=None, op0=ALU.mult), ["oh1", "sm"], ["t8"])
            D_(lambda e: e.scalar_tensor_tensor(out=gvec[:m, :], in0=oh2[:m, :], scalar=sm[:m, 11:12], in1=t8[:m, :], op0=ALU.mult, op1=ALU.add), ["oh2", "sm", "t8"], ["gvec"])
            D_(lambda e: e.tensor_tensor(out=t8[:m, :], in0=goh[:m, :], in1=Io_t[:m, :], op=ALU.mult), ["goh", "Io", "gvec"], ["t8"])
            D_(lambda e: e.tensor_reduce(out=sm[:m, 12:13], in_=t8[:m, :], axis=AX.X, op=ALU.add), ["t8"], ["sm"])
            S.op("pe", lambda e: e.matmul(ps[1][:m, 0:8], lhsT=U_t[:m, :m], rhs=goh[:m, :], start=True, stop=True), r=["U", "goh"], w=[("ps", 1)])
            S.op("pe", lambda e: e.matmul(ps[2][:, 0:8], lhsT=On_t[:m, :], rhs=goh[:m, :], start=True, stop=True), r=["On", "goh"], w=[("ps", 2)])
            D_(lambda e: e.tensor_tensor(out=pos8[:m, :], in0=ps[1][:m, 0:8], in1=carry[:m, :], op=ALU.add), [("ps", 1), "carry"], ["pos8"])
            D_(lambda e: e.tensor_tensor(out=carry[:, :], in0=carry[:, :], in1=ps[2][:, 0:8], op=ALU.add), [("ps", 2), "carry"], ["carry"])
            D_(lambda e: e.tensor_tensor(out=t8[:m, :], in0=goh[:m, :], in1=pos8[:m, :], op=ALU.mult), ["goh", "pos8", "sm"], ["t8"])
            D_(lambda e: e.tensor_reduce(out=sm[:m, 13:14], in_=t8[:m, :], axis=AX.X, op=ALU.add), ["t8"], ["sm"])
            D_(lambda e: e.tensor_scalar(out=sm[:m, 14:15], in0=sm[:m, 13:14], scalar1=float(CAP), scalar2=1.0e6, op0=ALU.is_ge, op1=ALU.mult), ["sm"], ["sm"])
            D_(lambda e: e.scalar_tensor_tensor(out=sm[:m, 15:16], in0=sm[:m, 12:13], scalar=float(CAP), in1=sm[:m, 13:14], op0=ALU.mult, op1=ALU.add), ["sm"], ["sm"])
            D_(lambda e: e.tensor_tensor(out=sm[:m, 15:16], in0=sm[:m, 15:16], in1=sm[:m, 14:15], op=ALU.add), ["sm"], ["sm"])
            D_(lambda e: e.tensor_copy(out=idx_t[:m, :], in_=sm[:m, 15:16]), ["sm"], ["idx_t"])
            outs.append(S.dma("pool", None, None, r=["h1t", "idx_t"], w=["xg"], fn=lambda e: e.indirect_dma_start(
                out=xg[:, :], out_offset=bass.IndirectOffsetOnAxis(ap=idx_t[:m, 0:1], axis=0), in_=h1t[:m, :], in_offset=None, bounds_check=8 * CAP - 1, oob_is_err=False)))
            outs.append(S.dma("pool", None, None, r=["gvec", "idx_t"], w=["gsc"], fn=lambda e: e.indirect_dma_start(
                out=gsc[:, :], out_offset=bass.IndirectOffsetOnAxis(ap=idx_t[:m, 0:1], axis=0), in_=gvec[:m, :], in_offset=None, bounds_check=8 * CAP - 1, oob_is_err=False)))
            outs.append(S.dma("sp", idx[t0:t0 + m, :], idx_t[:m, :], r=["idx_t"]))
        S.finish(outs, "pool")
        print("s3 ninst", S.ninst)
    return nc


def build_E(R=1536, D=4096, DE=512, RB=512):
    nc = new_nc()
    KC = D // 128; DC = DE // 128; NE = 8
    di = lambda n, s, d=F32: nc.dram_tensor(n, s, d, kind="ExternalInput").ap()
    X = di("X", [R, D]); G = di("G", [R, NE]); w1 = di("w1", [NE, D, DE]); w3 = di("w3", [NE, D, DE]); w2 = di("w2", [NE, DE, D]); cI = di("cI", [128, 128])
    Y = nc.dram_tensor("Y", [R, D], F32, kind="ExternalOutput").ap()
    RT = RB // 128
    es = contextlib.ExitStack()
    with es:
        S = Sched(nc, es)
        sb = lambda n, s, d=F32: es.enter_context(nc.sbuf_tensor(n, s, d))
        pA = [es.enter_context(nc.psum_tensor(f"pA{i}", [128, 512], F32)) for i in range(2)]
        pB = [es.enter_context(nc.psum_tensor(f"pB{i}", [128, 512], F32)) for i in range(2)]
        pY = [es.enter_context(nc.psum_tensor(f"pY{i}", [128, 512], F32)) for i in range(2)]
        pT = [es.enter_context(nc.psum_tensor(f"pT{i}", [128, 512], F32)) for i in range(2)]
        xr = [sb("xr0", [128, D])]; XT = sb("XT", [128, KC, RB], BF16); Ya = sb("Ya", [128, RT, D]); g_t = sb("g_t", [128, RT, NE])
        hT = sb("hT", [128, DC, RB], BF16); sl = sb("sl", [128, RB]); I_t = sb("I_t", [128, 128])
        w1s = sb("w1s", [128, KC, 128]); w3s = sb("w3s", [128, KC, 128]); w2s = sb("w2s", [128, DC, 512])
        w1b = [sb(f"w1b{i}", [128, KC, 128], BF16) for i in range(2)]; w3b = [sb(f"w3b{i}", [128, KC, 128], BF16) for i in range(2)]
        w2b = [sb(f"w2b{i}", [128, DC, 512], BF16) for i in range(2)]
        S.dma("sp", I_t[:], cI, w=["I"])
        outs = []
        wi = 0; w2i = 0; ti = 0; yi = 0; xi = 0
        for r0 in range(0, R, RB):
            S.dma("act", g_t[:], G[r0:r0 + RB, :].rearrange("(a p) e -> p a e", p=128), w=["g"])
            for a in range(RT):
                xb_ = xr[0]; xk = ("xr", 0); xi += 1
                S.dma("sp", xb_[:], X[r0 + a * 128:r0 + (a + 1) * 128, :], w=[xk])
                for k4 in range(0, KC, 4):
                    p = pT[ti % 2]; pk = ("pT", ti % 2)
                    for kk in range(4):
                        S.op("pe", lambda e: e.transpose(p[:, kk * 128:(kk + 1) * 128], xb_[:, (k4 + kk) * 128:(k4 + kk + 1) * 128], I_t[:]), r=[xk, "I"], w=[pk])
                    pv = p[:, :].rearrange("p (k q) -> p k q", q=128)
                    if ti % 2 == 0:
                        S.op("act", lambda e: e.copy(out=XT[:, k4:k4 + 4, a * 128:(a + 1) * 128], in_=pv), r=[pk], w=["XT"])
                    else:
                        S.op("dve", lambda e: e.tensor_copy(out=XT[:, k4:k4 + 4, a * 128:(a + 1) * 128], in_=pv), r=[pk], w=["XT"])
                    ti += 1
            for ex in range(NE):
                for dc in range(DC):
                    i = wi % 2; wi += 1
                    S.dma("sp", w1s[:], w1[ex, :, dc * 128:(dc + 1) * 128].rearrange("(kc p) d -> p kc d", p=128), w=["w1s"])
                    S.dma("act", w3s[:], w3[ex, :, dc * 128:(dc + 1) * 128].rearrange("(kc p) d -> p kc d", p=128), w=["w3s"])
                    S.op("dve", lambda e: e.tensor_copy(out=w1b[i][:], in_=w1s[:]), r=["w1s"], w=[("w1b", i)])
                    S.op("act", lambda e: e.copy(out=w3b[i][:], in_=w3s[:]), r=["w3s"], w=[("w3b", i)])
                    for kc in range(KC):
                        S.op("pe", lambda e: e.matmul(pA[i][:, :RB], lhsT=w1b[i][:, kc, :], rhs=XT[:, kc, :], start=(kc == 0), stop=(kc == KC - 1)), r=[("w1b", i), "XT"], w=[("pA", i)])
                    for kc in range(KC):
                        S.op("pe", lambda e: e.matmul(pB[i][:, :RB], lhsT=w3b[i][:, kc, :], rhs=XT[:, kc, :], start=(kc == 0), stop=(kc == KC - 1)), r=[("w3b", i), "XT"], w=[("pB", i)])
                    S.op("act", lambda e: e.activation(out=sl[:, :], in_=pA[i][:, :RB], func=AF.Silu), r=[("pA", i)], w=["sl"])
                    S.op("dve", lambda e: e.tensor_tensor(out=hT[:, dc, :], in0=sl[:, :], in1=pB[i][:, :RB], op=ALU.mult), r=["sl", ("pB", i)], w=["hT"])
                for cb in range(D // 512):
                    i = w2i % 2; w2i += 1
                    S.dma("sp" if cb % 2 == 0 else "act", w2s[:], w2[ex, :, cb * 512:(cb + 1) * 512].rearrange("(kc p) n -> p kc n", p=128), w=["w2s"])
                    if cb % 2 == 0:
                        S.op("act", lambda e: e.copy(out=w2b[i][:], in_=w2s[:]), r=["w2s"], w=[("w2b", i)])
                    else:
                        S.op("dve", lambda e: e.tensor_copy(out=w2b[i][:], in_=w2s[:]), r=["w2s"], w=[("w2b", i)])
                    for a in range(RT):
                        p = pY[yi % 2]; pk = ("pY", yi % 2); yi += 1
                        for kc in range(DC):
                            S.op("pe", lambda e: e.matmul(p[:, :], lhsT=hT[:, kc, a * 128:(a + 1) * 128], rhs=w2b[i][:, kc, :], start=(kc == 0), stop=(kc == DC - 1)), r=["hT", ("w2b", i)], w=[pk])
                        if ex == 0:
                            S.op("dve", lambda e: e.tensor_scalar(out=Ya[:, a, cb * 512:(cb + 1) * 512], in0=p[:, :], scalar1=g_t[:, a, ex:ex + 1], scalar2=None, op0=ALU.mult), r=[pk, "g"], w=[("Ya", a, cb)])
                        else:
                            S.op("dve", lambda e: e.scalar_tensor_tensor(out=Ya[:, a, cb * 512:(cb + 1) * 512], in0=p[:, :], scalar=g_t[:, a, ex:ex + 1], in1=Ya[:, a, cb * 512:(cb + 1) * 512], op0=ALU.mult, op1=ALU.add),
                                 r=[pk, "g", ("Ya", a, cb)], w=[("Ya", a, cb)])
            outs.append(S.dma("pool", Y[r0:r0 + RB, :].rearrange("(a p) d -> p a d", p=128), Ya[:], r=[("Ya", a, cb) for a in range(RT) for cb in range(D // 512)]))
        S.finish(outs, "pool")
        print("E ninst", S.ninst)
    return nc


def build_C(NT=1028, D=4096, CAP=192):
    nc = new_nc()
    di = lambda n, s, d=F32: nc.dram_tensor(n, s, d, kind="ExternalInput").ap()
    Yg = di("Yg", [8 * CAP, D]); idx = di("idx", [NT, 1], I32); h1 = di("h1", [NT, D]); lng = di("lng", [128, D]); lnb = di("lnb", [128, D])
    h2 = nc.dram_tensor("h2", [NT, D], F32, kind="ExternalOutput").ap()
    es = contextlib.ExitStack()
    with es:
        S = Sched(nc, es)
        sb = lambda n, s, d=F32: es.enter_context(nc.sbuf_tensor(n, s, d))
        gb = sb("gb", [128, D]); bb = sb("bb", [128, D]); ff = sb("ff", [128, D]); ht = sb("ht", [128, D]); sq = sb("sq", [128, D]); ot = sb("ot", [128, D])
        st = sb("st", [128, 4]); idx_t = sb("idx_t", [128, 1], I32)
        S.dma("sp", gb[:], lng, w=["lngb"]); S.dma("act", bb[:], lnb, w=["lngb"])
        outs = []
        for t0 in range(0, NT, 128):
            m = min(128, NT - t0)
            S.dma("sp", idx_t[:m, :], idx[t0:t0 + m, :], w=["idx_t"])
            S.dma("act", ht[:m, :], h1[t0:t0 + m, :], w=["ht"])
            S.dma("pool", None, None, r=["idx_t"], w=["ff"], fn=lambda e: e.indirect_dma_start(
                out=ff[:m, :], out_offset=None, in_=Yg[:, :], in_offset=bass.IndirectOffsetOnAxis(ap=idx_t[:m, 0:1], axis=0), bounds_check=8 * CAP - 1, oob_is_err=False))
            S.op("dve", lambda e: e.scalar_tensor_tensor(out=ff[:m, :], in0=ht[:m, :], scalar=ALPHA, in1=ff[:m, :], op0=ALU.mult, op1=ALU.add), r=["ht", "ff"], w=["ff"])
            ln_rows(S, ff, m, D, gb, bb, ot, sq, st, "ff", "ot")
            outs.append(S.dma("sp", h2[t0:t0 + m, :], ot[:m, :], r=["ot"]))
        S.finish(outs, "sp")
    return nc


def s3_consts():
    U = np.triu(np.ones((128, 128), np.float32), 1)
    return dict(cU=U, cOnes=np.ones((128, 128), np.float32), cIota=np.ascontiguousarray(np.broadcast_to(np.arange(8, dtype=np.float32)[None, :], (128, 8))),
                cI=np.eye(128, dtype=np.float32))

def bc128(a):
    return np.ascontiguousarray(np.broadcast_to(a[None, :], (128, a.shape[0])).astype(np.float32))


B_, SEQ_, D_, NMETA_ = 2, 4096, 4096, 16
T_ = SEQ_ + NMETA_
NT_ = T_ // 4
CAP_ = 192
_NC = {}

def _get(name, fn):
    if name not in _NC:
        t0 = time.time()
        _NC[name] = fn()
        print(f"[kernel] built {name} in {time.time() - t0:.1f}s", flush=True)
    return _NC[name]

def _run(name, nc, ims):
    t0 = time.time()
    res = run_bass_kernel_spmd(nc, ims, core_ids=list(range(8))).results
    print(f"[kernel] ran {name} in {time.time() - t0:.1f}s", flush=True)
    return res

def kernel(x, meta, w_in, rw_mu, rw_w2, rw_w0, rw_a2, rw_a0, rw_g2, rw_kk, rw_ka, rw_rk,
           rw_gn_g, rw_gn_b, at_qnorm_g, at_w_uq, at_w_iq, at_kidx_g, at_kidx_b, pool_w,
           pool_scale, w_out, ln1_g, ln1_b, router_g_w, router_g_b, router_e_w, router_e_b,
           exp_w1, exp_w3, exp_w2, ln2_g, ln2_b):
    A = lambda a: np.ascontiguousarray(np.asarray(a, dtype=np.float32))
    x = np.asarray(x, np.float32); meta = np.asarray(meta, np.float32)
    h = np.concatenate([np.broadcast_to(meta[None], (B_, NMETA_, D_)), x], axis=1).reshape(B_ * T_, D_)
    cs = s3_consts()
    for l in range(2):
        nc = _get("mm", lambda: build_mm(NT_, D_, 10088))
        wl = A(w_in[l])
        res = _run("proj", nc, [dict(xT=A(h[c * NT_:(c + 1) * NT_].T), w=wl) for c in range(8)])
        proj = np.concatenate([res[c]["out"] for c in range(8)]).reshape(B_, T_, 10088)
        del res, wl
        o = 0
        def take(n):
            nonlocal o
            s = slice(o, o + n); o += n
            return s
        s_r, s_k, s_v, s_dw, s_da, s_dg = take(1536), take(1536), take(1536), take(128), take(128), take(480)
        s_cq, s_ka, s_va, s_kidx, s_widx, s_pin = take(512), take(1536), take(1536), take(128), take(8), take(1024)
        mu = np.asarray(rw_mu[l], np.float32)
        nc = _get("rwkv", lambda: build_rwkv(T_, same=True))
        ims = []
        for c in range(8):
            b, hq = c // 4, c % 4
            ch = slice(hq * 384, (hq + 1) * 384)
            P = proj[b]
            sub = lambda s: P[:, s][:, ch]
            ims.append(rwkv_inmap(sub(s_r), sub(s_k), sub(s_v), P[:, s_dw], P[:, s_da], P[:, s_dg],
                                  mu[s_r][ch], mu[s_k][ch], mu[s_v][ch], mu[s_dw], mu[s_da], mu[s_dg],
                                  np.asarray(rw_w2[l])[:, ch], np.asarray(rw_w0[l])[ch], np.asarray(rw_a2[l])[:, ch], np.asarray(rw_a0[l])[ch],
                                  np.asarray(rw_g2[l])[:, ch], np.asarray(rw_kk[l])[ch], np.asarray(rw_ka[l])[ch], np.asarray(rw_rk[l]).reshape(-1)[ch],
                                  np.asarray(rw_gn_g[l])[ch], np.asarray(rw_gn_b[l])[ch]))
            ims[-1] = {k: A(v) for k, v in ims[-1].items()}
        res = _run("rwkv", nc, ims)
        mix = np.empty((B_, T_, D_), np.float32)
        for c in range(8):
            b, hq = c // 4, c % 4
            mix[b, :, hq * 384:(hq + 1) * 384] = res[c]["yT"].T
        del res, ims
        nc = _get("dsa", lambda: build_dsa(8, 12))
        ims = []
        for c in range(8):
            b, r = c // 4, c % 4
            P = proj[b]
            im = dsa_inmap(r, 8, P[:, s_cq], P[:, s_ka], P[:, s_va], P[:, s_kidx], P[:, s_widx], np.asarray(at_qnorm_g[l], np.float32),
                           np.asarray(at_w_uq[l], np.float32), np.asarray(at_w_iq[l], np.float32), np.asarray(at_kidx_g[l], np.float32), np.asarray(at_kidx_b[l], np.float32))
            ims.append({k: A(v) for k, v in im.items()})
        res = _run("dsa", nc, ims)
        for c in range(8):
            b, r = c // 4, c % 4
            mix[b, core_qpos(r, 8), 1536:3072] = res[c]["y"]
        del res, ims
        nc = _get("pool", lambda: build_pool(NT_))
        ims = []
        for c in range(8):
            b, r = c // 4, c % 4
            im = pool_inmap(proj[b][:, s_pin], r * NT_, NT_, np.asarray(pool_w[l], np.float32), np.asarray(pool_scale[l], np.float32))
            ims.append({k: A(v) for k, v in im.items()})
        res = _run("pool", nc, ims)
        for c in range(8):
            b, r = c // 4, c % 4
            mix[b, r * NT_:(r + 1) * NT_, 3072:4096] = res[c]["yT"].T
        del res, ims, proj
        mix = mix.reshape(B_ * T_, D_)
        nc = _get("s3", lambda: build_s3(NT_, D_, CAP_))
        wo = A(w_out[l]); g1 = bc128(np.asarray(ln1_g[l])); b1 = bc128(np.asarray(ln1_b[l]))
        wr = A(np.concatenate([np.asarray(router_g_w[l]), np.asarray(router_e_w[l])], axis=1))
        rbias = bc128(np.concatenate([np.asarray(router_g_b[l]), np.asarray(router_e_b[l])]))
        ims = [dict(mixT=A(mix[c * NT_:(c + 1) * NT_].T), hrow=A(h[c * NT_:(c + 1) * NT_]), w_out=wo, lng=g1, lnb=b1, wr=wr, rbias=rbias, **cs) for c in range(8)]
        r3 = _run("s3", nc, ims)
        del ims, mix, wo
        nc = _get("E", lambda: build_E(8 * CAP_, D_, 512))
        ims = [dict(X=A(np.concatenate([r3[s]["xg"][g * CAP_:(g + 1) * CAP_] for s in range(8)])),
                    G=A(np.concatenate([r3[s]["gsc"][g * CAP_:(g + 1) * CAP_] for s in range(8)])),
                    w1=A(exp_w1[l][8 * g:8 * g + 8]), w3=A(exp_w3[l][8 * g:8 * g + 8]), w2=A(exp_w2[l][8 * g:8 * g + 8]), cI=cs["cI"]) for g in range(8)]
        rE = _run("E", nc, ims)
        del ims
        nc = _get("C", lambda: build_C(NT_, D_, CAP_))
        g2 = bc128(np.asarray(ln2_g[l])); b2 = bc128(np.asarray(ln2_b[l]))
        ims = [dict(Yg=A(np.concatenate([rE[g]["Y"][s * CAP_:(s + 1) * CAP_] for g in range(8)])), idx=np.ascontiguousarray(r3[s]["idx"]),
                    h1=A(r3[s]["h1"]), lng=g2, lnb=b2) for s in range(8)]
        rC = _run("C", nc, ims)
        h = np.concatenate([rC[c]["h2"] for c in range(8)])
        del ims, rE, r3, rC
    return np.ascontiguousarray(h.reshape(B_, T_, D_)[:, NMETA_:]).astype(np.float32)
```

```python
import time, math
import contextlib
import numpy as np
import concourse.bass as bass
import concourse.mybir as mybir
from concourse.bass_utils import run_bass_kernel_spmd

F32 = mybir.dt.float32
BF16 = mybir.dt.bfloat16
I32 = mybir.dt.int32
U32 = mybir.dt.uint32
ALU = mybir.AluOpType
AF = mybir.ActivationFunctionType
AX = mybir.AxisListType

SEM_ROLL = 20000
import threading


class Coop(threading.Thread):
    def __init__(self, fn):
        super().__init__(daemon=True)
        self.fn = fn
        self.tickets = threading.Semaphore(0)
        self.consumed = threading.Semaphore(0)
        self.finished = False
        self.err = None
        self.start()

    def run(self):
        try:
            self.fn()
        except BaseException as e:
            self.err = e
        self.finished = True
        self.consumed.release()

    def advance(self, k=1):
        for _ in range(k):
            if self.finished:
                break
            self.tickets.release()
            self.consumed.acquire()
        if self.err is not None:
            raise self.err

    def finish(self):
        while not self.finished:
            self.advance(1)
        if self.err is not None:
            raise self.err


class Sched:
    def __init__(self, nc, es, ndsem=6, same_eng_sync=True):
        self.nc = nc
        self.es = es
        self.eng = dict(pe=nc.tensor, dve=nc.vector, act=nc.scalar, pool=nc.gpsimd, sp=nc.sync)
        self.same = same_eng_sync
        self.nsem = 0
        self.csem = {}
        self.ccnt = {}
        self.cep = {}
        for k in ("pe", "dve", "act", "pool"):
            self._new_csem(k, 0)
        self.dsem = {}
        self.dcnt = {}
        self.drr = {}
        for q in ("sp", "act", "pool"):
            self.dsem[q] = [self._sem(f"d_{q}_{i}") for i in range(ndsem)]
            self.dcnt[q] = [0] * ndsem
            self.drr[q] = 0
        self.seen = {k: {} for k in self.eng}
        self.lastw = {}
        self.readers = {}
        self.ninst = 0

    def _sem(self, name):
        self.nsem += 1
        return self.es.enter_context(self.nc.semaphore(name))

    def _new_csem(self, k, ep):
        self.cep[k] = ep
        self.csem[k] = self._sem(f"c_{k}_{ep}")
        self.ccnt[k] = 0

    def _wait(self, e, tok):
        sem, val, key, src = tok
        if src == e and src in ("pe",):
            return
        if src == e and not self.same and src in ("dve", "act", "pool") and key[0] == "c":
            return
        if self.seen[e].get(key, 0) >= val:
            return
        self.eng[e].wait_ge(sem, val)
        self.seen[e][key] = val
        self.ninst += 1

    def _deps(self, e, r, w):
        for k in r:
            t = self.lastw.get(k)
            if t is not None:
                self._wait(e, t)
        for k in w:
            t = self.lastw.get(k)
            if t is not None:
                self._wait(e, t)
            for t in self.readers.get(k, ()):
                self._wait(e, t)

    def _record(self, tok, r, w):
        for k in r:
            self.readers.setdefault(k, []).append(tok)
        for k in w:
            self.lastw[k] = tok
            self.readers[k] = []

    def op(self, e, fn, r=(), w=()):
        cw = threading.current_thread()
        if isinstance(cw, Coop):
            cw.tickets.acquire()
            try:
                return self._op(e, fn, r, w)
            finally:
                cw.consumed.release()
        return self._op(e, fn, r, w)

    def _op(self, e, fn, r=(), w=()):
        self._deps(e, r, w)
        inst = fn(self.eng[e])
        if self.ccnt[e] >= SEM_ROLL:
            self._new_csem(e, self.cep[e] + 1)
        self.ccnt[e] += 1
        inst.then_inc(self.csem[e], 1)
        tok = (self.csem[e], self.ccnt[e], ("c", e, self.cep[e]), e)
        self._record(tok, r, w)
        self.ninst += 1
        return tok

    def dma(self, q, out, in_, r=(), w=(), fn=None, **kw):
        cw = threading.current_thread()
        if isinstance(cw, Coop):
            cw.tickets.acquire()
            try:
                return self._dma(q, out, in_, r, w, fn, **kw)
            finally:
                cw.consumed.release()
        return self._dma(q, out, in_, r, w, fn, **kw)

    def _dma(self, q, out, in_, r=(), w=(), fn=None, **kw):
        i = self.drr[q]
        self.drr[q] = (i + 1) % len(self.dsem[q])
        sem = self.dsem[q][i]
        key = ("d", q, i)
        if self.dcnt[q][i] > 0:
            self._wait(q, (sem, 16 * self.dcnt[q][i], key, "dma"))
        self._deps(q, r, w)
        if fn is None:
            inst = self.eng[q].dma_start(out=out, in_=in_, **kw)
        else:
            inst = fn(self.eng[q])
        self.dcnt[q][i] += 1
        inst.then_inc(sem, 16)
        tok = (sem, 16 * self.dcnt[q][i], key, "dma")
        self._record(tok, r, w)
        self.ninst += 1
        return tok

    def barrier(self):
        toks = []
        for k in ("pe", "dve", "act", "pool"):
            if self.ccnt[k] > 0:
                toks.append((self.csem[k], self.ccnt[k], ("c", k, self.cep[k]), "bar"))
        for q in self.dsem:
            for i, sem in enumerate(self.dsem[q]):
                if self.dcnt[q][i] > 0:
                    toks.append((sem, 16 * self.dcnt[q][i], ("d", q, i), "bar"))
        for e in ("pe", "dve", "act", "pool", "sp"):
            for t in toks:
                self._wait(e, t)

    def finish(self, toks, e="sp"):
        for t in toks:
            self._wait(e, t)


def new_nc():
    return bass.Bass("TRN2", target_bir_lowering=False)


def run(nc, in_maps):
    res = run_bass_kernel_spmd(nc, in_maps, core_ids=list(range(len(in_maps))))
    return res.results


def mm_phase(nc, S, es, xT, w, dst, ntok, K, N, ps, cb=256, dst_key=None):
    KC = K // 128
    sbt = lambda n, s, d: es.enter_context(nc.sbuf_tensor(n, s, d))
    xb = sbt("mm_xb", [128, KC, ntok], BF16)
    xs = sbt("mm_xs", [128, KC // 4, ntok], F32)
    ws = sbt("mm_ws", [128, KC, cb], F32)
    wb = [sbt(f"mm_wb{i}", [128, KC, cb], BF16) for i in range(2)]
    ot = [sbt(f"mm_ot{i}", [128, cb], F32) for i in range(4)]
    xv = xT.rearrange("(kc p) t -> p kc t", p=128)
    wv = w.rearrange("(kc p) n -> p kc n", p=128)
    q = KC // 4
    for i in range(4):
        S.dma("sp" if i % 2 == 0 else "act", xs[:, :, :], xv[:, i * q:(i + 1) * q, :], w=["mm_xs"])
        h2 = q // 2
        if h2 > 0:
            S.op("dve", lambda e: e.tensor_copy(out=xb[:, i * q:i * q + h2, :], in_=xs[:, :h2, :]), r=["mm_xs"], w=[("mm_xb", i)])
        S.op("act", lambda e: e.copy(out=xb[:, i * q + h2:(i + 1) * q, :], in_=xs[:, h2:, :]), r=["mm_xs"], w=[("mm_xb", i)])
    toks = [(t0, min(128, ntok - t0)) for t0 in range(0, ntok, 128)]
    ncb = (N + cb - 1) // cb
    it = 0
    outs = []
    h = KC // 2
    for c in range(ncb):
        c0 = c * cb
        cn = min(cb, N - c0)
        wbb = wb[c % 2]
        S.dma("sp", ws[:, :h, :cn], wv[:, :h, c0:c0 + cn], w=[("mm_ws", 0)])
        S.dma("act", ws[:, h:, :cn], wv[:, h:, c0:c0 + cn], w=[("mm_ws", 1)])
        S.op("dve", lambda e: e.tensor_copy(out=wbb[:, :h, :cn], in_=ws[:, :h, :cn]), r=[("mm_ws", 0)], w=[("mm_wb", c % 2, 0)])
        S.op("act", lambda e: e.copy(out=wbb[:, h:, :cn], in_=ws[:, h:, :cn]), r=[("mm_ws", 1)], w=[("mm_wb", c % 2, 1)])
        for (t0, m) in toks:
            p = ps[it % 4]
            o = ot[it % 4]
            for kc in range(KC):
                S.op("pe", lambda e: e.matmul(p[:m, :cn], lhsT=xb[:, kc, t0:t0 + m], rhs=wbb[:, kc, :cn], start=(kc == 0), stop=(kc == KC - 1)),
                     r=[("mm_xb", kc // q), ("mm_wb", c % 2, 0 if kc < h else 1)], w=[("mm_ps", it % 4)])
            if it % 2 == 0:
                S.op("dve", lambda e: e.tensor_copy(out=o[:m, :cn], in_=p[:m, :cn]), r=[("mm_ps", it % 4)], w=[("mm_ot", it % 4)])
            else:
                S.op("act", lambda e: e.copy(out=o[:m, :cn], in_=p[:m, :cn]), r=[("mm_ps", it % 4)], w=[("mm_ot", it % 4)])
            outs.append(S.dma("pool", dst[t0:t0 + m, c0:c0 + cn], o[:m, :cn], r=[("mm_ot", it % 4)], w=([(dst_key, t0)] if dst_key else [])))
            it += 1
    return outs


def build_mm(ntok, K, N, cb=256, name="mm"):
    nc = new_nc()
    xT = nc.dram_tensor("xT", [K, ntok], F32, kind="ExternalInput").ap()
    w = nc.dram_tensor("w", [K, N], F32, kind="ExternalInput").ap()
    out = nc.dram_tensor("out", [ntok, N], F32, kind="ExternalOutput").ap()
    es = contextlib.ExitStack()
    with es:
        S = Sched(nc, es)
        ps = [es.enter_context(nc.psum_tensor(f"ps{i}", [128, 512], F32)) for i in range(4)]
        outs = mm_phase(nc, S, es, xT, w, out, ntok, K, N, ps, cb)
        S.finish(outs, "pool")
        print("ninst", S.ninst, "nsem", S.nsem)
    return nc


import math

GN_EPS = 64e-5
DEBUG = False
NV = 10
C_MUR, C_MUK, C_MUV, C_W0, C_A0, C_KK, C_KA, C_RK, C_GG, C_GB = range(10)

def build_rwkv(T, TC=256, TCB=32, same=False):
    nc = new_nc()
    G = 3
    di = lambda n, s: nc.dram_tensor(n, s, F32, kind="ExternalInput").ap()
    prT = di("prT", [384, T + 1]); pkT = di("pkT", [384, T + 1]); pvT = di("pvT", [384, T + 1])
    pdwT = di("pdwT", [128, T + 1]); pdaT = di("pdaT", [128, T + 1]); pdgT = di("pdgT", [480, T + 1])
    pv_tm = di("pv_tm", [T + 1, 384])
    vec = di("vec", [384, NV]); vlo = di("vlo", [128, 2]); vdg = di("vdg", [480, 1]); muvb = di("muvb", [128, 384])
    w2s = di("w2s", [128, 384]); a2s = di("a2s", [128, 384]); g2s = di("g2s", [480, 384])
    cM = di("cM", [128, 128]); cSel = di("cSel", [64, 2, 128])
    yT = nc.dram_tensor("yT", [384, T], F32, kind="ExternalOutput").ap()
    vscr = nc.dram_tensor("vscr", [T, 384], F32, kind="Internal").ap()
    dbg = nc.dram_tensor("dbg", [12, 128, 3, TC], F32, kind="ExternalOutput").ap() if DEBUG else None
    es = contextlib.ExitStack()
    with es:
        S = Sched(nc, es, same_eng_sync=same)
        sb = lambda n, s, d=F32: es.enter_context(nc.sbuf_tensor(n, s, d))
        pt = lambda n, s: es.enter_context(nc.psum_tensor(n, s, F32))
        vec_t = sb("vec_t", [128, G, NV]); vlo_t = sb("vlo_t", [128, 2]); vdg_t = sb("vdg_t", [128, 4, 1]); muvb_t = sb("muvb_t", [128, 384])
        w2_t = sb("w2_t", [128, 384]); a2_t = sb("a2_t", [128, 384]); g2_t = sb("g2_t", [128, 4, 384])
        M_t = sb("M_t", [128, 128]); Mavg_t = sb("Mavg_t", [128, 128]); Sel_t = sb("Sel_t", [64, 2, 128]); Mrk_t = sb("Mrk_t", [128, G, 128])
        S.dma("sp", vec_t[:], vec.rearrange("(g p) n -> p g n", p=128), w=["vec"])
        S.dma("sp", vlo_t[:], vlo, w=["vlo"])
        for kc in range(4):
            n = 128 if kc < 3 else 96
            S.dma("sp", vdg_t[:n, kc, :], vdg[kc * 128:kc * 128 + n, :], w=["vdg"])
            S.dma("act", g2_t[:n, kc, :], g2s[kc * 128:kc * 128 + n, :], w=["g2"])
        S.dma("sp", muvb_t[:], muvb, w=["muvb"])
        S.dma("act", w2_t[:], w2s, w=["w2"]); S.dma("act", a2_t[:], a2s, w=["a2"])
        S.dma("sp", M_t[:], cM, w=["M"]); S.dma("sp", Sel_t[:], cSel, w=["Sel"])
        S.op("dve", lambda e: e.tensor_scalar(out=Mavg_t[:], in0=M_t[:], scalar1=1.0 / 64, scalar2=None, op0=ALU.mult), r=["M"], w=["Mavg"])
        for g in range(G):
            S.op("dve", lambda e: e.tensor_scalar(out=Mrk_t[:, g, :], in0=M_t[:], scalar1=vec_t[:, g, C_RK:C_RK + 1], scalar2=None, op0=ALU.mult), r=["M", "vec"], w=["Mrk"])
        W1 = TC + 1
        ld_r = sb("ld_r", [128, G, W1]); ld_k = sb("ld_k", [128, G, W1]); ld_v = sb("ld_v", [128, G, W1])
        ld_dw = sb("ld_dw", [128, W1]); ld_da = sb("ld_da", [128, W1]); ld_dg = sb("ld_dg", [128, 4, W1])
        tmA = sb("tmA", [128, 384]); tmB = sb("tmB", [128, 384]); tmD = sb("tmD", [128, 384])
        rs = sb("rs", [128, G, TC]); ks = sb("ks", [128, G, TC]); vs = sb("vs", [128, G, TC])
        dws = sb("dws", [128, TC]); das = sb("das", [128, TC]); dgs = sb("dgs", [128, 4, TC])
        aa = sb("aa", [128, G, TC])
        dec2 = [sb(f"dec{q}", [128, G, TC]) for q in range(2)]; gg2 = [sb(f"gg{q}", [128, G, TC]) for q in range(2)]
        nkk2 = [sb(f"nkk{q}", [128, G, TC]) for q in range(2)]; bb2 = [sb(f"bb{q}", [128, G, TC]) for q in range(2)]
        kmod2 = [sb(f"kmod{q}", [128, G, TC]) for q in range(2)]
        Rm2 = [sb(f"Rm{q}", [128, G, TC, 2]) for q in range(2)]; bonv2 = [sb(f"bonv{q}", [128, G, TC]) for q in range(2)]
        t1 = sb("t1", [128, TC]); t2 = sb("t2", [128, TC]); t3 = sb("t3", [128, TC])
        u1 = sb("u1", [128, TC]); u2 = sb("u2", [128, TC])
        ysb = sb("ysb", [64, G, TC * 2]); yfm = sb("yfm", [128, TC]); yo = sb("yo", [128, G, TC])
        SD = [[sb(f"SD{q}_{g}", [128, 64]) for g in range(G)] for q in range(2)]
        S1 = [sb(f"S1_{g}", [128, 64]) for g in range(G)]
        S2 = [sb(f"S2_{g}", [128, 64]) for g in range(G)]
        Zall = sb("Zall", [128, G * 64], mybir.dt.float32r)
        M_r = sb("M_r", [128, 128], mybir.dt.float32r)
        NL = 4
        Lr = [[sb(f"L{g}_{i}", [128, 128]) for i in range(NL)] for g in range(G)]
        vb = [[sb(f"vb{g}_{i}", [128, TCB, 64]) for i in range(2)] for g in range(G)]
        ps1 = [pt(f"ps1_{g}", [128, 512]) for g in range(G)]
        psy = [pt(f"psy_{g}", [128, 512]) for g in range(G)]
        psA = pt("psA", [128, 512]); psB = pt("psB", [128, 512])
        for g in range(G):
            for q in range(2):
                S.op("pool", lambda e: e.memset(SD[q][g][:], 0.0), w=[("SD", q, g)])
        S.op("dve", lambda e: e.tensor_copy(out=M_r[:], in_=M_t[:]), r=["M"], w=["M_r"])
        out_toks = []
        nvb = 0
        lcnt = [0] * G
        def pre(c0, par):
            n = min(TC, T - c0)
            dec, gg, nkk, bb, kmod, Rm, bonv = dec2[par], gg2[par], nkk2[par], bb2[par], kmod2[par], Rm2[par], bonv2[par]
            for r0 in range(c0, c0 + n, 128):
                m = min(128, c0 + n - r0)
                S.dma("sp", tmA[:m, :], pv_tm[r0 + 1:r0 + 1 + m, :], w=["tmA"])
                S.dma("act", tmB[:m, :], pv_tm[r0:r0 + m, :], w=["tmB"])
                S.op("dve", lambda e: e.tensor_tensor(out=tmD[:m, :], in0=tmB[:m, :], in1=tmA[:m, :], op=ALU.subtract), r=["tmA", "tmB"], w=["tmD"])
                S.op("dve", lambda e: e.tensor_tensor(out=tmD[:m, :], in0=tmD[:m, :], in1=muvb_t[:m, :], op=ALU.mult), r=["tmD", "muvb"], w=["tmD"])
                S.op("dve", lambda e: e.tensor_tensor(out=tmD[:m, :], in0=tmD[:m, :], in1=tmA[:m, :], op=ALU.add), r=["tmD", "tmA"], w=["tmD"])
                S.dma("sp", vscr[r0:r0 + m, :], tmD[:m, :], r=["tmD"], w=[("vscr", r0 // TCB + i) for i in range((m + TCB - 1) // TCB)])
            S.dma("sp", ld_r[:, :, :n + 1], prT[:, c0:c0 + n + 1].rearrange("(g p) t -> p g t", p=128), w=["ld_r"])
            S.dma("act", ld_k[:, :, :n + 1], pkT[:, c0:c0 + n + 1].rearrange("(g p) t -> p g t", p=128), w=["ld_k"])
            S.dma("sp", ld_v[:, :, :n + 1], pvT[:, c0:c0 + n + 1].rearrange("(g p) t -> p g t", p=128), w=["ld_v"])
            S.dma("act", ld_dw[:, :n + 1], pdwT[:, c0:c0 + n + 1], w=["ld_dw"])
            S.dma("sp", ld_da[:, :n + 1], pdaT[:, c0:c0 + n + 1], w=["ld_da"])
            for kc in range(4):
                kn = 128 if kc < 3 else 96
                S.dma("act", ld_dg[:kn, kc, :n + 1], pdgT[kc * 128:kc * 128 + kn, c0:c0 + n + 1], w=[("ld_dg", kc)])

            def shift(dst, src, mu, P, rk, wk):
                S.op("dve", lambda e: e.tensor_tensor(out=t1[:P, :n], in0=src[:P, 0:n], in1=src[:P, 1:n + 1], op=ALU.subtract), r=[rk], w=["t1"])
                S.op("dve", lambda e: e.scalar_tensor_tensor(out=dst[:P, :n], in0=t1[:P, :n], scalar=mu, in1=src[:P, 1:n + 1], op0=ALU.mult, op1=ALU.add), r=["t1", rk, "vec", "vlo", "vdg"], w=[wk])
            for g in range(G):
                shift(rs[:, g, :], ld_r[:, g, :], vec_t[:, g, C_MUR:C_MUR + 1], 128, "ld_r", ("rs", g))
                shift(ks[:, g, :], ld_k[:, g, :], vec_t[:, g, C_MUK:C_MUK + 1], 128, "ld_k", ("ks", g))
                shift(vs[:, g, :], ld_v[:, g, :], vec_t[:, g, C_MUV:C_MUV + 1], 128, "ld_v", ("vs", g))
            shift(dws, ld_dw, vlo_t[:, 0:1], 128, "ld_dw", "dws")
            shift(das, ld_da, vlo_t[:, 1:2], 128, "ld_da", "das")
            for kc in range(4):
                kn = 128 if kc < 3 else 96
                shift(dgs[:, kc, :], ld_dg[:, kc, :], vdg_t[:kn, kc, :], kn, ("ld_dg", kc), ("dgs", kc))
            S.op("act", lambda e: e.activation(out=dws[:, :n], in_=dws[:, :n], func=AF.Tanh), r=["dws"], w=["dws"])
            for kc in range(4):
                kn = 128 if kc < 3 else 96
                S.op("act", lambda e: e.activation(out=dgs[:kn, kc, :n], in_=dgs[:kn, kc, :n], func=AF.Sigmoid), r=[("dgs", kc)], w=[("dgs", kc)])
            for g in range(G):
                gs = slice(g * 128, (g + 1) * 128)
                S.op("pe", lambda e: e.matmul(psA[:, :n], lhsT=w2_t[:, gs], rhs=dws[:, :n], start=True, stop=True), r=["w2", "dws"], w=["psA"])
                S.op("act", lambda e: e.activation(out=t2[:, :n], in_=psA[:, :n], func=AF.Sigmoid, bias=vec_t[:, g, C_W0:C_W0 + 1]), r=["psA", "vec"], w=["t2"])
                S.op("act", lambda e: e.activation(out=dec[:, g, :n], in_=t2[:, :n], func=AF.Exp, scale=-math.exp(-0.5)), r=["t2"], w=[("dec", par, g)])
                S.op("pe", lambda e: e.matmul(psB[:, :n], lhsT=a2_t[:, gs], rhs=das[:, :n], start=True, stop=True), r=["a2", "das"], w=["psB"])
                S.op("act", lambda e: e.activation(out=aa[:, g, :n], in_=psB[:, :n], func=AF.Sigmoid, bias=vec_t[:, g, C_A0:C_A0 + 1]), r=["psB", "vec"], w=[("aa", g)])
                for kc in range(4):
                    kn = 128 if kc < 3 else 96
                    S.op("pe", lambda e: e.matmul(psA[:, :n], lhsT=g2_t[:kn, kc, gs], rhs=dgs[:kn, kc, :n], start=(kc == 0), stop=(kc == 3)), r=["g2", ("dgs", kc)], w=["psA"])
                S.op("act", lambda e: e.copy(out=gg[:, g, :n], in_=psA[:, :n]), r=["psA"], w=[("gg", par, g)])
                S.op("dve", lambda e: e.tensor_scalar(out=t1[:, :n], in0=ks[:, g, :n], scalar1=vec_t[:, g, C_KK:C_KK + 1], scalar2=None, op0=ALU.mult), r=[("ks", g), "vec"], w=["t1"])
                S.op("dve", lambda e: e.tensor_tensor(out=t3[:, :n], in0=t1[:, :n], in1=t1[:, :n], op=ALU.mult), r=["t1"], w=["t3"])
                S.op("pe", lambda e: e.matmul(psB[:, :n], lhsT=M_t[:], rhs=t3[:, :n], start=True, stop=True), r=["M", "t3"], w=["psB"])
                S.op("act", lambda e: e.activation(out=t3[:, :n], in_=psB[:, :n], func=AF.Sqrt), r=["psB"], w=["t3"])
                S.op("dve", lambda e: e.tensor_scalar(out=t3[:, :n], in0=t3[:, :n], scalar1=1e-12, scalar2=None, op0=ALU.max), r=["t3"], w=["t3"])
                S.op("dve", lambda e: e.reciprocal(out=t3[:, :n], in_=t3[:, :n]), r=["t3"], w=["t3"])
                S.op("dve", lambda e: e.scalar_tensor_tensor(out=nkk[:, g, :n], in0=t1[:, :n], scalar=-1.0, in1=t3[:, :n], op0=ALU.mult, op1=ALU.mult), r=["t1", "t3"], w=[("nkk", par, g)])
                S.op("dve", lambda e: e.scalar_tensor_tensor(out=bb[:, g, :n], in0=nkk[:, g, :n], scalar=-1.0, in1=aa[:, g, :n], op0=ALU.mult, op1=ALU.mult), r=[("nkk", par, g), ("aa", g)], w=[("bb", par, g)])
                S.op("dve", lambda e: e.tensor_scalar(out=t1[:, :n], in0=aa[:, g, :n], scalar1=-1.0, scalar2=vec_t[:, g, C_KA:C_KA + 1], op0=ALU.add, op1=ALU.mult), r=[("aa", g), "vec"], w=["t1"])
                S.op("dve", lambda e: e.scalar_tensor_tensor(out=kmod[:, g, :n], in0=t1[:, :n], scalar=1.0, in1=ks[:, g, :n], op0=ALU.add, op1=ALU.mult), r=["t1", ("ks", g)], w=[("kmod", par, g)])
                S.op("dve", lambda e: e.tensor_tensor(out=t1[:, :n], in0=rs[:, g, :n], in1=kmod[:, g, :n], op=ALU.mult), r=[("rs", g), ("kmod", par, g)], w=["t1"])
                S.op("pe", lambda e: e.matmul(psB[:, :n], lhsT=Mrk_t[:, g, :], rhs=t1[:, :n], start=True, stop=True), r=["Mrk", "t1"], w=["psB"])
                S.op("dve", lambda e: e.tensor_tensor(out=bonv[:, g, :n], in0=psB[:, :n], in1=vs[:, g, :n], op=ALU.mult), r=["psB", ("vs", g)], w=[("bonv", par, g)])
                S.op("pool", lambda e: e.memset(Rm[:, g, :n, :], 0.0), w=[("Rm", par, g)])
                S.op("pool", lambda e: e.tensor_copy(out=Rm[0:64, g, :n, 0], in_=rs[0:64, g, :n]), r=[("rs", g)], w=[("Rm", par, g)])
                S.op("pool", lambda e: e.tensor_copy(out=Rm[64:128, g, :n, 1], in_=rs[64:128, g, :n]), r=[("rs", g)], w=[("Rm", par, g)])
        def load_block(kb):
            t0b = kb * TCB
            if t0b >= T:
                return
            nb = min(TCB, T - t0b)
            ci_ = t0b // TC; parb = ci_ % 2; s0 = t0b - ci_ * TC; vbi_ = kb % 2
            for g in range(G):
                for h in range(2):
                    src = vscr[t0b:t0b + nb, g * 128 + h * 64:g * 128 + (h + 1) * 64]
                    S.dma("sp" if h == 0 else "act", vb[g][vbi_][h * 64:(h + 1) * 64, :nb, :], src.partition_broadcast(64),
                          r=[("vscr", kb)], w=[("vb", g, vbi_)])
                S.op("dve", lambda e: e.tensor_tensor(out=vb[g][vbi_][:, :nb, :], in0=vb[g][vbi_][:, :nb, :],
                                                      in1=kmod2[parb][:, g, s0:s0 + nb].unsqueeze(2).to_broadcast([128, nb, 64]), op=ALU.mult),
                     r=[("vb", g, vbi_), ("kmod", parb, g)], w=[("vb", g, vbi_)])

        def scan(c0, par, hook):
            nonlocal nvb
            n = min(TC, T - c0)
            dec, nkk, bb, kmod, Rm = dec2[par], nkk2[par], bb2[par], kmod2[par], Rm2[par]
            for s in range(n):
                t = c0 + s
                hook(s)
                if t % TCB == 0:
                    kb = t // TCB
                    if kb == 0:
                        load_block(0)
                    load_block(kb + 1)
                    vbi = kb % 2
                sl = t % TCB
                pr_, pw_ = (t + 1) % 2, t % 2
                for g in range(G):
                    S.op("act", lambda e: e.activation(out=Zall[:, g * 64:(g + 1) * 64], in_=SD[pr_][g][:], func=AF.Copy, scale=nkk[:, g, s:s + 1]), r=[("SD", pr_, g), ("nkk", par, g)], w=[("Z", g)])
                for g in range(G):
                    S.op("dve", lambda e: e.scalar_tensor_tensor(out=S1[g][:], in0=SD[pr_][g][:], scalar=dec[:, g, s:s + 1], in1=vb[g][vbi][:, sl, :], op0=ALU.mult, op1=ALU.add),
                         r=[("SD", pr_, g), ("dec", par, g), ("vb", g, vbi)], w=[("S1", g)])
                for g in range(G):
                    S.op("pe", lambda e: e.matmul(ps1[g][:, 0:64], lhsT=M_r[:], rhs=Zall[:, g * 64:(g + 1) * 64], start=True, stop=True), r=["M_r", ("Z", g)], w=[("ps1", g)])
                if s > 0:
                    for g in range(G):
                        S.op("pe", lambda e: e.matmul(psy[g][0:64, 2 * (s - 1):2 * (s - 1) + 2], lhsT=SD[pr_][g][:], rhs=Rm[:, g, s - 1, :], start=True, stop=True), r=[("SD", pr_, g), ("Rm", par, g)], w=[("psy", g)])
                for g in range(G):
                    S.op("dve", lambda e: e.scalar_tensor_tensor(out=SD[pw_][g][:], in0=ps1[g][:, 0:64], scalar=bb[:, g, s:s + 1], in1=S1[g][:], op0=ALU.mult, op1=ALU.add),
                         r=[("ps1", g), ("bb", par, g), ("S1", g)], w=[("SD", pw_, g)])
            lastp = (c0 + n - 1) % 2
            for g in range(G):
                S.op("pe", lambda e: e.matmul(psy[g][0:64, 2 * (n - 1):2 * (n - 1) + 2], lhsT=SD[lastp][g][:], rhs=Rm[:, g, n - 1, :], start=True, stop=True), r=[("SD", lastp, g), ("Rm", par, g)], w=[("psy", g)])
        def post(c0, par):
            n = min(TC, T - c0)
            gg, bonv = gg2[par], bonv2[par]
            for g in range(G):
                S.op("act", lambda e: e.copy(out=ysb[:, g, :2 * n], in_=psy[g][0:64, :2 * n]), r=[("psy", g)], w=[("ysb", g)])
            for g in range(G):
                yv = ysb[:, g, :2 * n].rearrange("p (t h) -> p t h", h=2)
                for h in range(2):
                    S.op("pe", lambda e: e.matmul(psA[:, :n], lhsT=Sel_t[:, h, :], rhs=yv[:, :, h], start=(h == 0), stop=(h == 1)), r=["Sel", ("ysb", g)], w=["psA"])
                S.op("act", lambda e: e.copy(out=yfm[:, :n], in_=psA[:, :n]), r=["psA"], w=["yfm"])
                S.op("pe", lambda e: e.matmul(psB[:, :n], lhsT=Mavg_t[:], rhs=yfm[:, :n], start=True, stop=True), r=["Mavg", "yfm"], w=["psB"])
                S.op("dve", lambda e: e.tensor_tensor(out=u1[:, :n], in0=yfm[:, :n], in1=psB[:, :n], op=ALU.subtract), r=["yfm", "psB"], w=["u1"])
                S.op("dve", lambda e: e.tensor_tensor(out=u2[:, :n], in0=u1[:, :n], in1=u1[:, :n], op=ALU.mult), r=["u1"], w=["u2"])
                S.op("pe", lambda e: e.matmul(psB[:, :n], lhsT=Mavg_t[:], rhs=u2[:, :n], start=True, stop=True), r=["Mavg", "u2"], w=["psB"])
                S.op("dve", lambda e: e.tensor_scalar(out=u2[:, :n], in0=psB[:, :n], scalar1=GN_EPS, scalar2=None, op0=ALU.add), r=["psB"], w=["u2"])
                S.op("act", lambda e: e.activation(out=u2[:, :n], in_=u2[:, :n], func=AF.Sqrt), r=["u2"], w=["u2"])
                S.op("dve", lambda e: e.reciprocal(out=u2[:, :n], in_=u2[:, :n]), r=["u2"], w=["u2"])
                S.op("dve", lambda e: e.tensor_tensor(out=u1[:, :n], in0=u1[:, :n], in1=u2[:, :n], op=ALU.mult), r=["u1", "u2"], w=["u1"])
                S.op("dve", lambda e: e.tensor_scalar(out=u1[:, :n], in0=u1[:, :n], scalar1=vec_t[:, g, C_GG:C_GG + 1], scalar2=vec_t[:, g, C_GB:C_GB + 1], op0=ALU.mult, op1=ALU.add), r=["u1", "vec"], w=["u1"])
                S.op("dve", lambda e: e.tensor_tensor(out=u1[:, :n], in0=u1[:, :n], in1=bonv[:, g, :n], op=ALU.add), r=["u1", ("bonv", par, g)], w=["u1"])
                S.op("dve", lambda e: e.tensor_tensor(out=yo[:, g, :n], in0=u1[:, :n], in1=gg[:, g, :n], op=ALU.mult), r=["u1", ("gg", par, g)], w=[("yo", g)])
                out_toks.append(S.dma("pool", yT[g * 128:(g + 1) * 128, c0:c0 + n], yo[:, g, :n], r=[("yo", g)]))
        chunks = list(range(0, T, TC))
        NPRE, NPOST = 400, 60
        pre(chunks[0], 0)
        wpost = None
        for ci, c0 in enumerate(chunks):
            par = ci % 2
            n = min(TC, T - c0)
            wpre = Coop(lambda c1=(chunks[ci + 1] if ci + 1 < len(chunks) else None), p1=(ci + 1) % 2: pre(c1, p1)) if ci + 1 < len(chunks) else None
            if wpost is not None:
                wpost.advance(G)
            kpre = (NPRE + n - 1) // n; kpost = (NPOST + n - 1) // n
            def hook(s, wpre=wpre, wpost=wpost):
                if wpost is not None and not wpost.finished:
                    wpost.advance(1)
                elif wpre is not None:
                    wpre.advance(1)
            scan(c0, par, hook)
            if wpost is not None:
                wpost.finish()
            if wpre is not None:
                wpre.finish()
            wpost = Coop(lambda c1=c0, p1=par: post(c1, p1))
        wpost.finish()
        S.finish(out_toks, "pool")
        print("ninst", S.ninst, "nsem", S.nsem)
    return nc


def consts():
    M = np.zeros((128, 128), np.float32); M[:64, :64] = 1; M[64:, 64:] = 1
    Sel = np.zeros((64, 2, 128), np.float32)
    for i in range(64):
        Sel[i, 0, i] = 1; Sel[i, 1, 64 + i] = 1
    return M, Sel


def rwkv_inmap(pr, pk, pv, pdw, pda, pdg, mu_r, mu_k, mu_v, mu_dw, mu_da, mu_dg, w2, w0, a2, a0, g2, k_k, k_a, r_k, gn_g, gn_b):
    M, Sel = consts()
    z = lambda a: np.ascontiguousarray(np.concatenate([np.zeros((a.shape[1], 1), np.float32), a.T], axis=1))
    vec = np.stack([mu_r, mu_k, mu_v, w0, a0, k_k, k_a, r_k, gn_g, gn_b], axis=1).astype(np.float32)
    return dict(prT=z(pr), pkT=z(pk), pvT=z(pv), pdwT=z(pdw), pdaT=z(pda), pdgT=z(pdg),
                pv_tm=np.ascontiguousarray(np.concatenate([np.zeros((1, pv.shape[1]), np.float32), pv], axis=0)),
                vec=np.ascontiguousarray(vec), vlo=np.ascontiguousarray(np.stack([mu_dw, mu_da], axis=1)),
                vdg=np.ascontiguousarray(mu_dg[:, None]), muvb=np.ascontiguousarray(np.broadcast_to(mu_v[None, :], (128, 384))),
                w2s=np.ascontiguousarray(w2), a2s=np.ascontiguousarray(a2), g2s=np.ascontiguousarray(g2), cM=M, cSel=Sel)


import math
import ml_dtypes

NEG = -1.0e30
LN_EPS = 1e-5

def build_dsa(NS=8, NH=12, topk=256):
    T = 512 * NS + 16
    NQ = 128 * NS + 4
    NKT = (T + 127) // 128
    nc = new_nc()
    di = lambda n, s, d=F32: nc.dram_tensor(n, s, d, kind="ExternalInput").ap()
    cq = di("cq", [NQ, 512]); widx = di("widx", [NQ, 8]); qpos = di("qpos", [NQ, 1])
    kT = di("kT", [NH * 128, T]); v = di("v", [T, NH * 128]); kidx = di("kidx", [T, 128])
    w_uq = di("w_uq", [512, NH * 128]); w_iq = di("w_iq", [512, 1024])
    qg = di("qg", [128, 4]); kig = di("kig", [128, 128]); kib = di("kib", [128, 128])
    cos_fm = di("cos_fm", [128, T]); sin_fm = di("sin_fm", [128, T])
    cosq = di("cosq", [128, NQ]); sinq = di("sinq", [128, NQ])
    kpos = di("kpos", [128, T]); cR = di("cR", [128, 128]); cI = di("cI", [128, 128])
    y = nc.dram_tensor("y", [NQ, NH * 128], F32, kind="ExternalOutput").ap()
    slots = [(j * 128, 128, 512 * (j + 1)) for j in range(NS)] + [(128 * NS, 4, T)]
    es = contextlib.ExitStack()
    with es:
        S = Sched(nc, es)
        sb = lambda n, s, d=F32, st=es: st.enter_context(nc.sbuf_tensor(n, s, d))
        pt = lambda n, s, d=F32: es.enter_context(nc.psum_tensor(n, s, d))
        def op(eng, fn, r, w): return S.op(eng, fn, r=r, w=w)
        maskT = sb("maskT", [128, NKT, NQ], BF16)
        cqnT = sb("cqnT", [128, 4, NQ]); cosq_t = sb("cosq_t", [128, NQ]); sinq_t = sb("sinq_t", [128, NQ])
        ctab = [sb(f"ctab{i}", [128, 512]) for i in range(2)]; stab = [sb(f"stab{i}", [128, 512]) for i in range(2)]
        xT = sb("xT", [128, 512])
        R_t = sb("R_t", [128, 128]); I_t = sb("I_t", [128, 128]); Ib_t = sb("Ib_t", [128, 128], BF16)
        tA = sb("tA", [128, 512]); tB = sb("tB", [128, 512])
        psR = [pt(f"psR{i}", [128, 512]) for i in range(2)]
        psM = [pt(f"psM{i}", [128, 512]) for i in range(2)]
        psO = [pt(f"psO{i}", [128, 512]) for i in range(2)]
        psT = [pt(f"psT{i}", [128, 512], BF16) for i in range(2)]
        S.dma("sp", R_t[:], cR, w=["R"]); S.dma("sp", I_t[:], cI, w=["I"])
        S.dma("sp", cosq_t[:], cosq, w=["cosq"]); S.dma("act", sinq_t[:], sinq, w=["sinq"])
        tcnt = [0]
        def rope_k(dst, src, c, n, rk, wk):
            i = tcnt[0] % 2; tcnt[0] += 1
            S.dma("sp", ctab[i][:, :n], cos_fm[:, c:c + n], w=[("ctab", i)])
            S.dma("act", stab[i][:, :n], sin_fm[:, c:c + n], w=[("stab", i)])
            rope_fm(dst, src, n, ctab[i][:, :n], stab[i][:, :n], rk, wk, [("ctab", i), ("stab", i)])
        op("dve", lambda e: e.tensor_copy(out=Ib_t[:], in_=I_t[:]), ["I"], ["Ib"])
        rcnt = [0]
        def rope_fm(dst, src, n, ct, st_, rk, wk, ck):
            i = rcnt[0] % 2; rcnt[0] += 1
            op("pe", lambda e: e.matmul(psR[i][:, :n], lhsT=R_t[:], rhs=src, start=True, stop=True), ["R"] + rk, [("psR", i)])
            op("dve", lambda e: e.tensor_tensor(out=tA[:, :n], in0=psR[i][:, :n], in1=st_, op=ALU.mult), [("psR", i)] + ck, ["tA"])
            op("pool", lambda e: e.tensor_tensor(out=tB[:, :n], in0=src, in1=ct, op=ALU.mult), rk + ck, ["tB"])
            op("dve", lambda e: e.tensor_tensor(out=dst, in0=tA[:, :n], in1=tB[:, :n], op=ALU.add), ["tA", "tB"], wk)

        p1 = contextlib.ExitStack()
        with p1:
            sb1 = lambda n, s, d=F32: sb(n, s, d, p1)
            qiS = sb1("qiS", [128, 8, 128]); kiT = sb1("kiT", [128, T]); wiq_t = sb1("wiq_t", [128, 4, 1024])
            wq_t = sb1("wq_t", [128, NS + 1, 8]); qpos_t = sb1("qpos_t", [128, NS + 1, 1])
            qg_t = sb1("qg_t", [128, 4]); kig_t = sb1("kig_t", [128, 128]); kib_t = sb1("kib_t", [128, 128])
            kpos_t = sb1("kpos_t", [128, 512]); qpc = sb1("qpc", [128, 1])
            xin = sb1("xin", [128, 512]); xsq = sb1("xsq", [128, 512]); st1 = sb1("st1", [128, 4])
            acc = sb1("acc", [128, T]); wrk = sb1("wrk", [128, T]); mk = sb1("mk", [128, T], BF16)
            m8 = sb1("m8", [128, 8]); thr = sb1("thr", [128, 1]); rl = sb1("rl", [128, 512])
            for kc in range(4):
                S.dma("sp" if kc % 2 == 0 else "act", wiq_t[:, kc, :], w_iq[kc * 128:(kc + 1) * 128, :], w=["wiq"])
            S.dma("sp", qg_t[:], qg, w=["qg"]); S.dma("sp", kig_t[:], kig, w=["kig"]); S.dma("act", kib_t[:], kib, w=["kib"])
            S.dma("sp", kpos_t[:], kpos[:, 0:512], w=["kpos"])
            for j, (q0, m, nk) in enumerate(slots):
                S.dma("sp", wq_t[:m, j, :], widx[q0:q0 + m, :], w=["wq"])
                S.dma("act", qpos_t[:m, j, :], qpos[q0:q0 + m, :], w=["qpos"])
            for j, (q0, m, nk) in enumerate(slots):
                S.dma("sp", xin[:m, :], cq[q0:q0 + m, :], w=["xin"])
                op("dve", lambda e: e.tensor_tensor(out=xsq[:m, :], in0=xin[:m, :], in1=xin[:m, :], op=ALU.mult), ["xin"], ["xsq"])
                op("dve", lambda e: e.tensor_reduce(out=st1[:m, 0:1], in_=xsq[:m, :], axis=AX.X, op=ALU.add), ["xsq"], ["st1"])
                op("dve", lambda e: e.tensor_scalar(out=st1[:m, 1:2], in0=st1[:m, 0:1], scalar1=1.0 / 512, scalar2=1e-6, op0=ALU.mult, op1=ALU.add), ["st1"], ["st1"])
                op("act", lambda e: e.activation(out=st1[:m, 2:3], in_=st1[:m, 1:2], func=AF.Sqrt), ["st1"], ["st1"])
                op("dve", lambda e: e.reciprocal(out=st1[:m, 3:4], in_=st1[:m, 2:3]), ["st1"], ["st1"])
                op("dve", lambda e: e.tensor_scalar(out=xsq[:m, :], in0=xin[:m, :], scalar1=st1[:m, 3:4], scalar2=None, op0=ALU.mult), ["xin", "st1"], ["xsq"])
                for kc in range(4):
                    i = kc % 2
                    op("pe", lambda e: e.transpose(psM[i][:, :m], xsq[:m, kc * 128:(kc + 1) * 128], I_t[:m, :m]), ["xsq", "I"], [("psM", i)])
                    op("act", lambda e: e.activation(out=cqnT[:, kc, q0:q0 + m], in_=psM[i][:, :m], func=AF.Copy, scale=qg_t[:, kc:kc + 1]), [("psM", i), "qg"], ["cqnT"])
            for kt in range(NKT):
                t0 = kt * 128; m = min(128, T - t0)
                S.dma("sp", xin[:m, :128], kidx[t0:t0 + m, :], w=["xin"])
                op("dve", lambda e: e.tensor_reduce(out=st1[:m, 0:1], in_=xin[:m, :128], axis=AX.X, op=ALU.add), ["xin"], ["st1"])
                op("dve", lambda e: e.tensor_scalar(out=st1[:m, 1:2], in0=st1[:m, 0:1], scalar1=-1.0 / 128, scalar2=None, op0=ALU.mult), ["st1"], ["st1"])
                op("dve", lambda e: e.tensor_scalar(out=xsq[:m, :128], in0=xin[:m, :128], scalar1=st1[:m, 1:2], scalar2=None, op0=ALU.add), ["xin", "st1"], ["xsq"])
                op("dve", lambda e: e.tensor_tensor(out=xsq[:m, 128:256], in0=xsq[:m, :128], in1=xsq[:m, :128], op=ALU.mult), ["xsq"], ["xsq"])
                op("dve", lambda e: e.tensor_reduce(out=st1[:m, 0:1], in_=xsq[:m, 128:256], axis=AX.X, op=ALU.add), ["xsq"], ["st1"])
                op("dve", lambda e: e.tensor_scalar(out=st1[:m, 1:2], in0=st1[:m, 0:1], scalar1=1.0 / 128, scalar2=LN_EPS, op0=ALU.mult, op1=ALU.add), ["st1"], ["st1"])
                op("act", lambda e: e.activation(out=st1[:m, 2:3], in_=st1[:m, 1:2], func=AF.Sqrt), ["st1"], ["st1"])
                op("dve", lambda e: e.reciprocal(out=st1[:m, 3:4], in_=st1[:m, 2:3]), ["st1"], ["st1"])
                op("dve", lambda e: e.scalar_tensor_tensor(out=xsq[:m, 256:384], in0=xsq[:m, :128], scalar=st1[:m, 3:4], in1=kig_t[:m, :], op0=ALU.mult, op1=ALU.mult), ["xsq", "st1", "kig"], ["xsq"])
                op("dve", lambda e: e.tensor_tensor(out=xsq[:m, 384:512], in0=xsq[:m, 256:384], in1=kib_t[:m, :], op=ALU.add), ["xsq", "kib"], ["xsq"])
                i = kt % 2
                op("pe", lambda e: e.transpose(psM[i][:, :m], xsq[:m, 384:512], I_t[:m, :m]), ["xsq", "I"], [("psM", i)])
                op("act", lambda e: e.copy(out=acc[:, t0:t0 + m], in_=psM[i][:, :m]), [("psM", i)], ["acc"])
            for c in range(0, T, 512):
                n = min(512, T - c)
                rope_k(kiT[:, c:c + n], acc[:, c:c + n], c, n, ["acc"], ["kiT"])
            cscale = (8 ** -0.5) * (128 ** -0.5)
            for j, (q0, m, nk) in enumerate(slots):
                for hh in range(8):
                    i = hh % 2
                    for kc in range(4):
                        op("pe", lambda e: e.matmul(psM[i][:, :m], lhsT=wiq_t[:, kc, hh * 128:(hh + 1) * 128], rhs=cqnT[:, kc, q0:q0 + m], start=(kc == 0), stop=(kc == 3)),
                           ["wiq", "cqnT"], [("psM", i)])
                    op("act", lambda e: e.copy(out=xT[:, :m], in_=psM[i][:, :m]), [("psM", i)], ["xT"])
                    rope_fm(qiS[:, hh, :m], xT[:, :m], m, cosq_t[:, q0:q0 + m], sinq_t[:, q0:q0 + m], ["xT"], ["qiS"], ["cosq", "sinq"])
                for c in range(0, nk, 512):
                    n = min(512, nk - c)
                    for hh in range(8):
                        i = hh % 2
                        op("pe", lambda e: e.matmul(psM[i][:m, :n], lhsT=qiS[:, hh, :m], rhs=kiT[:, c:c + n], start=True, stop=True), ["qiS", "kiT"], [("psM", i)])
                        op("act", lambda e: e.activation(out=rl[:m, :n], in_=psM[i][:m, :n], func=AF.Relu, scale=cscale), [("psM", i)], ["rl"])
                        if hh == 0:
                            op("dve", lambda e: e.tensor_scalar(out=acc[:m, c:c + n], in0=rl[:m, :n], scalar1=wq_t[:m, j, hh:hh + 1], scalar2=None, op0=ALU.mult), ["rl", "wq"], ["acc"])
                        else:
                            op("dve", lambda e: e.scalar_tensor_tensor(out=acc[:m, c:c + n], in0=rl[:m, :n], scalar=wq_t[:m, j, hh:hh + 1], in1=acc[:m, c:c + n], op0=ALU.mult, op1=ALU.add), ["rl", "wq", "acc"], ["acc"])
                for c in range(0, nk, 512):
                    n = min(512, nk - c)
                    op("dve", lambda e: e.tensor_scalar(out=qpc[:m, :], in0=qpos_t[:m, j, :], scalar1=float(-c), scalar2=None, op0=ALU.add), ["qpos"], ["qpc"])
                    op("dve", lambda e: e.tensor_scalar(out=wrk[:m, c:c + n], in0=kpos_t[:m, :n], scalar1=qpc[:m, :], scalar2=NEG, op0=ALU.is_gt, op1=ALU.mult), ["kpos", "qpc"], ["wrk"])
                op("dve", lambda e: e.tensor_tensor(out=acc[:m, :nk], in0=acc[:m, :nk], in1=wrk[:m, :nk], op=ALU.add), ["acc", "wrk"], ["acc"])
                src = acc
                for rnd in range(topk // 8):
                    op("dve", lambda e: e.max(out=m8[:m, :], in_=src[:m, :nk]), ["acc", "wrk"], ["m8"])
                    if rnd < topk // 8 - 1:
                        op("dve", lambda e: e.match_replace(out=wrk[:m, :nk], in_to_replace=m8[:m, :], in_values=src[:m, :nk], imm_value=-3.0e38), ["acc", "wrk", "m8"], ["wrk"])
                    src = wrk
                op("dve", lambda e: e.tensor_scalar(out=thr[:m, :], in0=m8[:m, 7:8], scalar1=-1.0e29, scalar2=None, op0=ALU.max), ["m8"], ["thr"])
                op("dve", lambda e: e.tensor_scalar(out=mk[:m, :nk], in0=acc[:m, :nk], scalar1=thr[:m, :], scalar2=None, op0=ALU.is_ge), ["acc", "thr"], ["mk"])
                for kt in range((nk + 127) // 128):
                    k0 = kt * 128; kn = min(128, nk - k0)
                    i = kt % 2
                    op("pe", lambda e: e.transpose(psT[i][:kn, :m], mk[:m, k0:k0 + kn], Ib_t[:m, :m]), ["mk", "Ib"], [("psT", i)])
                    op("act", lambda e: e.copy(out=maskT[:kn, kt, q0:q0 + m], in_=psT[i][:kn, :m]), [("psT", i)], ["maskT"])
        S.barrier()
        kr = sb("kr", [128, T], BF16); kraw = sb("kraw", [128, T]); va = sb("va", [128, NKT, 132], BF16); va32 = sb("va32", [128, NKT, 128])
        qTh = sb("qTh", [128, NQ], BF16); wuq_t = sb("wuq_t", [128, 4, 128])
        qchunks = [(c, min(512, NQ - c)) for c in range(0, NQ, 512)]
        ex = [sb(f"ex{i}", [128, 512]) for i in range(2)]
        pp = [sb(f"pp{i}", [128, 512], BF16) for i in range(2)]
        ot = [sb(f"ot{i}", [128, 128]) for i in range(2)]
        rs_ = sb("rs_", [128, 1])
        scale = 128 ** -0.5
        outs = []
        op("pool", lambda e: e.memset(va[:, :, 128:129], 1.0), [], ["va1"])
        gi = 0; oi = 0
        for h in range(NH):
            S.dma("sp", kraw[:, :], kT[h * 128:(h + 1) * 128, :], w=["kraw"])
            nfull = T // 128
            S.dma("act", va32[:, :nfull, :], v[0:nfull * 128, h * 128:(h + 1) * 128].rearrange("(kt p) d -> p kt d", p=128), w=["va32"])
            op("act", lambda e: e.copy(out=va[:, :nfull, 0:128], in_=va32[:, :nfull, :]), ["va32"], ["va"])
            if T % 128:
                S.dma("act", va32[:T % 128, nfull, :], v[nfull * 128:T, h * 128:(h + 1) * 128], w=["va32"])
                op("act", lambda e: e.copy(out=va[:T % 128, nfull, 0:128], in_=va32[:T % 128, nfull, :]), ["va32"], ["va"])
            for c in range(0, T, 512):
                n = min(512, T - c)
                rope_k(kr[:, c:c + n], kraw[:, c:c + n], c, n, ["kraw"], ["kr"])
            S.dma("sp", wuq_t[:], w_uq[:, h * 128:(h + 1) * 128].rearrange("(kc p) d -> p kc d", p=128), w=["wuq"])
            for (c, n) in qchunks:
                i = (c // 512) % 2
                for kc in range(4):
                    op("pe", lambda e: e.matmul(psR[i][:, :n], lhsT=wuq_t[:, kc, :], rhs=cqnT[:, kc, c:c + n], start=(kc == 0), stop=(kc == 3)), ["wuq", "cqnT"], [("psR", i)])
                op("act", lambda e: e.copy(out=xT[:, :n], in_=psR[i][:, :n]), [("psR", i)], ["xT"])
                rope_fm(qTh[:, c:c + n], xT[:, :n], n, cosq_t[:, c:c + n], sinq_t[:, c:c + n], ["xT"], ["qTh"], ["cosq", "sinq"])
            for j, (q0, m, nk) in enumerate(slots):
                nkt = (nk + 127) // 128
                po = psO[oi % 2]; pok = ("psO", oi % 2)
                for g0 in range(0, nkt, 4):
                    gn = min(4, nkt - g0)
                    i = gi % 2; gi += 1
                    for kk_ in range(gn):
                        kt = g0 + kk_; k0 = kt * 128; kn = min(128, nk - k0)
                        op("pe", lambda e: e.matmul(psM[i][:kn, kk_ * 128:kk_ * 128 + m], lhsT=kr[:, k0:k0 + kn], rhs=qTh[:, q0:q0 + m], start=True, stop=True), ["kr", "qTh"], [("psM", i)])
                    kn_last = min(128, nk - (g0 + gn - 1) * 128)
                    pv = psM[i][:, :gn * 128].rearrange("p (a q) -> p a q", q=128)
                    ev = ex[i][:, :gn * 128].rearrange("p (a q) -> p a q", q=128)
                    ppv = pp[i][:, :gn * 128].rearrange("p (a q) -> p a q", q=128)
                    if kn_last == 128:
                        op("act", lambda e: e.activation(out=ev[:, :, :m], in_=pv[:, :, :m], func=AF.Exp, scale=scale), [("psM", i)], [("ex", i)])
                        op("dve", lambda e: e.tensor_tensor(out=ppv[:, :, :m], in0=ev[:, :, :m], in1=maskT[:, g0:g0 + gn, q0:q0 + m], op=ALU.mult), [("ex", i), "maskT"], [("pp", i)])
                    else:
                        if gn > 1:
                            op("act", lambda e: e.activation(out=ev[:, :gn - 1, :m], in_=pv[:, :gn - 1, :m], func=AF.Exp, scale=scale), [("psM", i)], [("ex", i)])
                            op("dve", lambda e: e.tensor_tensor(out=ppv[:, :gn - 1, :m], in0=ev[:, :gn - 1, :m], in1=maskT[:, g0:g0 + gn - 1, q0:q0 + m], op=ALU.mult), [("ex", i), "maskT"], [("pp", i)])
                        op("act", lambda e: e.activation(out=ev[:kn_last, gn - 1, :m], in_=pv[:kn_last, gn - 1, :m], func=AF.Exp, scale=scale), [("psM", i)], [("ex", i)])
                        op("dve", lambda e: e.tensor_tensor(out=ppv[:kn_last, gn - 1, :m], in0=ev[:kn_last, gn - 1, :m], in1=maskT[:kn_last, g0 + gn - 1, q0:q0 + m], op=ALU.mult), [("ex", i), "maskT"], [("pp", i)])
                    for kk_ in range(gn):
                        kt = g0 + kk_; kn = min(128, nk - kt * 128)
                        op("pe", lambda e: e.matmul(po[:m, :129], lhsT=ppv[:kn, kk_, :m], rhs=va[:kn, kt, :129], start=(kt == 0), stop=(kt == nkt - 1)), [("pp", i), "va", "va1"], [pok])
                o = ot[oi % 2]
                op("dve", lambda e: e.reciprocal(out=rs_[:m, :], in_=po[:m, 128:129]), [pok], ["rs_"])
                op("dve", lambda e: e.tensor_scalar(out=o[:m, :], in0=po[:m, :128], scalar1=rs_[:m, :], scalar2=None, op0=ALU.mult), [pok, "rs_"], [("ot", oi % 2)])
                outs.append(S.dma("pool", y[q0:q0 + m, h * 128:(h + 1) * 128], o[:m, :], r=[("ot", oi % 2)]))
                oi += 1
        S.finish(outs, "pool")
        print("ninst", S.ninst, "nsem", S.nsem)
    return nc


def rope_tables(pos):
    inv = (10000.0 ** (-np.arange(0, 128, 2, dtype=np.float32) / 128)).astype(np.float32)
    ang = (pos.astype(np.float32)[:, None] * inv[None, :]).astype(np.float32)
    c = np.cos(ang.astype(np.float64)).astype(np.float32); s = np.sin(ang.astype(np.float64)).astype(np.float32)
    cos_fm = np.concatenate([c, c], axis=1).T
    sin_fm = np.concatenate([-s, s], axis=1).T
    return np.ascontiguousarray(cos_fm), np.ascontiguousarray(sin_fm)


def dsa_consts(T):
    R = np.zeros((128, 128), np.float32)
    for i in range(64):
        R[i + 64, i] = 1; R[i, i + 64] = 1
    I = np.eye(128, dtype=np.float32)
    kpos = np.ascontiguousarray(np.broadcast_to(np.arange(T, dtype=np.float32)[None, :], (128, T)))
    return R, I, kpos


def core_qpos(r, NS):
    pos = []
    for j in range(NS):
        pos.extend(range(512 * j + 128 * r, 512 * j + 128 * r + 128))
    pos.extend(range(512 * NS + 4 * r, 512 * NS + 4 * r + 4))
    return np.array(pos)


def dsa_inmap(r, NS, cq, ka, va, kidx, widx, qnorm_g, w_uq, w_iq, kidx_g, kidx_b):
    T = cq.shape[0]
    pos = core_qpos(r, NS)
    R, I, kpos = dsa_consts(T)
    cos_fm, sin_fm = rope_tables(np.arange(T))
    cosq, sinq = rope_tables(pos)
    bc = lambda a: np.ascontiguousarray(np.broadcast_to(a[None, :], (128, a.shape[0])).astype(np.float32))
    return dict(cq=np.ascontiguousarray(cq[pos]), widx=np.ascontiguousarray(widx[pos]), qpos=pos.astype(np.float32)[:, None].copy(),
                kT=np.ascontiguousarray(ka.T), v=np.ascontiguousarray(va), kidx=np.ascontiguousarray(kidx),
                w_uq=np.ascontiguousarray(w_uq), w_iq=np.ascontiguousarray(w_iq),
                qg=np.ascontiguousarray(qnorm_g.reshape(4, 128).T), kig=bc(kidx_g), kib=bc(kidx_b),
                cos_fm=cos_fm, sin_fm=sin_fm, cosq=cosq, sinq=sinq, kpos=kpos, cR=R, cI=I)


WINS = (2, 4, 8, 16)

def build_pool(NT=1028):
    nc = new_nc()
    di = lambda n, s: nc.dram_tensor(n, s, F32, kind="ExternalInput").ap()
    pinT = di("pinT", [1024, NT + 15]); icnt = di("icnt", [4, 128, NT]); wp = di("wp", [4, 256, 256]); psc = di("psc", [128, 8])
    yT = nc.dram_tensor("yT", [1024, NT], F32, kind="ExternalOutput").ap()
    es = contextlib.ExitStack()
    with es:
        S = Sched(nc, es)
        sb = lambda n, s, d=F32: es.enter_context(nc.sbuf_tensor(n, s, d))
        W = NT + 15
        x = sb("x", [128, 8, W]); a = sb("a", [128, W]); b = sb("b", [128, W]); pl = sb("pl", [128, 8, NT])
        ic = sb("ic", [128, 4, NT]); wt = sb("wt", [128, 4, 2, 256]); sc = sb("sc", [128, 8]); o = [sb(f"o{i}", [128, NT]) for i in range(2)]
        ps = [es.enter_context(nc.psum_tensor(f"ps{i}", [128, 512], F32)) for i in range(2)]
        S.dma("sp", x[:], pinT.rearrange("(c p) t -> p c t", p=128), w=["x"])
        S.dma("act", ic[:], icnt.rearrange("w p t -> p w t"), w=["ic"])
        S.dma("act", wt[:], wp.rearrange("g (kc p) d -> p g kc d", p=128), w=["wt"])
        S.dma("sp", sc[:], psc, w=["sc"])
        for c in range(8):
            g = c // 2; win = WINS[g]
            src = x[:, c, :]; sh = 1; cur = None
            bufs = [a, b]; bi = 0
            prev = src
            while sh < win:
                dst = bufs[bi]; bi ^= 1
                op_eng = "dve" if c % 2 == 0 else "pool"
                S.op(op_eng, lambda e: e.tensor_tensor(out=dst[:, sh:W], in0=prev[:, sh:W], in1=prev[:, 0:W - sh], op=ALU.add), r=["x", "a", "b"], w=["a" if dst is a else "b"])
                if sh > 0:
                    S.op(op_eng, lambda e: e.tensor_copy(out=dst[:, 0:sh], in_=prev[:, 0:sh]), r=["x", "a", "b"], w=["a" if dst is a else "b"])
                prev = dst; sh *= 2
            S.op("dve", lambda e: e.tensor_tensor(out=pl[:, c, :], in0=prev[:, 15:W], in1=ic[:, g, :], op=ALU.mult), r=["a", "b", "ic"], w=[("pl", c)])
            S.op("dve", lambda e: e.tensor_tensor(out=pl[:, c, :], in0=pl[:, c, :], in1=x[:, c, 15:W], op=ALU.subtract), r=[("pl", c), "x"], w=[("pl", c)])
        outs = []
        k = 0
        for g in range(4):
            for oc in range(2):
                ot = o[k % 2]
                for t0 in range(0, NT, 512):
                    n = min(512, NT - t0)
                    p = ps[(k + t0 // 512) % 2]; pk = ("ps", (k + t0 // 512) % 2)
                    for kc in range(2):
                        S.op("pe", lambda e: e.matmul(p[:, :n], lhsT=wt[:, g, kc, oc * 128:(oc + 1) * 128], rhs=pl[:, 2 * g + kc, t0:t0 + n], start=(kc == 0), stop=(kc == 1)),
                             r=["wt", ("pl", 2 * g + kc)], w=[pk])
                    S.op("act", lambda e: e.activation(out=ot[:, t0:t0 + n], in_=p[:, :n], func=AF.Copy, scale=sc[:, 2 * g + oc:2 * g + oc + 1]), r=[pk, "sc"], w=[("o", k % 2)])
                outs.append(S.dma("pool", yT[(2 * g + oc) * 128:(2 * g + oc + 1) * 128, :], ot[:], r=[("o", k % 2)]))
                k += 1
        S.finish(outs, "pool")
    return nc

def pool_inmap(pin_b, t0, NT, pool_w, pool_scale):
    T = pin_b.shape[0]
    pad = np.concatenate([np.zeros((15, 1024), np.float32), pin_b], axis=0)
    sl = pad[t0:t0 + NT + 15]
    tt = np.arange(t0, t0 + NT)
    icnt = np.stack([np.broadcast_to((1.0 / np.minimum(tt + 1, w)).astype(np.float32)[None, :], (128, NT)) for w in WINS])
    return dict(pinT=np.ascontiguousarray(sl.T), icnt=np.ascontiguousarray(icnt), wp=np.ascontiguousarray(pool_w),
                psc=np.ascontiguousarray(pool_scale.reshape(8, 128).T))


import math

ALPHA = 2.0 ** 0.5
LN_EPS = 1e-5
NEGB = -1.0e30


def ln_rows(S, u, m, D, gb, bb, out, sq, st, uk, ok):
    S.op("dve", lambda e: e.tensor_reduce(out=st[:m, 0:1], in_=u[:m, :], axis=AX.X, op=ALU.add), r=[uk], w=["ln_st"])
    S.op("dve", lambda e: e.tensor_scalar(out=st[:m, 1:2], in0=st[:m, 0:1], scalar1=-1.0 / D, scalar2=None, op0=ALU.mult), r=["ln_st"], w=["ln_st"])
    S.op("dve", lambda e: e.tensor_scalar(out=u[:m, :], in0=u[:m, :], scalar1=st[:m, 1:2], scalar2=None, op0=ALU.add), r=[uk, "ln_st"], w=[uk])
    S.op("pool", lambda e: e.tensor_tensor(out=sq[:m, :], in0=u[:m, :], in1=u[:m, :], op=ALU.mult), r=[uk], w=["ln_sq"])
    S.op("dve", lambda e: e.tensor_reduce(out=st[:m, 0:1], in_=sq[:m, :], axis=AX.X, op=ALU.add), r=["ln_sq"], w=["ln_st"])
    S.op("dve", lambda e: e.tensor_scalar(out=st[:m, 1:2], in0=st[:m, 0:1], scalar1=1.0 / D, scalar2=LN_EPS, op0=ALU.mult, op1=ALU.add), r=["ln_st"], w=["ln_st"])
    S.op("act", lambda e: e.activation(out=st[:m, 2:3], in_=st[:m, 1:2], func=AF.Sqrt), r=["ln_st"], w=["ln_st"])
    S.op("dve", lambda e: e.reciprocal(out=st[:m, 3:4], in_=st[:m, 2:3]), r=["ln_st"], w=["ln_st"])
    S.op("dve", lambda e: e.scalar_tensor_tensor(out=sq[:m, :], in0=u[:m, :], scalar=st[:m, 3:4], in1=gb[:m, :], op0=ALU.mult, op1=ALU.mult), r=[uk, "ln_st", "lngb"], w=["ln_sq"])
    S.op("pool", lambda e: e.tensor_tensor(out=out[:m, :], in0=sq[:m, :], in1=bb[:m, :], op=ALU.add), r=["ln_sq", "lngb"], w=[ok])


def build_s3(NT=1028, D=4096, CAP=192, cb=256):
    nc = new_nc()
    KC = D // 128
    di = lambda n, s, d=F32: nc.dram_tensor(n, s, d, kind="ExternalInput").ap()
    do = lambda n, s, d=F32: nc.dram_tensor(n, s, d, kind="ExternalOutput").ap()
    mixT = di("mixT", [D, NT]); hrow = di("hrow", [NT, D]); w_out = di("w_out", [D, D])
    lng = di("lng", [128, D]); lnb = di("lnb", [128, D]); wr = di("wr", [D, 72]); rbias = di("rbias", [128, 72])
    cU = di("cU", [128, 128]); cOnes = di("cOnes", [128, 128]); cIota = di("cIota", [128, 8]); cI = di("cI", [128, 128])
    h1 = do("h1", [NT, D]); xg = do("xg", [8 * CAP, D]); gsc = do("gsc", [8 * CAP, 8]); idx = do("idx", [NT, 1], I32)
    z = nc.dram_tensor("z", [NT, D], F32, kind="Internal").ap()
    toks = [(t0, min(128, NT - t0)) for t0 in range(0, NT, 128)]
    es = contextlib.ExitStack()
    with es:
        S = Sched(nc, es)
        sb = lambda n, s, d=F32, st=es: st.enter_context(nc.sbuf_tensor(n, s, d))
        ps = [es.enter_context(nc.psum_tensor(f"ps{i}", [128, 512], F32)) for i in range(4)]
        pa = contextlib.ExitStack()
        with pa:
            mm_phase(nc, S, pa, mixT, w_out, z, NT, D, D, ps, cb, dst_key="z")
        S.barrier()
        gb = sb("gb", [128, D]); bb = sb("bb", [128, D]); zt = sb("zt", [128, D]); ht = sb("ht", [128, D]); sq = sb("sq", [128, D]); h1t = sb("h1t", [128, D])
        h1T = sb("h1T", [128, KC, 128]); wr_t = sb("wr_t", [128, KC, 72]); rb_t = sb("rb_t", [128, 72])
        U_t = sb("U_t", [128, 128]); On_t = sb("On_t", [128, 128]); Io_t = sb("Io_t", [128, 8]); I_t = sb("I_t", [128, 128]); zero = sb("zero", [128, D])
        st = sb("st", [128, 4]); lg = sb("lg", [128, 72]); sm = sb("sm", [128, 16]); goh = sb("goh", [128, 8]); ex8 = sb("ex8", [128, 8])
        esel = sb("esel", [128, 8]); e2 = sb("e2", [128, 8]); oh1 = sb("oh1", [128, 8]); oh2 = sb("oh2", [128, 8]); gvec = sb("gvec", [128, 8]); t8 = sb("t8", [128, 8])
        carry = sb("carry", [128, 8]); pos8 = sb("pos8", [128, 8]); idx_t = sb("idx_t", [128, 1], I32)
        S.dma("sp", gb[:], lng, w=["lngb"]); S.dma("act", bb[:], lnb, w=["lngb"])
        S.dma("sp", wr_t[:], wr.rearrange("(kc p) n -> p kc n", p=128), w=["wr"]); S.dma("act", rb_t[:], rbias, w=["rb"])
        S.dma("sp", U_t[:], cU, w=["U"]); S.dma("act", On_t[:], cOnes, w=["On"]); S.dma("sp", Io_t[:], cIota, w=["Io"]); S.dma("act", I_t[:], cI, w=["I"])
        S.op("pool", lambda e: e.memset(zero[:], 0.0), w=["zero"])
        S.op("pool", lambda e: e.memset(carry[:], 0.0), w=["carry"])
        for r0 in range(0, 8 * CAP, 128):
            S.dma("sp" if (r0 // 128) % 2 == 0 else "act", xg[r0:r0 + 128, :], zero[:], r=["zero"], w=["xg"])
        S.dma("sp", gsc.rearrange("(a p) e -> p a e", p=128), zero[:, :8 * CAP // 128 * 8].rearrange("p (a e) -> p a e", e=8), r=["zero"], w=["gsc"])
        outs = []
        for (t0, m) in toks:
            S.dma("sp", zt[:m, :], z[t0:t0 + m, :], r=[("z", t0)], w=["zt"])
            S.dma("act", ht[:m, :], hrow[t0:t0 + m, :], w=["ht"])
            S.op("dve", lambda e: e.scalar_tensor_tensor(out=zt[:m, :], in0=ht[:m, :], scalar=ALPHA, in1=zt[:m, :], op0=ALU.mult, op1=ALU.add), r=["ht", "zt"], w=["zt"])
            ln_rows(S, zt, m, D, gb, bb, h1t, sq, st, "zt", "h1t")
            outs.append(S.dma("sp", h1[t0:t0 + m, :], h1t[:m, :], r=["h1t"]))
            for k4 in range(0, KC, 4):
                p = ps[(k4 // 4) % 4]; pk = ("ps", (k4 // 4) % 4)
                for kk in range(4):
                    kc = k4 + kk
                    S.op("pe", lambda e: e.transpose(p[:, kk * 128:kk * 128 + m], h1t[:m, kc * 128:(kc + 1) * 128], I_t[:m, :m]), r=["h1t", "I"], w=[pk])
                pv = p[:, :].rearrange("p (a q) -> p a q", q=128)
                S.op("act" if (k4 // 4) % 2 == 0 else "dve", (lambda e: e.copy(out=h1T[:, k4:k4 + 4, :m], in_=pv[:, :, :m])) if (k4 // 4) % 2 == 0 else (lambda e: e.tensor_copy(out=h1T[:, k4:k4 + 4, :m], in_=pv[:, :, :m])), r=[pk], w=["h1T"])
            pl = ps[0]
            for kc in range(KC):
                S.op("pe", lambda e: e.matmul(pl[:m, :72], lhsT=h1T[:, kc, :m], rhs=wr_t[:, kc, :], start=(kc == 0), stop=(kc == KC - 1)), r=["h1T", "wr"], w=[("ps", 0)])
            S.op("dve", lambda e: e.tensor_tensor(out=lg[:m, :], in0=pl[:m, :72], in1=rb_t[:m, :], op=ALU.add), r=[("ps", 0), "rb"], w=["lg"])
            D_ = lambda fn, r, w: S.op("dve", fn, r=r, w=w)
            D_(lambda e: e.tensor_reduce(out=sm[:m, 0:1], in_=lg[:m, 0:8], axis=AX.X, op=ALU.max), ["lg"], ["sm"])
            D_(lambda e: e.tensor_scalar(out=goh[:m, :], in0=lg[:m, 0:8], scalar1=sm[:m, 0:1], scalar2=None, op0=ALU.is_equal), ["lg", "sm"], ["goh"])
            D_(lambda e: e.tensor_scalar(out=sm[:m, 1:2], in0=sm[:m, 0:1], scalar1=-1.0, scalar2=None, op0=ALU.mult), ["sm"], ["sm"])
            S.op("act", lambda e: e.activation(out=ex8[:m, :], in_=lg[:m, 0:8], func=AF.Exp, bias=sm[:m, 1:2]), r=["lg", "sm"], w=["ex8"])
            D_(lambda e: e.tensor_reduce(out=sm[:m, 2:3], in_=ex8[:m, :], axis=AX.X, op=ALU.add), ["ex8"], ["sm"])
            D_(lambda e: e.reciprocal(out=sm[:m, 3:4], in_=sm[:m, 2:3]), ["sm"], ["sm"])
            for g in range(8):
                if g == 0:
                    D_(lambda e: e.tensor_scalar(out=esel[:m, :], in0=lg[:m, 8:16], scalar1=goh[:m, 0:1], scalar2=None, op0=ALU.mult), ["lg", "goh"], ["esel"])
                else:
                    D_(lambda e: e.scalar_tensor_tensor(out=esel[:m, :], in0=lg[:m, 8 + 8 * g:16 + 8 * g], scalar=goh[:m, g:g + 1], in1=esel[:m, :], op0=ALU.mult, op1=ALU.add), ["lg", "goh", "esel"], ["esel"])
            D_(lambda e: e.tensor_reduce(out=sm[:m, 4:5], in_=esel[:m, :], axis=AX.X, op=ALU.max), ["esel"], ["sm"])
            D_(lambda e: e.tensor_scalar(out=oh1[:m, :], in0=esel[:m, :], scalar1=sm[:m, 4:5], scalar2=None, op0=ALU.is_equal), ["esel", "sm"], ["oh1"])
            D_(lambda e: e.scalar_tensor_tensor(out=e2[:m, :], in0=oh1[:m, :], scalar=NEGB, in1=esel[:m, :], op0=ALU.mult, op1=ALU.add), ["oh1", "esel"], ["e2"])
            D_(lambda e: e.tensor_reduce(out=sm[:m, 5:6], in_=e2[:m, :], axis=AX.X, op=ALU.max), ["e2"], ["sm"])
            D_(lambda e: e.tensor_scalar(out=oh2[:m, :], in0=e2[:m, :], scalar1=sm[:m, 5:6], scalar2=None, op0=ALU.is_equal), ["e2", "sm"], ["oh2"])
            D_(lambda e: e.tensor_tensor(out=sm[:m, 6:7], in0=sm[:m, 5:6], in1=sm[:m, 4:5], op=ALU.subtract), ["sm"], ["sm"])
            S.op("act", lambda e: e.activation(out=sm[:m, 7:8], in_=sm[:m, 6:7], func=AF.Exp), r=["sm"], w=["sm"])
            D_(lambda e: e.tensor_scalar(out=sm[:m, 8:9], in0=sm[:m, 7:8], scalar1=1.0, scalar2=None, op0=ALU.add), ["sm"], ["sm"])
            D_(lambda e: e.reciprocal(out=sm[:m, 9:10], in_=sm[:m, 8:9]), ["sm"], ["sm"])
            D_(lambda e: e.tensor_tensor(out=sm[:m, 10:11], in0=sm[:m, 9:10], in1=sm[:m, 3:4], op=ALU.mult), ["sm"], ["sm"])
            D_(lambda e: e.tensor_tensor(out=sm[:m, 11:12], in0=sm[:m, 3:4], in1=sm[:m, 10:11], op=ALU.subtract), ["sm"], ["sm"])
            D_(lambda e: e.tensor_scalar(out=t8[:m, :], in0=oh1[:m, :], scalar1=sm[:m, 10:11], scalar2=None, op0=ALU.mult), ["oh1", "sm"], ["t8"])
            D_(lambda e: e.scalar_tensor_tensor(out=gvec[:m, :], in0=oh2[:m, :], scalar=sm[:m, 11:12], in1=t8[:m, :], op0=ALU.mult, op1=ALU.add), ["oh2", "sm", "t8"], ["gvec"])
            D_(lambda e: e.tensor_tensor(out=t8[:m, :], in0=goh[:m, :], in1=Io_t[:m, :], op=ALU.mult), ["goh", "Io", "gvec"], ["t8"])
            D_(lambda e: e.tensor_reduce(out=sm[:m, 12:13], in_=t8[:m, :], axis=AX.X, op=ALU.add), ["t8"], ["sm"])
            S.op("pe", lambda e: e.matmul(ps[1][:m, 0:8], lhsT=U_t[:m, :m], rhs=goh[:m, :], start=True, stop=True), r=["U", "goh"], w=[("ps", 1)])
            S.op("pe", lambda e: e.matmul(ps[2][:, 0:8], lhsT=On_t[:m, :], rhs=goh[:m, :], start=True, stop=True), r=["On", "goh"], w=[("ps", 2)])
            D_(lambda e: e.tensor_tensor(out=pos8[:m, :], in0=ps[1][:m, 0:8], in1=carry[:m, :], op=ALU.add), [("ps", 1), "carry"], ["pos8"])
            D_(lambda e: e.tensor_tensor(out=carry[:, :], in0=carry[:, :], in1=ps[2][:, 0:8], op=ALU.add), [("ps", 2), "carry"], ["carry"])
            D_(lambda e: e.tensor_tensor(out=t8[:m, :], in0=goh[:m, :], in1=pos8[:m, :], op=ALU.mult), ["goh", "pos8", "sm"], ["t8"])
            D_(lambda e: e.tensor_reduce(out=sm[:m, 13:14], in_=t8[:m, :], axis=AX.X, op=ALU.add), ["t8"], ["sm"])
            D_(lambda e: e.tensor_scalar(out=sm[:m, 14:15], in0=sm[:m, 13:14], scalar1=float(CAP), scalar2=1.0e6, op0=ALU.is_ge, op1=ALU.mult), ["sm"], ["sm"])
            D_(lambda e: e.scalar_tensor_tensor(out=sm[:m, 15:16], in0=sm[:m, 12:13], scalar=float(CAP), in1=sm[:m, 13:14], op0=ALU.mult, op1=ALU.add), ["sm"], ["sm"])
            D_(lambda e: e.tensor_tensor(out=sm[:m, 15:16], in0=sm[:m, 15:16], in1=sm[:m, 14:15], op=ALU.add), ["sm"], ["sm"])
            D_(lambda e: e.tensor_copy(out=idx_t[:m, :], in_=sm[:m, 15:16]), ["sm"], ["idx_t"])
            outs.append(S.dma("pool", None, None, r=["h1t", "idx_t"], w=["xg"], fn=lambda e: e.indirect_dma_start(
                out=xg[:, :], out_offset=bass.IndirectOffsetOnAxis(ap=idx_t[:m, 0:1], axis=0), in_=h1t[:m, :], in_offset=None, bounds_check=8 * CAP - 1, oob_is_err=False)))
            outs.append(S.dma("pool", None, None, r=["gvec", "idx_t"], w=["gsc"], fn=lambda e: e.indirect_dma_start(
                out=gsc[:, :], out_offset=bass.IndirectOffsetOnAxis(ap=idx_t[:m, 0:1], axis=0), in_=gvec[:m, :], in_offset=None, bounds_check=8 * CAP - 1, oob_is_err=False)))
            outs.append(S.dma("sp", idx[t0:t0 + m, :], idx_t[:m, :], r=["idx_t"]))
        S.finish(outs, "pool")
        print("s3 ninst", S.ninst)
    return nc


def build_E(R=1536, D=4096, DE=512, RB=512):
    nc = new_nc()
    KC = D // 128; DC = DE // 128; NE = 8
    di = lambda n, s, d=F32: nc.dram_tensor(n, s, d, kind="ExternalInput").ap()
    X = di("X", [R, D]); G = di("G", [R, NE]); w1 = di("w1", [NE, D, DE]); w3 = di("w3", [NE, D, DE]); w2 = di("w2", [NE, DE, D]); cI = di("cI", [128, 128])
    Y = nc.dram_tensor("Y", [R, D], F32, kind="ExternalOutput").ap()
    RT = RB // 128
    es = contextlib.ExitStack()
    with es:
        S = Sched(nc, es)
        sb = lambda n, s, d=F32: es.enter_context(nc.sbuf_tensor(n, s, d))
        pA = [es.enter_context(nc.psum_tensor(f"pA{i}", [128, 512], F32)) for i in range(2)]
        pB = [es.enter_context(nc.psum_tensor(f"pB{i}", [128, 512], F32)) for i in range(2)]
        pY = [es.enter_context(nc.psum_tensor(f"pY{i}", [128, 512], F32)) for i in range(2)]
        pT = [es.enter_context(nc.psum_tensor(f"pT{i}", [128, 512], F32)) for i in range(2)]
        xr = [sb("xr0", [128, D])]; XT = sb("XT", [128, KC, RB], BF16); Ya = sb("Ya", [128, RT, D]); g_t = sb("g_t", [128, RT, NE])
        hT = sb("hT", [128, DC, RB], BF16); sl = sb("sl", [128, RB]); I_t = sb("I_t", [128, 128])
        w1s = sb("w1s", [128, KC, 128]); w3s = sb("w3s", [128, KC, 128]); w2s = sb("w2s", [128, DC, 512])
        w1b = [sb(f"w1b{i}", [128, KC, 128], BF16) for i in range(2)]; w3b = [sb(f"w3b{i}", [128, KC, 128], BF16) for i in range(2)]
        w2b = [sb(f"w2b{i}", [128, DC, 512], BF16) for i in range(2)]
        S.dma("sp", I_t[:], cI, w=["I"])
        outs = []
        wi = 0; w2i = 0; ti = 0; yi = 0; xi = 0
        for r0 in range(0, R, RB):
            S.dma("act", g_t[:], G[r0:r0 + RB, :].rearrange("(a p) e -> p a e", p=128), w=["g"])
            for a in range(RT):
                xb_ = xr[0]; xk = ("xr", 0); xi += 1
                S.dma("sp", xb_[:], X[r0 + a * 128:r0 + (a + 1) * 128, :], w=[xk])
                for k4 in range(0, KC, 4):
                    p = pT[ti % 2]; pk = ("pT", ti % 2)
                    for kk in range(4):
                        S.op("pe", lambda e: e.transpose(p[:, kk * 128:(kk + 1) * 128], xb_[:, (k4 + kk) * 128:(k4 + kk + 1) * 128], I_t[:]), r=[xk, "I"], w=[pk])
                    pv = p[:, :].rearrange("p (k q) -> p k q", q=128)
                    if ti % 2 == 0:
                        S.op("act", lambda e: e.copy(out=XT[:, k4:k4 + 4, a * 128:(a + 1) * 128], in_=pv), r=[pk], w=["XT"])
                    else:
                        S.op("dve", lambda e: e.tensor_copy(out=XT[:, k4:k4 + 4, a * 128:(a + 1) * 128], in_=pv), r=[pk], w=["XT"])
                    ti += 1
            for ex in range(NE):
                for dc in range(DC):
                    i = wi % 2; wi += 1
                    S.dma("sp", w1s[:], w1[ex, :, dc * 128:(dc + 1) * 128].rearrange("(kc p) d -> p kc d", p=128), w=["w1s"])
                    S.dma("act", w3s[:], w3[ex, :, dc * 128:(dc + 1) * 128].rearrange("(kc p) d -> p kc d", p=128), w=["w3s"])
                    S.op("dve", lambda e: e.tensor_copy(out=w1b[i][:], in_=w1s[:]), r=["w1s"], w=[("w1b", i)])
                    S.op("act", lambda e: e.copy(out=w3b[i][:], in_=w3s[:]), r=["w3s"], w=[("w3b", i)])
                    for kc in range(KC):
                        S.op("pe", lambda e: e.matmul(pA[i][:, :RB], lhsT=w1b[i][:, kc, :], rhs=XT[:, kc, :], start=(kc == 0), stop=(kc == KC - 1)), r=[("w1b", i), "XT"], w=[("pA", i)])
                    for kc in range(KC):
                        S.op("pe", lambda e: e.matmul(pB[i][:, :RB], lhsT=w3b[i][:, kc, :], rhs=XT[:, kc, :], start=(kc == 0), stop=(kc == KC - 1)), r=[("w3b", i), "XT"], w=[("pB", i)])
                    S.op("act", lambda e: e.activation(out=sl[:, :], in_=pA[i][:, :RB], func=AF.Silu), r=[("pA", i)], w=["sl"])
                    S.op("dve", lambda e: e.tensor_tensor(out=hT[:, dc, :], in0=sl[:, :], in1=pB[i][:, :RB], op=ALU.mult), r=["sl", ("pB", i)], w=["hT"])
                for cb in range(D // 512):
                    i = w2i % 2; w2i += 1
                    S.dma("sp" if cb % 2 == 0 else "act", w2s[:], w2[ex, :, cb * 512:(cb + 1) * 512].rearrange("(kc p) n -> p kc n", p=128), w=["w2s"])
                    if cb % 2 == 0:
                        S.op("act", lambda e: e.copy(out=w2b[i][:], in_=w2s[:]), r=["w2s"], w=[("w2b", i)])
                    else:
                        S.op("dve", lambda e: e.tensor_copy(out=w2b[i][:], in_=w2s[:]), r=["w2s"], w=[("w2b", i)])
                    for a in range(RT):
                        p = pY[yi % 2]; pk = ("pY", yi % 2); yi += 1
                        for kc in range(DC):
                            S.op("pe", lambda e: e.matmul(p[:, :], lhsT=hT[:, kc, a * 128:(a + 1) * 128], rhs=w2b[i][:, kc, :], start=(kc == 0), stop=(kc == DC - 1)), r=["hT", ("w2b", i)], w=[pk])
                        if ex == 0:
                            S.op("dve", lambda e: e.tensor_scalar(out=Ya[:, a, cb * 512:(cb + 1) * 512], in0=p[:, :], scalar1=g_t[:, a, ex:ex + 1], scalar2=None, op0=ALU.mult), r=[pk, "g"], w=[("Ya", a, cb)])
                        else:
                            S.op("dve", lambda e: e.scalar_tensor_tensor(out=Ya[:, a, cb * 512:(cb + 1) * 512], in0=p[:, :], scalar=g_t[:, a, ex:ex + 1], in1=Ya[:, a, cb * 512:(cb + 1) * 512], op0=ALU.mult, op1=ALU.add),
                                 r=[pk, "g", ("Ya", a, cb)], w=[("Ya", a, cb)])
            outs.append(S.dma("pool", Y[r0:r0 + RB, :].rearrange("(a p) d -> p a d", p=128), Ya[:], r=[("Ya", a, cb) for a in range(RT) for cb in range(D // 512)]))
        S.finish(outs, "pool")
        print("E ninst", S.ninst)
    return nc


def build_C(NT=1028, D=4096, CAP=192):
    nc = new_nc()
    di = lambda n, s, d=F32: nc.dram_tensor(n, s, d, kind="ExternalInput").ap()
    Yg = di("Yg", [8 * CAP, D]); idx = di("idx", [NT, 1], I32); h1 = di("h1", [NT, D]); lng = di("lng", [128, D]); lnb = di("lnb", [128, D])
    h2 = nc.dram_tensor("h2", [NT, D], F32, kind="ExternalOutput").ap()
    es = contextlib.ExitStack()
    with es:
        S = Sched(nc, es)
        sb = lambda n, s, d=F32: es.enter_context(nc.sbuf_tensor(n, s, d))
        gb = sb("gb", [128, D]); bb = sb("bb", [128, D]); ff = sb("ff", [128, D]); ht = sb("ht", [128, D]); sq = sb("sq", [128, D]); ot = sb("ot", [128, D])
        st = sb("st", [128, 4]); idx_t = sb("idx_t", [128, 1], I32)
        S.dma("sp", gb[:], lng, w=["lngb"]); S.dma("act", bb[:], lnb, w=["lngb"])
        outs = []
        for t0 in range(0, NT, 128):
            m = min(128, NT - t0)
            S.dma("sp", idx_t[:m, :], idx[t0:t0 + m, :], w=["idx_t"])
            S.dma("act", ht[:m, :], h1[t0:t0 + m, :], w=["ht"])
            S.dma("pool", None, None, r=["idx_t"], w=["ff"], fn=lambda e: e.indirect_dma_start(
                out=ff[:m, :], out_offset=None, in_=Yg[:, :], in_offset=bass.IndirectOffsetOnAxis(ap=idx_t[:m, 0:1], axis=0), bounds_check=8 * CAP - 1, oob_is_err=False))
            S.op("dve", lambda e: e.scalar_tensor_tensor(out=ff[:m, :], in0=ht[:m, :], scalar=ALPHA, in1=ff[:m, :], op0=ALU.mult, op1=ALU.add), r=["ht", "ff"], w=["ff"])
            ln_rows(S, ff, m, D, gb, bb, ot, sq, st, "ff", "ot")
            outs.append(S.dma("sp", h2[t0:t0 + m, :], ot[:m, :], r=["ot"]))
        S.finish(outs, "sp")
    return nc


def s3_consts():
    U = np.triu(np.ones((128, 128), np.float32), 1)
    return dict(cU=U, cOnes=np.ones((128, 128), np.float32), cIota=np.ascontiguousarray(np.broadcast_to(np.arange(8, dtype=np.float32)[None, :], (128, 8))),
                cI=np.eye(128, dtype=np.float32))

def bc128(a):
    return np.ascontiguousarray(np.broadcast_to(a[None, :], (128, a.shape[0])).astype(np.float32))


B_, SEQ_, D_, NMETA_ = 2, 4096, 4096, 16
T_ = SEQ_ + NMETA_
NT_ = T_ // 4
CAP_ = 192
_NC = {}

def _get(name, fn):
    if name not in _NC:
        t0 = time.time()
        _NC[name] = fn()
        print(f"[kernel] built {name} in {time.time() - t0:.1f}s", flush=True)
    return _NC[name]

def _run(name, nc, ims):
    t0 = time.time()
    res = run_bass_kernel_spmd(nc, ims, core_ids=list(range(8))).results
    print(f"[kernel] ran {name} in {time.time() - t0:.1f}s", flush=True)
    return res

def kernel(x, meta, w_in, rw_mu, rw_w2, rw_w0, rw_a2, rw_a0, rw_g2, rw_kk, rw_ka, rw_rk,
           rw_gn_g, rw_gn_b, at_qnorm_g, at_w_uq, at_w_iq, at_kidx_g, at_kidx_b, pool_w,
           pool_scale, w_out, ln1_g, ln1_b, router_g_w, router_g_b, router_e_w, router_e_b,
           exp_w1, exp_w3, exp_w2, ln2_g, ln2_b):
    A = lambda a: np.ascontiguousarray(np.asarray(a, dtype=np.float32))
    x = np.asarray(x, np.float32); meta = np.asarray(meta, np.float32)
    h = np.concatenate([np.broadcast_to(meta[None], (B_, NMETA_, D_)), x], axis=1).reshape(B_ * T_, D_)
    cs = s3_consts()
    for l in range(2):
        nc = _get("mm", lambda: build_mm(NT_, D_, 10088))
        wl = A(w_in[l])
        res = _run("proj", nc, [dict(xT=A(h[c * NT_:(c + 1) * NT_].T), w=wl) for c in range(8)])
        proj = np.concatenate([res[c]["out"] for c in range(8)]).reshape(B_, T_, 10088)
        del res, wl
        o = 0
        def take(n):
            nonlocal o
            s = slice(o, o + n); o += n
            return s
        s_r, s_k, s_v, s_dw, s_da, s_dg = take(1536), take(1536), take(1536), take(128), take(128), take(480)
        s_cq, s_ka, s_va, s_kidx, s_widx, s_pin = take(512), take(1536), take(1536), take(128), take(8), take(1024)
        mu = np.asarray(rw_mu[l], np.float32)
        nc = _get("rwkv", lambda: build_rwkv(T_, same=True))
        ims = []
        for c in range(8):
            b, hq = c // 4, c % 4
            ch = slice(hq * 384, (hq + 1) * 384)
            P = proj[b]
            sub = lambda s: P[:, s][:, ch]
            ims.append(rwkv_inmap(sub(s_r), sub(s_k), sub(s_v), P[:, s_dw], P[:, s_da], P[:, s_dg],
                                  mu[s_r][ch], mu[s_k][ch], mu[s_v][ch], mu[s_dw], mu[s_da], mu[s_dg],
                                  np.asarray(rw_w2[l])[:, ch], np.asarray(rw_w0[l])[ch], np.asarray(rw_a2[l])[:, ch], np.asarray(rw_a0[l])[ch],
                                  np.asarray(rw_g2[l])[:, ch], np.asarray(rw_kk[l])[ch], np.asarray(rw_ka[l])[ch], np.asarray(rw_rk[l]).reshape(-1)[ch],
                                  np.asarray(rw_gn_g[l])[ch], np.asarray(rw_gn_b[l])[ch]))
            ims[-1] = {k: A(v) for k, v in ims[-1].items()}
        res = _run("rwkv", nc, ims)
        mix = np.empty((B_, T_, D_), np.float32)
        for c in range(8):
            b, hq = c // 4, c % 4
            mix[b, :, hq * 384:(hq + 1) * 384] = res[c]["yT"].T
        del res, ims
        nc = _get("dsa", lambda: build_dsa(8, 12))
        ims = []
        for c in range(8):
            b, r = c // 4, c % 4
            P = proj[b]
            im = dsa_inmap(r, 8, P[:, s_cq], P[:, s_ka], P[:, s_va], P[:, s_kidx], P[:, s_widx], np.asarray(at_qnorm_g[l], np.float32),
                           np.asarray(at_w_uq[l], np.float32), np.asarray(at_w_iq[l], np.float32), np.asarray(at_kidx_g[l], np.float32), np.asarray(at_kidx_b[l], np.float32))
            ims.append({k: A(v) for k, v in im.items()})
        res = _run("dsa", nc, ims)
        for c in range(8):
            b, r = c // 4, c % 4
            mix[b, core_qpos(r, 8), 1536:3072] = res[c]["y"]
        del res, ims
        nc = _get("pool", lambda: build_pool(NT_))
        ims = []
        for c in range(8):
            b, r = c // 4, c % 4
            im = pool_inmap(proj[b][:, s_pin], r * NT_, NT_, np.asarray(pool_w[l], np.float32), np.asarray(pool_scale[l], np.float32))
            ims.append({k: A(v) for k, v in im.items()})
        res = _run("pool", nc, ims)
        for c in range(8):
            b, r = c // 4, c % 4
            mix[b, r * NT_:(r + 1) * NT_, 3072:4096] = res[c]["yT"].T
        del res, ims, proj
        mix = mix.reshape(B_ * T_, D_)
        nc = _get("s3", lambda: build_s3(NT_, D_, CAP_))
        wo = A(w_out[l]); g1 = bc128(np.asarray(ln1_g[l])); b1 = bc128(np.asarray(ln1_b[l]))
        wr = A(np.concatenate([np.asarray(router_g_w[l]), np.asarray(router_e_w[l])], axis=1))
        rbias = bc128(np.concatenate([np.asarray(router_g_b[l]), np.asarray(router_e_b[l])]))
        ims = [dict(mixT=A(mix[c * NT_:(c + 1) * NT_].T), hrow=A(h[c * NT_:(c + 1) * NT_]), w_out=wo, lng=g1, lnb=b1, wr=wr, rbias=rbias, **cs) for c in range(8)]
        r3 = _run("s3", nc, ims)
        del ims, mix, wo
        nc = _get("E", lambda: build_E(8 * CAP_, D_, 512))
        ims = [dict(X=A(np.concatenate([r3[s]["xg"][g * CAP_:(g + 1) * CAP_] for s in range(8)])),
                    G=A(np.concatenate([r3[s]["gsc"][g * CAP_:(g + 1) * CAP_] for s in range(8)])),
                    w1=A(exp_w1[l][8 * g:8 * g + 8]), w3=A(exp_w3[l][8 * g:8 * g + 8]), w2=A(exp_w2[l][8 * g:8 * g + 8]), cI=cs["cI"]) for g in range(8)]
        rE = _run("E", nc, ims)
        del ims
        nc = _get("C", lambda: build_C(NT_, D_, CAP_))
        g2 = bc128(np.asarray(ln2_g[l])); b2 = bc128(np.asarray(ln2_b[l]))
        ims = [dict(Yg=A(np.concatenate([rE[g]["Y"][s * CAP_:(s + 1) * CAP_] for g in range(8)])), idx=np.ascontiguousarray(r3[s]["idx"]),
                    h1=A(r3[s]["h1"]), lng=g2, lnb=b2) for s in range(8)]
        rC = _run("C", nc, ims)
        h = np.concatenate([rC[c]["h2"] for c in range(8)])
        del ims, rE, r3, rC
    return np.ascontiguousarray(h.reshape(B_, T_, D_)[:, NMETA_:]).astype(np.float32)
```

```python
import time, math
import contextlib
import numpy as np
import concourse.bass as bass
import concourse.mybir as mybir
from concourse.bass_utils import run_bass_kernel_spmd

F32 = mybir.dt.float32
BF16 = mybir.dt.bfloat16
I32 = mybir.dt.int32
U32 = mybir.dt.uint32
ALU = mybir.AluOpType
AF = mybir.ActivationFunctionType
AX = mybir.AxisListType

SEM_ROLL = 20000
import threading


class Coop(threading.Thread):
    def __init__(self, fn):
        super().__init__(daemon=True)
        self.fn = fn
        self.tickets = threading.Semaphore(0)
        self.consumed = threading.Semaphore(0)
        self.finished = False
        self.err = None
        self.start()

    def run(self):
        try:
            self.fn()
        except BaseException as e:
            self.err = e
        self.finished = True
        self.consumed.release()

    def advance(self, k=1):
        for _ in range(k):
            if self.finished:
                break
            self.tickets.release()
            self.consumed.acquire()
        if self.err is not None:
            raise self.err

    def finish(self):
        while not self.finished:
            self.advance(1)
        if self.err is not None:
            raise self.err


class Sched:
    def __init__(self, nc, es, ndsem=6, same_eng_sync=True):
        self.nc = nc
        self.es = es
        self.eng = dict(pe=nc.tensor, dve=nc.vector, act=nc.scalar, pool=nc.gpsimd, sp=nc.sync)
        self.same = same_eng_sync
        self.nsem = 0
        self.csem = {}
        self.ccnt = {}
        self.cep = {}
        for k in ("pe", "dve", "act", "pool"):
            self._new_csem(k, 0)
        self.dsem = {}
        self.dcnt = {}
        self.drr = {}
        for q in ("sp", "act", "pool"):
            self.dsem[q] = [self._sem(f"d_{q}_{i}") for i in range(ndsem)]
            self.dcnt[q] = [0] * ndsem
            self.drr[q] = 0
        self.seen = {k: {} for k in self.eng}
        self.lastw = {}
        self.readers = {}
        self.ninst = 0

    def _sem(self, name):
        self.nsem += 1
        return self.es.enter_context(self.nc.semaphore(name))

    def _new_csem(self, k, ep):
        self.cep[k] = ep
        self.csem[k] = self._sem(f"c_{k}_{ep}")
        self.ccnt[k] = 0

    def _wait(self, e, tok):
        sem, val, key, src = tok
        if src == e and src in ("pe",):
            return
        if src == e and not self.same and src in ("dve", "act", "pool") and key[0] == "c":
            return
        if self.seen[e].get(key, 0) >= val:
            return
        self.eng[e].wait_ge(sem, val)
        self.seen[e][key] = val
        self.ninst += 1

    def _deps(self, e, r, w):
        for k in r:
            t = self.lastw.get(k)
            if t is not None:
                self._wait(e, t)
        for k in w:
            t = self.lastw.get(k)
            if t is not None:
                self._wait(e, t)
            for t in self.readers.get(k, ()):
                self._wait(e, t)

    def _record(self, tok, r, w):
        for k in r:
            self.readers.setdefault(k, []).append(tok)
        for k in w:
            self.lastw[k] = tok
            self.readers[k] = []

    def op(self, e, fn, r=(), w=()):
        cw = threading.current_thread()
        if isinstance(cw, Coop):
            cw.tickets.acquire()
            try:
                return self._op(e, fn, r, w)
            finally:
                cw.consumed.release()
        return self._op(e, fn, r, w)

    def _op(self, e, fn, r=(), w=()):
        self._deps(e, r, w)
        inst = fn(self.eng[e])
        if self.ccnt[e] >= SEM_ROLL:
            self._new_csem(e, self.cep[e] + 1)
        self.ccnt[e] += 1
        inst.then_inc(self.csem[e], 1)
        tok = (self.csem[e], self.ccnt[e], ("c", e, self.cep[e]), e)
        self._record(tok, r, w)
        self.ninst += 1
        return tok

    def dma(self, q, out, in_, r=(), w=(), fn=None, **kw):
        cw = threading.current_thread()
        if isinstance(cw, Coop):
            cw.tickets.acquire()
            try:
                return self._dma(q, out, in_, r, w, fn, **kw)
            finally:
                cw.consumed.release()
        return self._dma(q, out, in_, r, w, fn, **kw)

    def _dma(self, q, out, in_, r=(), w=(), fn=None, **kw):
        i = self.drr[q]
        self.drr[q] = (i + 1) % len(self.dsem[q])
        sem = self.dsem[q][i]
        key = ("d", q, i)
        if self.dcnt[q][i] > 0:
            self._wait(q, (sem, 16 * self.dcnt[q][i], key, "dma"))
        self._deps(q, r, w)
        if fn is None:
            inst = self.eng[q].dma_start(out=out, in_=in_, **kw)
        else:
            inst = fn(self.eng[q])
        self.dcnt[q][i] += 1
        inst.then_inc(sem, 16)
        tok = (sem, 16 * self.dcnt[q][i], key, "dma")
        self._record(tok, r, w)
        self.ninst += 1
        return tok

    def barrier(self):
        toks = []
        for k in ("pe", "dve", "act", "pool"):
            if self.ccnt[k] > 0:
                toks.append((self.csem[k], self.ccnt[k], ("c", k, self.cep[k]), "bar"))
        for q in self.dsem:
            for i, sem in enumerate(self.dsem[q]):
                if self.dcnt[q][i] > 0:
                    toks.append((sem, 16 * self.dcnt[q][i], ("d", q, i), "bar"))
        for e in ("pe", "dve", "act", "pool", "sp"):
            for t in toks:
                self._wait(e, t)

    def finish(self, toks, e="sp"):
        for t in toks:
            self._wait(e, t)


def new_nc():
    return bass.Bass("TRN2", target_bir_lowering=False)


def run(nc, in_maps):
    res = run_bass_kernel_spmd(nc, in_maps, core_ids=list(range(len(in_maps))))
    return res.results


def mm_phase(nc, S, es, xT, w, dst, ntok, K, N, ps, cb=256, dst_key=None, xb=None, kx=None):
    KC = K // 128
    sbt = lambda n, s, d: es.enter_context(nc.sbuf_tensor(n, s, d))
    if xb is None:
        xb = sbt("mm_xb", [128, KC, ntok], BF16)
    kxc = KC if kx is None else kx // 128
    xs = sbt("mm_xs", [128, KC // 4, ntok], F32)
    ws = sbt("mm_ws", [128, KC, cb], F32)
    wb = [sbt(f"mm_wb{i}", [128, KC, cb], BF16) for i in range(2)]
    ot = [sbt(f"mm_ot{i}", [128, cb], F32) for i in range(4)]
    xv = xT.rearrange("(kc p) t -> p kc t", p=128)
    wv = w.rearrange("(kc p) n -> p kc n", p=128)
    q = KC // 4
    for i in range(4):
        if i * q >= kxc:
            continue
        S.dma("sp" if i % 2 == 0 else "act", xs[:, :, :], xv[:, i * q:(i + 1) * q, :], w=["mm_xs"])
        h2 = q // 2
        if h2 > 0:
            S.op("dve", lambda e: e.tensor_copy(out=xb[:, i * q:i * q + h2, :], in_=xs[:, :h2, :]), r=["mm_xs"], w=[("mm_xb", i)])
        S.op("act", lambda e: e.copy(out=xb[:, i * q + h2:(i + 1) * q, :], in_=xs[:, h2:, :]), r=["mm_xs"], w=[("mm_xb", i)])
    toks = [(t0, min(128, ntok - t0)) for t0 in range(0, ntok, 128)]
    ncb = (N + cb - 1) // cb
    it = 0
    outs = []
    h = KC // 2
    for c in range(ncb):
        c0 = c * cb
        cn = min(cb, N - c0)
        wbb = wb[c % 2]
        S.dma("sp", ws[:, :h, :cn], wv[:, :h, c0:c0 + cn], w=[("mm_ws", 0)])
        S.dma("act", ws[:, h:, :cn], wv[:, h:, c0:c0 + cn], w=[("mm_ws", 1)])
        S.op("dve", lambda e: e.tensor_copy(out=wbb[:, :h, :cn], in_=ws[:, :h, :cn]), r=[("mm_ws", 0)], w=[("mm_wb", c % 2, 0)])
        S.op("act", lambda e: e.copy(out=wbb[:, h:, :cn], in_=ws[:, h:, :cn]), r=[("mm_ws", 1)], w=[("mm_wb", c % 2, 1)])
        for (t0, m) in toks:
            p = ps[it % 4]
            o = ot[it % 4]
            for kc in range(KC):
                S.op("pe", lambda e: e.matmul(p[:m, :cn], lhsT=xb[:, kc, t0:t0 + m], rhs=wbb[:, kc, :cn], start=(kc == 0), stop=(kc == KC - 1)),
                     r=[("mm_xb", kc // q), ("mm_wb", c % 2, 0 if kc < h else 1)], w=[("mm_ps", it % 4)])
            if it % 2 == 0:
                S.op("dve", lambda e: e.tensor_copy(out=o[:m, :cn], in_=p[:m, :cn]), r=[("mm_ps", it % 4)], w=[("mm_ot", it % 4)])
            else:
                S.op("act", lambda e: e.copy(out=o[:m, :cn], in_=p[:m, :cn]), r=[("mm_ps", it % 4)], w=[("mm_ot", it % 4)])
            outs.append(S.dma("pool", dst[t0:t0 + m, c0:c0 + cn], o[:m, :cn], r=[("mm_ot", it % 4)], w=([(dst_key, t0)] if dst_key else [])))
            it += 1
    return outs


def build_mm(ntok, K, N, cb=256, name="mm"):
    nc = new_nc()
    xT = nc.dram_tensor("xT", [K, ntok], F32, kind="ExternalInput").ap()
    w = nc.dram_tensor("w", [K, N], F32, kind="ExternalInput").ap()
    out = nc.dram_tensor("out", [ntok, N], F32, kind="ExternalOutput").ap()
    es = contextlib.ExitStack()
    with es:
        S = Sched(nc, es)
        ps = [es.enter_context(nc.psum_tensor(f"ps{i}", [128, 512], F32)) for i in range(4)]
        outs = mm_phase(nc, S, es, xT, w, out, ntok, K, N, ps, cb)
        S.finish(outs, "pool")
        print("ninst", S.ninst, "nsem", S.nsem)
    return nc


import math

GN_EPS = 64e-5
DEBUG = False
NV = 10
C_MUR, C_MUK, C_MUV, C_W0, C_A0, C_KK, C_KA, C_RK, C_GG, C_GB = range(10)

def build_rwkv(T, TC=256, TCB=32, same=False):
    nc = new_nc()
    G = 3
    di = lambda n, s: nc.dram_tensor(n, s, F32, kind="ExternalInput").ap()
    prT = di("prT", [384, T + 1]); pkT = di("pkT", [384, T + 1]); pvT = di("pvT", [384, T + 1])
    pdwT = di("pdwT", [128, T + 1]); pdaT = di("pdaT", [128, T + 1]); pdgT = di("pdgT", [480, T + 1])
    pv_tm = di("pv_tm", [T + 1, 384])
    vec = di("vec", [384, NV]); vlo = di("vlo", [128, 2]); vdg = di("vdg", [480, 1]); muvb = di("muvb", [128, 384])
    w2s = di("w2s", [128, 384]); a2s = di("a2s", [128, 384]); g2s = di("g2s", [480, 384])
    cM = di("cM", [128, 128]); cSel = di("cSel", [64, 2, 128])
    yT = nc.dram_tensor("yT", [384, T], F32, kind="ExternalOutput").ap()
    vscr = nc.dram_tensor("vscr", [T, 384], F32, kind="Internal").ap()
    dbg = nc.dram_tensor("dbg", [12, 128, 3, TC], F32, kind="ExternalOutput").ap() if DEBUG else None
    es = contextlib.ExitStack()
    with es:
        S = Sched(nc, es, same_eng_sync=same)
        sb = lambda n, s, d=F32: es.enter_context(nc.sbuf_tensor(n, s, d))
        pt = lambda n, s: es.enter_context(nc.psum_tensor(n, s, F32))
        vec_t = sb("vec_t", [128, G, NV]); vlo_t = sb("vlo_t", [128, 2]); vdg_t = sb("vdg_t", [128, 4, 1]); muvb_t = sb("muvb_t", [128, 384])
        w2_t = sb("w2_t", [128, 384]); a2_t = sb("a2_t", [128, 384]); g2_t = sb("g2_t", [128, 4, 384])
        M_t = sb("M_t", [128, 128]); Mavg_t = sb("Mavg_t", [128, 128]); Sel_t = sb("Sel_t", [64, 2, 128]); Mrk_t = sb("Mrk_t", [128, G, 128])
        S.dma("sp", vec_t[:], vec.rearrange("(g p) n -> p g n", p=128), w=["vec"])
        S.dma("sp", vlo_t[:], vlo, w=["vlo"])
        for kc in range(4):
            n = 128 if kc < 3 else 96
            S.dma("sp", vdg_t[:n, kc, :], vdg[kc * 128:kc * 128 + n, :], w=["vdg"])
            S.dma("act", g2_t[:n, kc, :], g2s[kc * 128:kc * 128 + n, :], w=["g2"])
        S.dma("sp", muvb_t[:], muvb, w=["muvb"])
        S.dma("act", w2_t[:], w2s, w=["w2"]); S.dma("act", a2_t[:], a2s, w=["a2"])
        S.dma("sp", M_t[:], cM, w=["M"]); S.dma("sp", Sel_t[:], cSel, w=["Sel"])
        S.op("dve", lambda e: e.tensor_scalar(out=Mavg_t[:], in0=M_t[:], scalar1=1.0 / 64, scalar2=None, op0=ALU.mult), r=["M"], w=["Mavg"])
        for g in range(G):
            S.op("dve", lambda e: e.tensor_scalar(out=Mrk_t[:, g, :], in0=M_t[:], scalar1=vec_t[:, g, C_RK:C_RK + 1], scalar2=None, op0=ALU.mult), r=["M", "vec"], w=["Mrk"])
        W1 = TC + 1
        ld_r = sb("ld_r", [128, G, W1]); ld_k = sb("ld_k", [128, G, W1]); ld_v = sb("ld_v", [128, G, W1])
        ld_dw = sb("ld_dw", [128, W1]); ld_da = sb("ld_da", [128, W1]); ld_dg = sb("ld_dg", [128, 4, W1])
        tmA = sb("tmA", [128, 384]); tmB = sb("tmB", [128, 384]); tmD = sb("tmD", [128, 384])
        rs = sb("rs", [128, G, TC]); ks = sb("ks", [128, G, TC]); vs = sb("vs", [128, G, TC])
        dws = sb("dws", [128, TC]); das = sb("das", [128, TC]); dgs = sb("dgs", [128, 4, TC])
        aa = sb("aa", [128, G, TC])
        dec2 = [sb(f"dec{q}", [128, G, TC]) for q in range(2)]; gg2 = [sb(f"gg{q}", [128, G, TC]) for q in range(2)]
        nkk2 = [sb(f"nkk{q}", [128, G, TC]) for q in range(2)]; bb2 = [sb(f"bb{q}", [128, G, TC]) for q in range(2)]
        kmod2 = [sb(f"kmod{q}", [128, G, TC]) for q in range(2)]
        Rm2 = [sb(f"Rm{q}", [128, G, TC, 2]) for q in range(2)]; bonv2 = [sb(f"bonv{q}", [128, G, TC]) for q in range(2)]
        t1 = sb("t1", [128, TC]); t2 = sb("t2", [128, TC]); t3 = sb("t3", [128, TC])
        u1 = sb("u1", [128, TC]); u2 = sb("u2", [128, TC])
        ysb = sb("ysb", [64, G, TC * 2]); yfm = sb("yfm", [128, TC]); yo = sb("yo", [128, G, TC])
        SD = [[sb(f"SD{q}_{g}", [128, 64]) for g in range(G)] for q in range(2)]
        S1 = [sb(f"S1_{g}", [128, 64]) for g in range(G)]
        S2 = [sb(f"S2_{g}", [128, 64]) for g in range(G)]
        Zall = sb("Zall", [128, G * 64], mybir.dt.float32r)
        M_r = sb("M_r", [128, 128], mybir.dt.float32r)
        NL = 4
        Lr = [[sb(f"L{g}_{i}", [128, 128]) for i in range(NL)] for g in range(G)]
        vb = [[sb(f"vb{g}_{i}", [128, TCB, 64]) for i in range(2)] for g in range(G)]
        ps1 = [pt(f"ps1_{g}", [128, 512]) for g in range(G)]
        psy = [pt(f"psy_{g}", [128, 512]) for g in range(G)]
        psA = pt("psA", [128, 512]); psB = pt("psB", [128, 512])
        for g in range(G):
            for q in range(2):
                S.op("pool", lambda e: e.memset(SD[q][g][:], 0.0), w=[("SD", q, g)])
        S.op("dve", lambda e: e.tensor_copy(out=M_r[:], in_=M_t[:]), r=["M"], w=["M_r"])
        out_toks = []
        nvb = 0
        lcnt = [0] * G
        def pre(c0, par):
            n = min(TC, T - c0)
            dec, gg, nkk, bb, kmod, Rm, bonv = dec2[par], gg2[par], nkk2[par], bb2[par], kmod2[par], Rm2[par], bonv2[par]
            for r0 in range(c0, c0 + n, 128):
                m = min(128, c0 + n - r0)
                S.dma("sp", tmA[:m, :], pv_tm[r0 + 1:r0 + 1 + m, :], w=["tmA"])
                S.dma("act", tmB[:m, :], pv_tm[r0:r0 + m, :], w=["tmB"])
                S.op("dve", lambda e: e.tensor_tensor(out=tmD[:m, :], in0=tmB[:m, :], in1=tmA[:m, :], op=ALU.subtract), r=["tmA", "tmB"], w=["tmD"])
                S.op("dve", lambda e: e.tensor_tensor(out=tmD[:m, :], in0=tmD[:m, :], in1=muvb_t[:m, :], op=ALU.mult), r=["tmD", "muvb"], w=["tmD"])
                S.op("dve", lambda e: e.tensor_tensor(out=tmD[:m, :], in0=tmD[:m, :], in1=tmA[:m, :], op=ALU.add), r=["tmD", "tmA"], w=["tmD"])
                S.dma("sp", vscr[r0:r0 + m, :], tmD[:m, :], r=["tmD"], w=[("vscr", r0 // TCB + i) for i in range((m + TCB - 1) // TCB)])
            S.dma("sp", ld_r[:, :, :n + 1], prT[:, c0:c0 + n + 1].rearrange("(g p) t -> p g t", p=128), w=["ld_r"])
            S.dma("act", ld_k[:, :, :n + 1], pkT[:, c0:c0 + n + 1].rearrange("(g p) t -> p g t", p=128), w=["ld_k"])
            S.dma("sp", ld_v[:, :, :n + 1], pvT[:, c0:c0 + n + 1].rearrange("(g p) t -> p g t", p=128), w=["ld_v"])
            S.dma("act", ld_dw[:, :n + 1], pdwT[:, c0:c0 + n + 1], w=["ld_dw"])
            S.dma("sp", ld_da[:, :n + 1], pdaT[:, c0:c0 + n + 1], w=["ld_da"])
            for kc in range(4):
                kn = 128 if kc < 3 else 96
                S.dma("act", ld_dg[:kn, kc, :n + 1], pdgT[kc * 128:kc * 128 + kn, c0:c0 + n + 1], w=[("ld_dg", kc)])

            def shift(dst, src, mu, P, rk, wk):
                S.op("dve", lambda e: e.tensor_tensor(out=t1[:P, :n], in0=src[:P, 0:n], in1=src[:P, 1:n + 1], op=ALU.subtract), r=[rk], w=["t1"])
                S.op("dve", lambda e: e.scalar_tensor_tensor(out=dst[:P, :n], in0=t1[:P, :n], scalar=mu, in1=src[:P, 1:n + 1], op0=ALU.mult, op1=ALU.add), r=["t1", rk, "vec", "vlo", "vdg"], w=[wk])
            for g in range(G):
                shift(rs[:, g, :], ld_r[:, g, :], vec_t[:, g, C_MUR:C_MUR + 1], 128, "ld_r", ("rs", g))
                shift(ks[:, g, :], ld_k[:, g, :], vec_t[:, g, C_MUK:C_MUK + 1], 128, "ld_k", ("ks", g))
                shift(vs[:, g, :], ld_v[:, g, :], vec_t[:, g, C_MUV:C_MUV + 1], 128, "ld_v", ("vs", g))
            shift(dws, ld_dw, vlo_t[:, 0:1], 128, "ld_dw", "dws")
            shift(das, ld_da, vlo_t[:, 1:2], 128, "ld_da", "das")
            for kc in range(4):
                kn = 128 if kc < 3 else 96
                shift(dgs[:, kc, :], ld_dg[:, kc, :], vdg_t[:kn, kc, :], kn, ("ld_dg", kc), ("dgs", kc))
            S.op("act", lambda e: e.activation(out=dws[:, :n], in_=dws[:, :n], func=AF.Tanh), r=["dws"], w=["dws"])
            for kc in range(4):
                kn = 128 if kc < 3 else 96
                S.op("act", lambda e: e.activation(out=dgs[:kn, kc, :n], in_=dgs[:kn, kc, :n], func=AF.Sigmoid), r=[("dgs", kc)], w=[("dgs", kc)])
            for g in range(G):
                gs = slice(g * 128, (g + 1) * 128)
                S.op("pe", lambda e: e.matmul(psA[:, :n], lhsT=w2_t[:, gs], rhs=dws[:, :n], start=True, stop=True), r=["w2", "dws"], w=["psA"])
                S.op("act", lambda e: e.activation(out=t2[:, :n], in_=psA[:, :n], func=AF.Sigmoid, bias=vec_t[:, g, C_W0:C_W0 + 1]), r=["psA", "vec"], w=["t2"])
                S.op("act", lambda e: e.activation(out=dec[:, g, :n], in_=t2[:, :n], func=AF.Exp, scale=-math.exp(-0.5)), r=["t2"], w=[("dec", par, g)])
                S.op("pe", lambda e: e.matmul(psB[:, :n], lhsT=a2_t[:, gs], rhs=das[:, :n], start=True, stop=True), r=["a2", "das"], w=["psB"])
                S.op("act", lambda e: e.activation(out=aa[:, g, :n], in_=psB[:, :n], func=AF.Sigmoid, bias=vec_t[:, g, C_A0:C_A0 + 1]), r=["psB", "vec"], w=[("aa", g)])
                for kc in range(4):
                    kn = 128 if kc < 3 else 96
                    S.op("pe", lambda e: e.matmul(psA[:, :n], lhsT=g2_t[:kn, kc, gs], rhs=dgs[:kn, kc, :n], start=(kc == 0), stop=(kc == 3)), r=["g2", ("dgs", kc)], w=["psA"])
                S.op("act", lambda e: e.copy(out=gg[:, g, :n], in_=psA[:, :n]), r=["psA"], w=[("gg", par, g)])
                S.op("dve", lambda e: e.tensor_scalar(out=t1[:, :n], in0=ks[:, g, :n], scalar1=vec_t[:, g, C_KK:C_KK + 1], scalar2=None, op0=ALU.mult), r=[("ks", g), "vec"], w=["t1"])
                S.op("dve", lambda e: e.tensor_tensor(out=t3[:, :n], in0=t1[:, :n], in1=t1[:, :n], op=ALU.mult), r=["t1"], w=["t3"])
                S.op("pe", lambda e: e.matmul(psB[:, :n], lhsT=M_t[:], rhs=t3[:, :n], start=True, stop=True), r=["M", "t3"], w=["psB"])
                S.op("act", lambda e: e.activation(out=t3[:, :n], in_=psB[:, :n], func=AF.Sqrt), r=["psB"], w=["t3"])
                S.op("dve", lambda e: e.tensor_scalar(out=t3[:, :n], in0=t3[:, :n], scalar1=1e-12, scalar2=None, op0=ALU.max), r=["t3"], w=["t3"])
                S.op("dve", lambda e: e.reciprocal(out=t3[:, :n], in_=t3[:, :n]), r=["t3"], w=["t3"])
                S.op("dve", lambda e: e.scalar_tensor_tensor(out=nkk[:, g, :n], in0=t1[:, :n], scalar=-1.0, in1=t3[:, :n], op0=ALU.mult, op1=ALU.mult), r=["t1", "t3"], w=[("nkk", par, g)])
                S.op("dve", lambda e: e.scalar_tensor_tensor(out=bb[:, g, :n], in0=nkk[:, g, :n], scalar=-1.0, in1=aa[:, g, :n], op0=ALU.mult, op1=ALU.mult), r=[("nkk", par, g), ("aa", g)], w=[("bb", par, g)])
                S.op("dve", lambda e: e.tensor_scalar(out=t1[:, :n], in0=aa[:, g, :n], scalar1=-1.0, scalar2=vec_t[:, g, C_KA:C_KA + 1], op0=ALU.add, op1=ALU.mult), r=[("aa", g), "vec"], w=["t1"])
                S.op("dve", lambda e: e.scalar_tensor_tensor(out=kmod[:, g, :n], in0=t1[:, :n], scalar=1.0, in1=ks[:, g, :n], op0=ALU.add, op1=ALU.mult), r=["t1", ("ks", g)], w=[("kmod", par, g)])
                S.op("dve", lambda e: e.tensor_tensor(out=t1[:, :n], in0=rs[:, g, :n], in1=kmod[:, g, :n], op=ALU.mult), r=[("rs", g), ("kmod", par, g)], w=["t1"])
                S.op("pe", lambda e: e.matmul(psB[:, :n], lhsT=Mrk_t[:, g, :], rhs=t1[:, :n], start=True, stop=True), r=["Mrk", "t1"], w=["psB"])
                S.op("dve", lambda e: e.tensor_tensor(out=bonv[:, g, :n], in0=psB[:, :n], in1=vs[:, g, :n], op=ALU.mult), r=["psB", ("vs", g)], w=[("bonv", par, g)])
                S.op("pool", lambda e: e.memset(Rm[:, g, :n, :], 0.0), w=[("Rm", par, g)])
                S.op("pool", lambda e: e.tensor_copy(out=Rm[0:64, g, :n, 0], in_=rs[0:64, g, :n]), r=[("rs", g)], w=[("Rm", par, g)])
                S.op("pool", lambda e: e.tensor_copy(out=Rm[64:128, g, :n, 1], in_=rs[64:128, g, :n]), r=[("rs", g)], w=[("Rm", par, g)])
        def load_block(kb):
            t0b = kb * TCB
            if t0b >= T:
                return
            nb = min(TCB, T - t0b)
            ci_ = t0b // TC; parb = ci_ % 2; s0 = t0b - ci_ * TC; vbi_ = kb % 2
            for g in range(G):
                for h in range(2):
                    src = vscr[t0b:t0b + nb, g * 128 + h * 64:g * 128 + (h + 1) * 64]
                    S.dma("sp" if h == 0 else "act", vb[g][vbi_][h * 64:(h + 1) * 64, :nb, :], src.partition_broadcast(64),
                          r=[("vscr", kb)], w=[("vb", g, vbi_)])
                S.op("dve", lambda e: e.tensor_tensor(out=vb[g][vbi_][:, :nb, :], in0=vb[g][vbi_][:, :nb, :],
                                                      in1=kmod2[parb][:, g, s0:s0 + nb].unsqueeze(2).to_broadcast([128, nb, 64]), op=ALU.mult),
                     r=[("vb", g, vbi_), ("kmod", parb, g)], w=[("vb", g, vbi_)])

        def scan(c0, par, hook):
            nonlocal nvb
            n = min(TC, T - c0)
            dec, nkk, bb, kmod, Rm = dec2[par], nkk2[par], bb2[par], kmod2[par], Rm2[par]
            for s in range(n):
                t = c0 + s
                hook(s)
                if t % TCB == 0:
                    kb = t // TCB
                    if kb == 0:
                        load_block(0)
                    load_block(kb + 1)
                    vbi = kb % 2
                sl = t % TCB
                pr_, pw_ = (t + 1) % 2, t % 2
                for g in range(G):
                    S.op("act", lambda e: e.activation(out=Zall[:, g * 64:(g + 1) * 64], in_=SD[pr_][g][:], func=AF.Copy, scale=nkk[:, g, s:s + 1]), r=[("SD", pr_, g), ("nkk", par, g)], w=[("Z", g)])
                for g in range(G):
                    S.op("dve", lambda e: e.scalar_tensor_tensor(out=S1[g][:], in0=SD[pr_][g][:], scalar=dec[:, g, s:s + 1], in1=vb[g][vbi][:, sl, :], op0=ALU.mult, op1=ALU.add),
                         r=[("SD", pr_, g), ("dec", par, g), ("vb", g, vbi)], w=[("S1", g)])
                for g in range(G):
                    S.op("pe", lambda e: e.matmul(ps1[g][:, 0:64], lhsT=M_r[:], rhs=Zall[:, g * 64:(g + 1) * 64], start=True, stop=True), r=["M_r", ("Z", g)], w=[("ps1", g)])
                if s > 0:
                    for g in range(G):
                        S.op("pe", lambda e: e.matmul(psy[g][0:64, 2 * (s - 1):2 * (s - 1) + 2], lhsT=SD[pr_][g][:], rhs=Rm[:, g, s - 1, :], start=True, stop=True), r=[("SD", pr_, g), ("Rm", par, g)], w=[("psy", g)])
                for g in range(G):
                    S.op("dve", lambda e: e.scalar_tensor_tensor(out=SD[pw_][g][:], in0=ps1[g][:, 0:64], scalar=bb[:, g, s:s + 1], in1=S1[g][:], op0=ALU.mult, op1=ALU.add),
                         r=[("ps1", g), ("bb", par, g), ("S1", g)], w=[("SD", pw_, g)])
            lastp = (c0 + n - 1) % 2
            for g in range(G):
                S.op("pe", lambda e: e.matmul(psy[g][0:64, 2 * (n - 1):2 * (n - 1) + 2], lhsT=SD[lastp][g][:], rhs=Rm[:, g, n - 1, :], start=True, stop=True), r=[("SD", lastp, g), ("Rm", par, g)], w=[("psy", g)])
        def post(c0, par):
            n = min(TC, T - c0)
            gg, bonv = gg2[par], bonv2[par]
            for g in range(G):
                S.op("act", lambda e: e.copy(out=ysb[:, g, :2 * n], in_=psy[g][0:64, :2 * n]), r=[("psy", g)], w=[("ysb", g)])
            for g in range(G):
                yv = ysb[:, g, :2 * n].rearrange("p (t h) -> p t h", h=2)
                for h in range(2):
                    S.op("pe", lambda e: e.matmul(psA[:, :n], lhsT=Sel_t[:, h, :], rhs=yv[:, :, h], start=(h == 0), stop=(h == 1)), r=["Sel", ("ysb", g)], w=["psA"])
                S.op("act", lambda e: e.copy(out=yfm[:, :n], in_=psA[:, :n]), r=["psA"], w=["yfm"])
                S.op("pe", lambda e: e.matmul(psB[:, :n], lhsT=Mavg_t[:], rhs=yfm[:, :n], start=True, stop=True), r=["Mavg", "yfm"], w=["psB"])
                S.op("dve", lambda e: e.tensor_tensor(out=u1[:, :n], in0=yfm[:, :n], in1=psB[:, :n], op=ALU.subtract), r=["yfm", "psB"], w=["u1"])
                S.op("dve", lambda e: e.tensor_tensor(out=u2[:, :n], in0=u1[:, :n], in1=u1[:, :n], op=ALU.mult), r=["u1"], w=["u2"])
                S.op("pe", lambda e: e.matmul(psB[:, :n], lhsT=Mavg_t[:], rhs=u2[:, :n], start=True, stop=True), r=["Mavg", "u2"], w=["psB"])
                S.op("dve", lambda e: e.tensor_scalar(out=u2[:, :n], in0=psB[:, :n], scalar1=GN_EPS, scalar2=None, op0=ALU.add), r=["psB"], w=["u2"])
                S.op("act", lambda e: e.activation(out=u2[:, :n], in_=u2[:, :n], func=AF.Sqrt), r=["u2"], w=["u2"])
                S.op("dve", lambda e: e.reciprocal(out=u2[:, :n], in_=u2[:, :n]), r=["u2"], w=["u2"])
                S.op("dve", lambda e: e.tensor_tensor(out=u1[:, :n], in0=u1[:, :n], in1=u2[:, :n], op=ALU.mult), r=["u1", "u2"], w=["u1"])
                S.op("dve", lambda e: e.tensor_scalar(out=u1[:, :n], in0=u1[:, :n], scalar1=vec_t[:, g, C_GG:C_GG + 1], scalar2=vec_t[:, g, C_GB:C_GB + 1], op0=ALU.mult, op1=ALU.add), r=["u1", "vec"], w=["u1"])
                S.op("dve", lambda e: e.tensor_tensor(out=u1[:, :n], in0=u1[:, :n], in1=bonv[:, g, :n], op=ALU.add), r=["u1", ("bonv", par, g)], w=["u1"])
                S.op("dve", lambda e: e.tensor_tensor(out=yo[:, g, :n], in0=u1[:, :n], in1=gg[:, g, :n], op=ALU.mult), r=["u1", ("gg", par, g)], w=[("yo", g)])
                out_toks.append(S.dma("pool", yT[g * 128:(g + 1) * 128, c0:c0 + n], yo[:, g, :n], r=[("yo", g)]))
        chunks = list(range(0, T, TC))
        NPRE, NPOST = 400, 60
        pre(chunks[0], 0)
        wpost = None
        for ci, c0 in enumerate(chunks):
            par = ci % 2
            n = min(TC, T - c0)
            wpre = Coop(lambda c1=(chunks[ci + 1] if ci + 1 < len(chunks) else None), p1=(ci + 1) % 2: pre(c1, p1)) if ci + 1 < len(chunks) else None
            if wpost is not None:
                wpost.advance(G)
            kpre = (NPRE + n - 1) // n; kpost = (NPOST + n - 1) // n
            def hook(s, wpre=wpre, wpost=wpost):
                if wpost is not None and not wpost.finished:
                    wpost.advance(1)
                elif wpre is not None:
                    wpre.advance(1)
            scan(c0, par, hook)
            if wpost is not None:
                wpost.finish()
            if wpre is not None:
                wpre.finish()
            wpost = Coop(lambda c1=c0, p1=par: post(c1, p1))
        wpost.finish()
        S.finish(out_toks, "pool")
        print("ninst", S.ninst, "nsem", S.nsem)
    return nc


def consts():
    M = np.zeros((128, 128), np.float32); M[:64, :64] = 1; M[64:, 64:] = 1
    Sel = np.zeros((64, 2, 128), np.float32)
    for i in range(64):
        Sel[i, 0, i] = 1; Sel[i, 1, 64 + i] = 1
    return M, Sel


def rwkv_inmap(pr, pk, pv, pdw, pda, pdg, mu_r, mu_k, mu_v, mu_dw, mu_da, mu_dg, w2, w0, a2, a0, g2, k_k, k_a, r_k, gn_g, gn_b):
    M, Sel = consts()
    z = lambda a: np.ascontiguousarray(np.concatenate([np.zeros((a.shape[1], 1), np.float32), a.T], axis=1))
    vec = np.stack([mu_r, mu_k, mu_v, w0, a0, k_k, k_a, r_k, gn_g, gn_b], axis=1).astype(np.float32)
    return dict(prT=z(pr), pkT=z(pk), pvT=z(pv), pdwT=z(pdw), pdaT=z(pda), pdgT=z(pdg),
                pv_tm=np.ascontiguousarray(np.concatenate([np.zeros((1, pv.shape[1]), np.float32), pv], axis=0)),
                vec=np.ascontiguousarray(vec), vlo=np.ascontiguousarray(np.stack([mu_dw, mu_da], axis=1)),
                vdg=np.ascontiguousarray(mu_dg[:, None]), muvb=np.ascontiguousarray(np.broadcast_to(mu_v[None, :], (128, 384))),
                w2s=np.ascontiguousarray(w2), a2s=np.ascontiguousarray(a2), g2s=np.ascontiguousarray(g2), cM=M, cSel=Sel)


import math
import ml_dtypes

NEG = -1.0e30
LN_EPS = 1e-5

def build_dsa(NS=8, NH=12, topk=256):
    T = 512 * NS + 16
    NQ = 128 * NS + 4
    NKT = (T + 127) // 128
    nc = new_nc()
    di = lambda n, s, d=F32: nc.dram_tensor(n, s, d, kind="ExternalInput").ap()
    cq = di("cq", [NQ, 512]); widx = di("widx", [NQ, 8]); qpos = di("qpos", [NQ, 1])
    kT = di("kT", [NH * 128, T]); v = di("v", [T, NH * 128]); kidx = di("kidx", [T, 128])
    w_uq = di("w_uq", [512, NH * 128]); w_iq = di("w_iq", [512, 1024])
    qg = di("qg", [128, 4]); kig = di("kig", [128, 128]); kib = di("kib", [128, 128])
    cos_fm = di("cos_fm", [128, T]); sin_fm = di("sin_fm", [128, T])
    cosq = di("cosq", [128, NQ]); sinq = di("sinq", [128, NQ])
    kpos = di("kpos", [128, T]); cR = di("cR", [128, 128]); cI = di("cI", [128, 128])
    y = nc.dram_tensor("y", [NQ, NH * 128], F32, kind="ExternalOutput").ap()
    slots = [(j * 128, 128, 512 * (j + 1)) for j in range(NS)] + [(128 * NS, 4, T)]
    es = contextlib.ExitStack()
    with es:
        S = Sched(nc, es)
        sb = lambda n, s, d=F32, st=es: st.enter_context(nc.sbuf_tensor(n, s, d))
        pt = lambda n, s, d=F32: es.enter_context(nc.psum_tensor(n, s, d))
        def op(eng, fn, r, w): return S.op(eng, fn, r=r, w=w)
        maskT = sb("maskT", [128, NKT, NQ], BF16)
        cqnT = sb("cqnT", [128, 4, NQ]); cosq_t = sb("cosq_t", [128, NQ]); sinq_t = sb("sinq_t", [128, NQ])
        ctab = [sb(f"ctab{i}", [128, 512]) for i in range(2)]; stab = [sb(f"stab{i}", [128, 512]) for i in range(2)]
        xT = sb("xT", [128, 512])
        R_t = sb("R_t", [128, 128]); I_t = sb("I_t", [128, 128]); Ib_t = sb("Ib_t", [128, 128], BF16)
        tA = sb("tA", [128, 512]); tB = sb("tB", [128, 512])
        psR = [pt(f"psR{i}", [128, 512]) for i in range(2)]
        psM = [pt(f"psM{i}", [128, 512]) for i in range(2)]
        psO = [pt(f"psO{i}", [128, 512]) for i in range(2)]
        psT = [pt(f"psT{i}", [128, 512], BF16) for i in range(2)]
        S.dma("sp", R_t[:], cR, w=["R"]); S.dma("sp", I_t[:], cI, w=["I"])
        S.dma("sp", cosq_t[:], cosq, w=["cosq"]); S.dma("act", sinq_t[:], sinq, w=["sinq"])
        tcnt = [0]
        def rope_k(dst, src, c, n, rk, wk):
            i = tcnt[0] % 2; tcnt[0] += 1
            S.dma("sp", ctab[i][:, :n], cos_fm[:, c:c + n], w=[("ctab", i)])
            S.dma("act", stab[i][:, :n], sin_fm[:, c:c + n], w=[("stab", i)])
            rope_fm(dst, src, n, ctab[i][:, :n], stab[i][:, :n], rk, wk, [("ctab", i), ("stab", i)])
        op("dve", lambda e: e.tensor_copy(out=Ib_t[:], in_=I_t[:]), ["I"], ["Ib"])
        rcnt = [0]
        def rope_fm(dst, src, n, ct, st_, rk, wk, ck):
            i = rcnt[0] % 2; rcnt[0] += 1
            op("pe", lambda e: e.matmul(psR[i][:, :n], lhsT=R_t[:], rhs=src, start=True, stop=True), ["R"] + rk, [("psR", i)])
            op("dve", lambda e: e.tensor_tensor(out=tA[:, :n], in0=psR[i][:, :n], in1=st_, op=ALU.mult), [("psR", i)] + ck, ["tA"])
            op("pool", lambda e: e.tensor_tensor(out=tB[:, :n], in0=src, in1=ct, op=ALU.mult), rk + ck, ["tB"])
            op("dve", lambda e: e.tensor_tensor(out=dst, in0=tA[:, :n], in1=tB[:, :n], op=ALU.add), ["tA", "tB"], wk)

        p1 = contextlib.ExitStack()
        with p1:
            sb1 = lambda n, s, d=F32: sb(n, s, d, p1)
            qiS = sb1("qiS", [128, 8, 128]); kiT = sb1("kiT", [128, T]); wiq_t = sb1("wiq_t", [128, 4, 1024])
            wq_t = sb1("wq_t", [128, NS + 1, 8]); qpos_t = sb1("qpos_t", [128, NS + 1, 1])
            qg_t = sb1("qg_t", [128, 4]); kig_t = sb1("kig_t", [128, 128]); kib_t = sb1("kib_t", [128, 128])
            kpos_t = sb1("kpos_t", [128, 512]); qpc = sb1("qpc", [128, 1])
            xin = sb1("xin", [128, 512]); xsq = sb1("xsq", [128, 512]); st1 = sb1("st1", [128, 4])
            acc = sb1("acc", [128, T]); wrk = sb1("wrk", [128, T]); mk = sb1("mk", [128, T], BF16)
            m8 = sb1("m8", [128, 8]); thr = sb1("thr", [128, 1]); rl = sb1("rl", [128, 512])
            for kc in range(4):
                S.dma("sp" if kc % 2 == 0 else "act", wiq_t[:, kc, :], w_iq[kc * 128:(kc + 1) * 128, :], w=["wiq"])
            S.dma("sp", qg_t[:], qg, w=["qg"]); S.dma("sp", kig_t[:], kig, w=["kig"]); S.dma("act", kib_t[:], kib, w=["kib"])
            S.dma("sp", kpos_t[:], kpos[:, 0:512], w=["kpos"])
            for j, (q0, m, nk) in enumerate(slots):
                S.dma("sp", wq_t[:m, j, :], widx[q0:q0 + m, :], w=["wq"])
                S.dma("act", qpos_t[:m, j, :], qpos[q0:q0 + m, :], w=["qpos"])
            for j, (q0, m, nk) in enumerate(slots):
                S.dma("sp", xin[:m, :], cq[q0:q0 + m, :], w=["xin"])
                op("dve", lambda e: e.tensor_tensor(out=xsq[:m, :], in0=xin[:m, :], in1=xin[:m, :], op=ALU.mult), ["xin"], ["xsq"])
                op("dve", lambda e: e.tensor_reduce(out=st1[:m, 0:1], in_=xsq[:m, :], axis=AX.X, op=ALU.add), ["xsq"], ["st1"])
                op("dve", lambda e: e.tensor_scalar(out=st1[:m, 1:2], in0=st1[:m, 0:1], scalar1=1.0 / 512, scalar2=1e-6, op0=ALU.mult, op1=ALU.add), ["st1"], ["st1"])
                op("act", lambda e: e.activation(out=st1[:m, 2:3], in_=st1[:m, 1:2], func=AF.Sqrt), ["st1"], ["st1"])
                op("dve", lambda e: e.reciprocal(out=st1[:m, 3:4], in_=st1[:m, 2:3]), ["st1"], ["st1"])
                op("dve", lambda e: e.tensor_scalar(out=xsq[:m, :], in0=xin[:m, :], scalar1=st1[:m, 3:4], scalar2=None, op0=ALU.mult), ["xin", "st1"], ["xsq"])
                for kc in range(4):
                    i = kc % 2
                    op("pe", lambda e: e.transpose(psM[i][:, :m], xsq[:m, kc * 128:(kc + 1) * 128], I_t[:m, :m]), ["xsq", "I"], [("psM", i)])
                    op("act", lambda e: e.activation(out=cqnT[:, kc, q0:q0 + m], in_=psM[i][:, :m], func=AF.Copy, scale=qg_t[:, kc:kc + 1]), [("psM", i), "qg"], ["cqnT"])
            for kt in range(NKT):
                t0 = kt * 128; m = min(128, T - t0)
                S.dma("sp", xin[:m, :128], kidx[t0:t0 + m, :], w=["xin"])
                op("dve", lambda e: e.tensor_reduce(out=st1[:m, 0:1], in_=xin[:m, :128], axis=AX.X, op=ALU.add), ["xin"], ["st1"])
                op("dve", lambda e: e.tensor_scalar(out=st1[:m, 1:2], in0=st1[:m, 0:1], scalar1=-1.0 / 128, scalar2=None, op0=ALU.mult), ["st1"], ["st1"])
                op("dve", lambda e: e.tensor_scalar(out=xsq[:m, :128], in0=xin[:m, :128], scalar1=st1[:m, 1:2], scalar2=None, op0=ALU.add), ["xin", "st1"], ["xsq"])
                op("dve", lambda e: e.tensor_tensor(out=xsq[:m, 128:256], in0=xsq[:m, :128], in1=xsq[:m, :128], op=ALU.mult), ["xsq"], ["xsq"])
                op("dve", lambda e: e.tensor_reduce(out=st1[:m, 0:1], in_=xsq[:m, 128:256], axis=AX.X, op=ALU.add), ["xsq"], ["st1"])
                op("dve", lambda e: e.tensor_scalar(out=st1[:m, 1:2], in0=st1[:m, 0:1], scalar1=1.0 / 128, scalar2=LN_EPS, op0=ALU.mult, op1=ALU.add), ["st1"], ["st1"])
                op("act", lambda e: e.activation(out=st1[:m, 2:3], in_=st1[:m, 1:2], func=AF.Sqrt), ["st1"], ["st1"])
                op("dve", lambda e: e.reciprocal(out=st1[:m, 3:4], in_=st1[:m, 2:3]), ["st1"], ["st1"])
                op("dve", lambda e: e.scalar_tensor_tensor(out=xsq[:m, 256:384], in0=xsq[:m, :128], scalar=st1[:m, 3:4], in1=kig_t[:m, :], op0=ALU.mult, op1=ALU.mult), ["xsq", "st1", "kig"], ["xsq"])
                op("dve", lambda e: e.tensor_tensor(out=xsq[:m, 384:512], in0=xsq[:m, 256:384], in1=kib_t[:m, :], op=ALU.add), ["xsq", "kib"], ["xsq"])
                i = kt % 2
                op("pe", lambda e: e.transpose(psM[i][:, :m], xsq[:m, 384:512], I_t[:m, :m]), ["xsq", "I"], [("psM", i)])
                op("act", lambda e: e.copy(out=acc[:, t0:t0 + m], in_=psM[i][:, :m]), [("psM", i)], ["acc"])
            for c in range(0, T, 512):
                n = min(512, T - c)
                rope_k(kiT[:, c:c + n], acc[:, c:c + n], c, n, ["acc"], ["kiT"])
            cscale = (8 ** -0.5) * (128 ** -0.5)
            for j, (q0, m, nk) in enumerate(slots):
                for hh in range(8):
                    i = hh % 2
                    for kc in range(4):
                        op("pe", lambda e: e.matmul(psM[i][:, :m], lhsT=wiq_t[:, kc, hh * 128:(hh + 1) * 128], rhs=cqnT[:, kc, q0:q0 + m], start=(kc == 0), stop=(kc == 3)),
                           ["wiq", "cqnT"], [("psM", i)])
                    op("act", lambda e: e.copy(out=xT[:, :m], in_=psM[i][:, :m]), [("psM", i)], ["xT"])
                    rope_fm(qiS[:, hh, :m], xT[:, :m], m, cosq_t[:, q0:q0 + m], sinq_t[:, q0:q0 + m], ["xT"], ["qiS"], ["cosq", "sinq"])
                for c in range(0, nk, 512):
                    n = min(512, nk - c)
                    for hh in range(8):
                        i = hh % 2
                        op("pe", lambda e: e.matmul(psM[i][:m, :n], lhsT=qiS[:, hh, :m], rhs=kiT[:, c:c + n], start=True, stop=True), ["qiS", "kiT"], [("psM", i)])
                        op("act", lambda e: e.activation(out=rl[:m, :n], in_=psM[i][:m, :n], func=AF.Relu, scale=cscale), [("psM", i)], ["rl"])
                        if hh == 0:
                            op("dve", lambda e: e.tensor_scalar(out=acc[:m, c:c + n], in0=rl[:m, :n], scalar1=wq_t[:m, j, hh:hh + 1], scalar2=None, op0=ALU.mult), ["rl", "wq"], ["acc"])
                        else:
                            op("dve", lambda e: e.scalar_tensor_tensor(out=acc[:m, c:c + n], in0=rl[:m, :n], scalar=wq_t[:m, j, hh:hh + 1], in1=acc[:m, c:c + n], op0=ALU.mult, op1=ALU.add), ["rl", "wq", "acc"], ["acc"])
                for c in range(0, nk, 512):
                    n = min(512, nk - c)
                    op("dve", lambda e: e.tensor_scalar(out=qpc[:m, :], in0=qpos_t[:m, j, :], scalar1=float(-c), scalar2=None, op0=ALU.add), ["qpos"], ["qpc"])
                    op("dve", lambda e: e.tensor_scalar(out=wrk[:m, c:c + n], in0=kpos_t[:m, :n], scalar1=qpc[:m, :], scalar2=NEG, op0=ALU.is_gt, op1=ALU.mult), ["kpos", "qpc"], ["wrk"])
                op("dve", lambda e: e.tensor_tensor(out=acc[:m, :nk], in0=acc[:m, :nk], in1=wrk[:m, :nk], op=ALU.add), ["acc", "wrk"], ["acc"])
                src = acc
                for rnd in range(topk // 8):
                    op("dve", lambda e: e.max(out=m8[:m, :], in_=src[:m, :nk]), ["acc", "wrk"], ["m8"])
                    if rnd < topk // 8 - 1:
                        op("dve", lambda e: e.match_replace(out=wrk[:m, :nk], in_to_replace=m8[:m, :], in_values=src[:m, :nk], imm_value=-3.0e38), ["acc", "wrk", "m8"], ["wrk"])
                    src = wrk
                op("dve", lambda e: e.tensor_scalar(out=thr[:m, :], in0=m8[:m, 7:8], scalar1=-1.0e29, scalar2=None, op0=ALU.max), ["m8"], ["thr"])
                op("dve", lambda e: e.tensor_scalar(out=mk[:m, :nk], in0=acc[:m, :nk], scalar1=thr[:m, :], scalar2=None, op0=ALU.is_ge), ["acc", "thr"], ["mk"])
                for kt in range((nk + 127) // 128):
                    k0 = kt * 128; kn = min(128, nk - k0)
                    i = kt % 2
                    op("pe", lambda e: e.transpose(psT[i][:kn, :m], mk[:m, k0:k0 + kn], Ib_t[:m, :m]), ["mk", "Ib"], [("psT", i)])
                    op("act", lambda e: e.copy(out=maskT[:kn, kt, q0:q0 + m], in_=psT[i][:kn, :m]), [("psT", i)], ["maskT"])
        S.barrier()
        kr = sb("kr", [128, T], BF16); kraw = sb("kraw", [128, T]); va = sb("va", [128, NKT, 132], BF16); va32 = sb("va32", [128, NKT, 128])
        qTh = sb("qTh", [128, NQ], BF16); wuq_t = sb("wuq_t", [128, 4, 128])
        qchunks = [(c, min(512, NQ - c)) for c in range(0, NQ, 512)]
        ex = [sb(f"ex{i}", [128, 512]) for i in range(2)]
        pp = [sb(f"pp{i}", [128, 512], BF16) for i in range(2)]
        ot = [sb(f"ot{i}", [128, 128]) for i in range(2)]
        rs_ = sb("rs_", [128, 1])
        scale = 128 ** -0.5
        outs = []
        op("pool", lambda e: e.memset(va[:, :, 128:129], 1.0), [], ["va1"])
        gi = 0; oi = 0
        for h in range(NH):
            S.dma("sp", kraw[:, :], kT[h * 128:(h + 1) * 128, :], w=["kraw"])
            nfull = T // 128
            S.dma("act", va32[:, :nfull, :], v[0:nfull * 128, h * 128:(h + 1) * 128].rearrange("(kt p) d -> p kt d", p=128), w=["va32"])
            op("act", lambda e: e.copy(out=va[:, :nfull, 0:128], in_=va32[:, :nfull, :]), ["va32"], ["va"])
            if T % 128:
                S.dma("act", va32[:T % 128, nfull, :], v[nfull * 128:T, h * 128:(h + 1) * 128], w=["va32"])
                op("act", lambda e: e.copy(out=va[:T % 128, nfull, 0:128], in_=va32[:T % 128, nfull, :]), ["va32"], ["va"])
            for c in range(0, T, 512):
                n = min(512, T - c)
                rope_k(kr[:, c:c + n], kraw[:, c:c + n], c, n, ["kraw"], ["kr"])
            S.dma("sp", wuq_t[:], w_uq[:, h * 128:(h + 1) * 128].rearrange("(kc p) d -> p kc d", p=128), w=["wuq"])
            for (c, n) in qchunks:
                i = (c // 512) % 2
                for kc in range(4):
                    op("pe", lambda e: e.matmul(psR[i][:, :n], lhsT=wuq_t[:, kc, :], rhs=cqnT[:, kc, c:c + n], start=(kc == 0), stop=(kc == 3)), ["wuq", "cqnT"], [("psR", i)])
                op("act", lambda e: e.copy(out=xT[:, :n], in_=psR[i][:, :n]), [("psR", i)], ["xT"])
                rope_fm(qTh[:, c:c + n], xT[:, :n], n, cosq_t[:, c:c + n], sinq_t[:, c:c + n], ["xT"], ["qTh"], ["cosq", "sinq"])
            for j, (q0, m, nk) in enumerate(slots):
                nkt = (nk + 127) // 128
                po = psO[oi % 2]; pok = ("psO", oi % 2)
                for g0 in range(0, nkt, 4):
                    gn = min(4, nkt - g0)
                    i = gi % 2; gi += 1
                    for kk_ in range(gn):
                        kt = g0 + kk_; k0 = kt * 128; kn = min(128, nk - k0)
                        op("pe", lambda e: e.matmul(psM[i][:kn, kk_ * 128:kk_ * 128 + m], lhsT=kr[:, k0:k0 + kn], rhs=qTh[:, q0:q0 + m], start=True, stop=True), ["kr", "qTh"], [("psM", i)])
                    kn_last = min(128, nk - (g0 + gn - 1) * 128)
                    pv = psM[i][:, :gn * 128].rearrange("p (a q) -> p a q", q=128)
                    ev = ex[i][:, :gn * 128].rearrange("p (a q) -> p a q", q=128)
                    ppv = pp[i][:, :gn * 128].rearrange("p (a q) -> p a q", q=128)
                    if kn_last == 128:
                        op("act", lambda e: e.activation(out=ev[:, :, :m], in_=pv[:, :, :m], func=AF.Exp, scale=scale), [("psM", i)], [("ex", i)])
                        op("dve", lambda e: e.tensor_tensor(out=ppv[:, :, :m], in0=ev[:, :, :m], in1=maskT[:, g0:g0 + gn, q0:q0 + m], op=ALU.mult), [("ex", i), "maskT"], [("pp", i)])
                    else:
                        if gn > 1:
                            op("act", lambda e: e.activation(out=ev[:, :gn - 1, :m], in_=pv[:, :gn - 1, :m], func=AF.Exp, scale=scale), [("psM", i)], [("ex", i)])
                            op("dve", lambda e: e.tensor_tensor(out=ppv[:, :gn - 1, :m], in0=ev[:, :gn - 1, :m], in1=maskT[:, g0:g0 + gn - 1, q0:q0 + m], op=ALU.mult), [("ex", i), "maskT"], [("pp", i)])
                        op("act", lambda e: e.activation(out=ev[:kn_last, gn - 1, :m], in_=pv[:kn_last, gn - 1, :m], func=AF.Exp, scale=scale), [("psM", i)], [("ex", i)])
                        op("dve", lambda e: e.tensor_tensor(out=ppv[:kn_last, gn - 1, :m], in0=ev[:kn_last, gn - 1, :m], in1=maskT[:kn_last, g0 + gn - 1, q0:q0 + m], op=ALU.mult), [("ex", i), "maskT"], [("pp", i)])
                    for kk_ in range(gn):
                        kt = g0 + kk_; kn = min(128, nk - kt * 128)
                        op("pe", lambda e: e.matmul(po[:m, :129], lhsT=ppv[:kn, kk_, :m], rhs=va[:kn, kt, :129], start=(kt == 0), stop=(kt == nkt - 1)), [("pp", i), "va", "va1"], [pok])
                o = ot[oi % 2]
                op("dve", lambda e: e.reciprocal(out=rs_[:m, :], in_=po[:m, 128:129]), [pok], ["rs_"])
                op("dve", lambda e: e.tensor_scalar(out=o[:m, :], in0=po[:m, :128], scalar1=rs_[:m, :], scalar2=None, op0=ALU.mult), [pok, "rs_"], [("ot", oi % 2)])
                outs.append(S.dma("pool", y[q0:q0 + m, h * 128:(h + 1) * 128], o[:m, :], r=[("ot", oi % 2)]))
                oi += 1
        S.finish(outs, "pool")
        print("ninst", S.ninst, "nsem", S.nsem)
    return nc


def rope_tables(pos):
    inv = (10000.0 ** (-np.arange(0, 128, 2, dtype=np.float32) / 128)).astype(np.float32)
    ang = (pos.astype(np.float32)[:, None] * inv[None, :]).astype(np.float32)
    c = np.cos(ang.astype(np.float64)).astype(np.float32); s = np.sin(ang.astype(np.float64)).astype(np.float32)
    cos_fm = np.concatenate([c, c], axis=1).T
    sin_fm = np.concatenate([-s, s], axis=1).T
    return np.ascontiguousarray(cos_fm), np.ascontiguousarray(sin_fm)


def dsa_consts(T):
    R = np.zeros((128, 128), np.float32)
    for i in range(64):
        R[i + 64, i] = 1; R[i, i + 64] = 1
    I = np.eye(128, dtype=np.float32)
    kpos = np.ascontiguousarray(np.broadcast_to(np.arange(T, dtype=np.float32)[None, :], (128, T)))
    return R, I, kpos


def core_qpos(r, NS):
    pos = []
    for j in range(NS):
        pos.extend(range(512 * j + 128 * r, 512 * j + 128 * r + 128))
    pos.extend(range(512 * NS + 4 * r, 512 * NS + 4 * r + 4))
    return np.array(pos)


def dsa_inmap(r, NS, cq, ka, va, kidx, widx, qnorm_g, w_uq, w_iq, kidx_g, kidx_b):
    T = cq.shape[0]
    pos = core_qpos(r, NS)
    R, I, kpos = dsa_consts(T)
    cos_fm, sin_fm = rope_tables(np.arange(T))
    cosq, sinq = rope_tables(pos)
    bc = lambda a: np.ascontiguousarray(np.broadcast_to(a[None, :], (128, a.shape[0])).astype(np.float32))
    return dict(cq=np.ascontiguousarray(cq[pos]), widx=np.ascontiguousarray(widx[pos]), qpos=pos.astype(np.float32)[:, None].copy(),
                kT=np.ascontiguousarray(ka.T), v=np.ascontiguousarray(va), kidx=np.ascontiguousarray(kidx),
                w_uq=np.ascontiguousarray(w_uq), w_iq=np.ascontiguousarray(w_iq),
                qg=np.ascontiguousarray(qnorm_g.reshape(4, 128).T), kig=bc(kidx_g), kib=bc(kidx_b),
                cos_fm=cos_fm, sin_fm=sin_fm, cosq=cosq, sinq=sinq, kpos=kpos, cR=R, cI=I)


WINS = (2, 4, 8, 16)

def pool_phase(nc, S, es, pinT, icnt, wp, psc, xb, kc0, NT, ps, piece):
    sb = lambda n, s, d=F32: es.enter_context(nc.sbuf_tensor(n, s, d))
    W = NT + 15
    x = sb("pl_x", [128, 8, W]); a = sb("pl_a", [128, W]); b = sb("pl_b", [128, W]); pl = sb("pl_pl", [128, 8, NT])
    ic = sb("pl_ic", [128, 4, NT]); wt = sb("pl_wt", [128, 4, 2, 256]); sc = sb("pl_sc", [128, 8])
    S.dma("sp", x[:], pinT.rearrange("(c p) t -> p c t", p=128), w=["pl_x"])
    S.dma("act", ic[:], icnt.rearrange("w p t -> p w t"), w=["pl_ic"])
    S.dma("act", wt[:], wp.rearrange("g (kc p) d -> p g kc d", p=128), w=["pl_wt"])
    S.dma("sp", sc[:], psc, w=["pl_sc"])
    for c in range(8):
        g = c // 2; win = WINS[g]
        sh = 1
        bufs = [a, b]; bi = 0
        prev = x[:, c, :]
        while sh < win:
            dst = bufs[bi]; bi ^= 1
            S.op("dve", lambda e: e.tensor_tensor(out=dst[:, sh:W], in0=prev[:, sh:W], in1=prev[:, 0:W - sh], op=ALU.add), r=["pl_x", "pl_a", "pl_b"], w=["pl_a" if dst is a else "pl_b"])
            S.op("dve", lambda e: e.tensor_copy(out=dst[:, 0:sh], in_=prev[:, 0:sh]), r=["pl_x", "pl_a", "pl_b"], w=["pl_a" if dst is a else "pl_b"])
            prev = dst; sh *= 2
        S.op("dve", lambda e: e.tensor_tensor(out=pl[:, c, :], in0=prev[:, 15:W], in1=ic[:, g, :], op=ALU.mult), r=["pl_a", "pl_b", "pl_ic"], w=[("pl_pl", c)])
        S.op("dve", lambda e: e.tensor_tensor(out=pl[:, c, :], in0=pl[:, c, :], in1=x[:, c, 15:W], op=ALU.subtract), r=[("pl_pl", c), "pl_x"], w=[("pl_pl", c)])
    k = 0
    for g in range(4):
        for oc in range(2):
            for t0 in range(0, NT, 512):
                n = min(512, NT - t0)
                pi = (k + t0 // 512) % 2
                p = ps[pi]; pk = ("pl_ps", pi)
                for kc in range(2):
                    S.op("pe", lambda e: e.matmul(p[:, :n], lhsT=wt[:, g, kc, oc * 128:(oc + 1) * 128], rhs=pl[:, 2 * g + kc, t0:t0 + n], start=(kc == 0), stop=(kc == 1)),
                         r=["pl_wt", ("pl_pl", 2 * g + kc)], w=[pk])
                S.op("act", lambda e: e.activation(out=xb[:, kc0 + 2 * g + oc, t0:t0 + n], in_=p[:, :n], func=AF.Copy, scale=sc[:, 2 * g + oc:2 * g + oc + 1]), r=[pk, "pl_sc"], w=[("mm_xb", piece)])
            k += 1


def pool_inmap(pin_b, t0, NT, pool_w, pool_scale):
    T = pin_b.shape[0]
    pad = np.concatenate([np.zeros((15, 1024), np.float32), pin_b], axis=0)
    sl = pad[t0:t0 + NT + 15]
    tt = np.arange(t0, t0 + NT)
    icnt = np.stack([np.broadcast_to((1.0 / np.minimum(tt + 1, w)).astype(np.float32)[None, :], (128, NT)) for w in WINS])
    return dict(pinT=np.ascontiguousarray(sl.T), icnt=np.ascontiguousarray(icnt), wp=np.ascontiguousarray(pool_w),
                psc=np.ascontiguousarray(pool_scale.reshape(8, 128).T))


import math

ALPHA = 2.0 ** 0.5
LN_EPS = 1e-5
NEGB = -1.0e30


def ln_rows(S, u, m, D, gb, bb, out, sq, st, uk, ok):
    S.op("dve", lambda e: e.tensor_reduce(out=st[:m, 0:1], in_=u[:m, :], axis=AX.X, op=ALU.add), r=[uk], w=["ln_st"])
    S.op("dve", lambda e: e.tensor_scalar(out=st[:m, 1:2], in0=st[:m, 0:1], scalar1=-1.0 / D, scalar2=None, op0=ALU.mult), r=["ln_st"], w=["ln_st"])
    S.op("dve", lambda e: e.tensor_scalar(out=u[:m, :], in0=u[:m, :], scalar1=st[:m, 1:2], scalar2=None, op0=ALU.add), r=[uk, "ln_st"], w=[uk])
    S.op("pool", lambda e: e.tensor_tensor(out=sq[:m, :], in0=u[:m, :], in1=u[:m, :], op=ALU.mult), r=[uk], w=["ln_sq"])
    S.op("dve", lambda e: e.tensor_reduce(out=st[:m, 0:1], in_=sq[:m, :], axis=AX.X, op=ALU.add), r=["ln_sq"], w=["ln_st"])
    S.op("dve", lambda e: e.tensor_scalar(out=st[:m, 1:2], in0=st[:m, 0:1], scalar1=1.0 / D, scalar2=LN_EPS, op0=ALU.mult, op1=ALU.add), r=["ln_st"], w=["ln_st"])
    S.op("act", lambda e: e.activation(out=st[:m, 2:3], in_=st[:m, 1:2], func=AF.Sqrt), r=["ln_st"], w=["ln_st"])
    S.op("dve", lambda e: e.reciprocal(out=st[:m, 3:4], in_=st[:m, 2:3]), r=["ln_st"], w=["ln_st"])
    S.op("dve", lambda e: e.scalar_tensor_tensor(out=sq[:m, :], in0=u[:m, :], scalar=st[:m, 3:4], in1=gb[:m, :], op0=ALU.mult, op1=ALU.mult), r=[uk, "ln_st", "lngb"], w=["ln_sq"])
    S.op("pool", lambda e: e.tensor_tensor(out=out[:m, :], in0=sq[:m, :], in1=bb[:m, :], op=ALU.add), r=["ln_sq", "lngb"], w=[ok])


def build_s3(NT=1028, D=4096, CAP=192, cb=256, POOLF=False):
    nc = new_nc()
    KC = D // 128
    di = lambda n, s, d=F32: nc.dram_tensor(n, s, d, kind="ExternalInput").ap()
    do = lambda n, s, d=F32: nc.dram_tensor(n, s, d, kind="ExternalOutput").ap()
    mixT = di("mixT", [D - 1024 if POOLF else D, NT]); hrow = di("hrow", [NT, D]); w_out = di("w_out", [D, D])
    lng = di("lng", [128, D]); lnb = di("lnb", [128, D]); wr = di("wr", [D, 72]); rbias = di("rbias", [128, 72])
    cU = di("cU", [128, 128]); cOnes = di("cOnes", [128, 128]); cIota = di("cIota", [128, 8]); cI = di("cI", [128, 128])
    h1 = do("h1", [NT, D]); xg = do("xg", [8 * CAP, D]); gsc = do("gsc", [8 * CAP, 8]); idx = do("idx", [NT, 1], I32)
    z = nc.dram_tensor("z", [NT, D], F32, kind="Internal").ap()
    if POOLF:
        pinT = di("pinT", [1024, NT + 15]); icnt = di("icnt", [4, 128, NT]); wp = di("wp", [4, 256, 256]); psc = di("psc", [128, 8])
    toks = [(t0, min(128, NT - t0)) for t0 in range(0, NT, 128)]
    es = contextlib.ExitStack()
    with es:
        S = Sched(nc, es)
        sb = lambda n, s, d=F32, st=es: st.enter_context(nc.sbuf_tensor(n, s, d))
        ps = [es.enter_context(nc.psum_tensor(f"ps{i}", [128, 512], F32)) for i in range(4)]
        pa = contextlib.ExitStack()
        with pa:
            if POOLF:
                xb = pa.enter_context(nc.sbuf_tensor("mm_xb", [128, KC, NT], BF16))
                pq = contextlib.ExitStack()
                with pq:
                    pool_phase(nc, S, pq, pinT, icnt, wp, psc, xb, KC - 8, NT, ps, 3)
                S.barrier()
                mm_phase(nc, S, pa, mixT, w_out, z, NT, D, D, ps, cb, dst_key="z", xb=xb, kx=D - 1024)
            else:
                mm_phase(nc, S, pa, mixT, w_out, z, NT, D, D, ps, cb, dst_key="z")
        S.barrier()
        gb = sb("gb", [128, D]); bb = sb("bb", [128, D]); zt = sb("zt", [128, D]); ht = sb("ht", [128, D]); sq = sb("sq", [128, D]); h1t = sb("h1t", [128, D])
        h1T = sb("h1T", [128, KC, 128]); wr_t = sb("wr_t", [128, KC, 72]); rb_t = sb("rb_t", [128, 72])
        U_t = sb("U_t", [128, 128]); On_t = sb("On_t", [128, 128]); Io_t = sb("Io_t", [128, 8]); I_t = sb("I_t", [128, 128]); zero = sb("zero", [128, D])
        st = sb("st", [128, 4]); lg = sb("lg", [128, 72]); sm = sb("sm", [128, 16]); goh = sb("goh", [128, 8]); ex8 = sb("ex8", [128, 8])
        esel = sb("esel", [128, 8]); e2 = sb("e2", [128, 8]); oh1 = sb("oh1", [128, 8]); oh2 = sb("oh2", [128, 8]); gvec = sb("gvec", [128, 8]); t8 = sb("t8", [128, 8])
        carry = sb("carry", [128, 8]); pos8 = sb("pos8", [128, 8]); idx_t = sb("idx_t", [128, 1], I32)
        S.dma("sp", gb[:], lng, w=["lngb"]); S.dma("act", bb[:], lnb, w=["lngb"])
        S.dma("sp", wr_t[:], wr.rearrange("(kc p) n -> p kc n", p=128), w=["wr"]); S.dma("act", rb_t[:], rbias, w=["rb"])
        S.dma("sp", U_t[:], cU, w=["U"]); S.dma("act", On_t[:], cOnes, w=["On"]); S.dma("sp", Io_t[:], cIota, w=["Io"]); S.dma("act", I_t[:], cI, w=["I"])
        S.op("pool", lambda e: e.memset(zero[:], 0.0), w=["zero"])
        S.op("pool", lambda e: e.memset(carry[:], 0.0), w=["carry"])
        for r0 in range(0, 8 * CAP, 128):
            S.dma("sp" if (r0 // 128) % 2 == 0 else "act", xg[r0:r0 + 128, :], zero[:], r=["zero"], w=["xg"])
        S.dma("sp", gsc.rearrange("(a p) e -> p a e", p=128), zero[:, :8 * CAP // 128 * 8].rearrange("p (a e) -> p a e", e=8), r=["zero"], w=["gsc"])
        outs = []
        for (t0, m) in toks:
            S.dma("sp", zt[:m, :], z[t0:t0 + m, :], r=[("z", t0)], w=["zt"])
            S.dma("act", ht[:m, :], hrow[t0:t0 + m, :], w=["ht"])
            S.op("dve", lambda e: e.scalar_tensor_tensor(out=zt[:m, :], in0=ht[:m, :], scalar=ALPHA, in1=zt[:m, :], op0=ALU.mult, op1=ALU.add), r=["ht", "zt"], w=["zt"])
            ln_rows(S, zt, m, D, gb, bb, h1t, sq, st, "zt", "h1t")
            outs.append(S.dma("sp", h1[t0:t0 + m, :], h1t[:m, :], r=["h1t"]))
            for k4 in range(0, KC, 4):
                p = ps[(k4 // 4) % 4]; pk = ("ps", (k4 // 4) % 4)
                for kk in range(4):
                    kc = k4 + kk
                    S.op("pe", lambda e: e.transpose(p[:, kk * 128:kk * 128 + m], h1t[:m, kc * 128:(kc + 1) * 128], I_t[:m, :m]), r=["h1t", "I"], w=[pk])
                pv = p[:, :].rearrange("p (a q) -> p a q", q=128)
                S.op("act" if (k4 // 4) % 2 == 0 else "dve", (lambda e: e.copy(out=h1T[:, k4:k4 + 4, :m], in_=pv[:, :, :m])) if (k4 // 4) % 2 == 0 else (lambda e: e.tensor_copy(out=h1T[:, k4:k4 + 4, :m], in_=pv[:, :, :m])), r=[pk], w=["h1T"])
            pl = ps[0]
            for kc in range(KC):
                S.op("pe", lambda e: e.matmul(pl[:m, :72], lhsT=h1T[:, kc, :m], rhs=wr_t[:, kc, :], start=(kc == 0), stop=(kc == KC - 1)), r=["h1T", "wr"], w=[("ps", 0)])
            S.op("dve", lambda e: e.tensor_tensor(out=lg[:m, :], in0=pl[:m, :72], in1=rb_t[:m, :], op=ALU.add), r=[("ps", 0), "rb"], w=["lg"])
            D_ = lambda fn, r, w: S.op("dve", fn, r=r, w=w)
            D_(lambda e: e.tensor_reduce(out=sm[:m, 0:1], in_=lg[:m, 0:8], axis=AX.X, op=ALU.max), ["lg"], ["sm"])
            D_(lambda e: e.tensor_scalar(out=goh[:m, :], in0=lg[:m, 0:8], scalar1=sm[:m, 0:1], scalar2=None, op0=ALU.is_equal), ["lg", "sm"], ["goh"])
            D_(lambda e: e.tensor_scalar(out=sm[:m, 1:2], in0=sm[:m, 0:1], scalar1=-1.0, scalar2=None, op0=ALU.mult), ["sm"], ["sm"])
            S.op("act", lambda e: e.activation(out=ex8[:m, :], in_=lg[:m, 0:8], func=AF.Exp, bias=sm[:m, 1:2]), r=["lg", "sm"], w=["ex8"])
            D_(lambda e: e.tensor_reduce(out=sm[:m, 2:3], in_=ex8[:m, :], axis=AX.X, op=ALU.add), ["ex8"], ["sm"])
            D_(lambda e: e.reciprocal(out=sm[:m, 3:4], in_=sm[:m, 2:3]), ["sm"], ["sm"])
            for g in range(8):
                if g == 0:
                    D_(lambda e: e.tensor_scalar(out=esel[:m, :], in0=lg[:m, 8:16], scalar1=goh[:m, 0:1], scalar2=None, op0=ALU.mult), ["lg", "goh"], ["esel"])
                else:
                    D_(lambda e: e.scalar_tensor_tensor(out=esel[:m, :], in0=lg[:m, 8 + 8 * g:16 + 8 * g], scalar=goh[:m, g:g + 1], in1=esel[:m, :], op0=ALU.mult, op1=ALU.add), ["lg", "goh", "esel"], ["esel"])
            D_(lambda e: e.tensor_reduce(out=sm[:m, 4:5], in_=esel[:m, :], axis=AX.X, op=ALU.max), ["esel"], ["sm"])
            D_(lambda e: e.tensor_scalar(out=oh1[:m, :], in0=esel[:m, :], scalar1=sm[:m, 4:5], scalar2=None, op0=ALU.is_equal), ["esel", "sm"], ["oh1"])
            D_(lambda e: e.scalar_tensor_tensor(out=e2[:m, :], in0=oh1[:m, :], scalar=NEGB, in1=esel[:m, :], op0=ALU.mult, op1=ALU.add), ["oh1", "esel"], ["e2"])
            D_(lambda e: e.tensor_reduce(out=sm[:m, 5:6], in_=e2[:m, :], axis=AX.X, op=ALU.max), ["e2"], ["sm"])
            D_(lambda e: e.tensor_scalar(out=oh2[:m, :], in0=e2[:m, :], scalar1=sm[:m, 5:6], scalar2=None, op0=ALU.is_equal), ["e2", "sm"], ["oh2"])
            D_(lambda e: e.tensor_tensor(out=sm[:m, 6:7], in0=sm[:m, 5:6], in1=sm[:m, 4:5], op=ALU.subtract), ["sm"], ["sm"])
            S.op("act", lambda e: e.activation(out=sm[:m, 7:8], in_=sm[:m, 6:7], func=AF.Exp), r=["sm"], w=["sm"])
            D_(lambda e: e.tensor_scalar(out=sm[:m, 8:9], in0=sm[:m, 7:8], scalar1=1.0, scalar2=None, op0=ALU.add), ["sm"], ["sm"])
            D_(lambda e: e.reciprocal(out=sm[:m, 9:10], in_=sm[:m, 8:9]), ["sm"], ["sm"])
            D_(lambda e: e.tensor_tensor(out=sm[:m, 10:11], in0=sm[:m, 9:10], in1=sm[:m, 3:4], op=ALU.mult), ["sm"], ["sm"])
            D_(lambda e: e.tensor_tensor(out=sm[:m, 11:12], in0=sm[:m, 3:4], in1=sm[:m, 10:11], op=ALU.subtract), ["sm"], ["sm"])
            D_(lambda e: e.tensor_scalar(out=t8[:m, :], in0=oh1[:m, :], scalar1=sm[:m, 10:11], scalar2=None, op0=ALU.mult), ["oh1", "sm"], ["t8"])
            D_(lambda e: e.scalar_tensor_tensor(out=gvec[:m, :], in0=oh2[:m, :], scalar=sm[:m, 11:12], in1=t8[:m, :], op0=ALU.mult, op1=ALU.add), ["oh2", "sm", "t8"], ["gvec"])
            D_(lambda e: e.tensor_tensor(out=t8[:m, :], in0=goh[:m, :], in1=Io_t[:m, :], op=ALU.mult), ["goh", "Io", "gvec"], ["t8"])
            D_(lambda e: e.tensor_reduce(out=sm[:m, 12:13], in_=t8[:m, :], axis=AX.X, op=ALU.add), ["t8"], ["sm"])
            S.op("pe", lambda e: e.matmul(ps[1][:m, 0:8], lhsT=U_t[:m, :m], rhs=goh[:m, :], start=True, stop=True), r=["U", "goh"], w=[("ps", 1)])
            S.op("pe", lambda e: e.matmul(ps[2][:, 0:8], lhsT=On_t[:m, :], rhs=goh[:m, :], start=True, stop=True), r=["On", "goh"], w=[("ps", 2)])
            D_(lambda e: e.tensor_tensor(out=pos8[:m, :], in0=ps[1][:m, 0:8], in1=carry[:m, :], op=ALU.add), [("ps", 1), "carry"], ["pos8"])
            D_(lambda e: e.tensor_tensor(out=carry[:, :], in0=carry[:, :], in1=ps[2][:, 0:8], op=ALU.add), [("ps", 2), "carry"], ["carry"])
            D_(lambda e: e.tensor_tensor(out=t8[:m, :], in0=goh[:m, :], in1=pos8[:m, :], op=ALU.mult), ["goh", "pos8", "sm"], ["t8"])
            D_(lambda e: e.tensor_reduce(out=sm[:m, 13:14], in_=t8[:m, :], axis=AX.X, op=ALU.add), ["t8"], ["sm"])
            D_(lambda e: e.tensor_scalar(out=sm[:m, 14:15], in0=sm[:m, 13:14], scalar1=float(CAP), scalar2=1.0e6, op0=ALU.is_ge, op1=ALU.mult), ["sm"], ["sm"])
            D_(lambda e: e.scalar_tensor_tensor(out=sm[:m, 15:16], in0=sm[:m, 12:13], scalar=float(CAP), in1=sm[:m, 13:14], op0=ALU.mult, op1=ALU.add), ["sm"], ["sm"])
            D_(lambda e: e.tensor_tensor(out=sm[:m, 15:16], in0=sm[:m, 15:16], in1=sm[:m, 14:15], op=ALU.add), ["sm"], ["sm"])
            D_(lambda e: e.tensor_copy(out=idx_t[:m, :], in_=sm[:m, 15:16]), ["sm"], ["idx_t"])
            outs.append(S.dma("pool", None, None, r=["h1t", "idx_t"], w=["xg"], fn=lambda e: e.indirect_dma_start(
                out=xg[:, :], out_offset=bass.IndirectOffsetOnAxis(ap=idx_t[:m, 0:1], axis=0), in_=h1t[:m, :], in_offset=None, bounds_check=8 * CAP - 1, oob_is_err=False)))
            outs.append(S.dma("pool", None, None, r=["gvec", "idx_t"], w=["gsc"], fn=lambda e: e.indirect_dma_start(
                out=gsc[:, :], out_offset=bass.IndirectOffsetOnAxis(ap=idx_t[:m, 0:1], axis=0), in_=gvec[:m, :], in_offset=None, bounds_check=8 * CAP - 1, oob_is_err=False)))
            outs.append(S.dma("sp", idx[t0:t0 + m, :], idx_t[:m, :], r=["idx_t"]))
        S.finish(outs, "pool")
        print("s3 ninst", S.ninst)
    return nc


def build_E(R=1536, D=4096, DE=512, RB=512):
    nc = new_nc()
    KC = D // 128; DC = DE // 128; NE = 8
    di = lambda n, s, d=F32: nc.dram_tensor(n, s, d, kind="ExternalInput").ap()
    X = di("X", [R, D]); G = di("G", [R, NE]); w1 = di("w1", [NE, D, DE]); w3 = di("w3", [NE, D, DE]); w2 = di("w2", [NE, DE, D]); cI = di("cI", [128, 128])
    Y = nc.dram_tensor("Y", [R, D], F32, kind="ExternalOutput").ap()
    RT = RB // 128
    es = contextlib.ExitStack()
    with es:
        S = Sched(nc, es)
        sb = lambda n, s, d=F32: es.enter_context(nc.sbuf_tensor(n, s, d))
        pA = [es.enter_context(nc.psum_tensor(f"pA{i}", [128, 512], F32)) for i in range(2)]
        pB = [es.enter_context(nc.psum_tensor(f"pB{i}", [128, 512], F32)) for i in range(2)]
        pY = [es.enter_context(nc.psum_tensor(f"pY{i}", [128, 512], F32)) for i in range(2)]
        pT = [es.enter_context(nc.psum_tensor(f"pT{i}", [128, 512], F32)) for i in range(2)]
        xr = [sb("xr0", [128, D])]; XT = sb("XT", [128, KC, RB], BF16); Ya = sb("Ya", [128, RT, D]); g_t = sb("g_t", [128, RT, NE])
        hT = sb("hT", [128, DC, RB], BF16); sl = sb("sl", [128, RB]); I_t = sb("I_t", [128, 128])
        w1s = sb("w1s", [128, KC, 128]); w3s = sb("w3s", [128, KC, 128]); w2s = sb("w2s", [128, DC, 512])
        w1b = [sb(f"w1b{i}", [128, KC, 128], BF16) for i in range(2)]; w3b = [sb(f"w3b{i}", [128, KC, 128], BF16) for i in range(2)]
        w2b = [sb(f"w2b{i}", [128, DC, 512], BF16) for i in range(2)]
        S.dma("sp", I_t[:], cI, w=["I"])
        outs = []
        wi = 0; w2i = 0; ti = 0; yi = 0; xi = 0
        for r0 in range(0, R, RB):
            S.dma("act", g_t[:], G[r0:r0 + RB, :].rearrange("(a p) e -> p a e", p=128), w=["g"])
            for a in range(RT):
                xb_ = xr[0]; xk = ("xr", 0); xi += 1
                S.dma("sp", xb_[:], X[r0 + a * 128:r0 + (a + 1) * 128, :], w=[xk])
                for k4 in range(0, KC, 4):
                    p = pT[ti % 2]; pk = ("pT", ti % 2)
                    for kk in range(4):
                        S.op("pe", lambda e: e.transpose(p[:, kk * 128:(kk + 1) * 128], xb_[:, (k4 + kk) * 128:(k4 + kk + 1) * 128], I_t[:]), r=[xk, "I"], w=[pk])
                    pv = p[:, :].rearrange("p (k q) -> p k q", q=128)
                    if ti % 2 == 0:
                        S.op("act", lambda e: e.copy(out=XT[:, k4:k4 + 4, a * 128:(a + 1) * 128], in_=pv), r=[pk], w=["XT"])
                    else:
                        S.op("dve", lambda e: e.tensor_copy(out=XT[:, k4:k4 + 4, a * 128:(a + 1) * 128], in_=pv), r=[pk], w=["XT"])
                    ti += 1
            for ex in range(NE):
                for dc in range(DC):
                    i = wi % 2; wi += 1
                    S.dma("sp", w1s[:], w1[ex, :, dc * 128:(dc + 1) * 128].rearrange("(kc p) d -> p kc d", p=128), w=["w1s"])
                    S.dma("act", w3s[:], w3[ex, :, dc * 128:(dc + 1) * 128].rearrange("(kc p) d -> p kc d", p=128), w=["w3s"])
                    S.op("dve", lambda e: e.tensor_copy(out=w1b[i][:], in_=w1s[:]), r=["w1s"], w=[("w1b", i)])
                    S.op("act", lambda e: e.copy(out=w3b[i][:], in_=w3s[:]), r=["w3s"], w=[("w3b", i)])
                    for kc in range(KC):
                        S.op("pe", lambda e: e.matmul(pA[i][:, :RB], lhsT=w1b[i][:, kc, :], rhs=XT[:, kc, :], start=(kc == 0), stop=(kc == KC - 1)), r=[("w1b", i), "XT"], w=[("pA", i)])
                    for kc in range(KC):
                        S.op("pe", lambda e: e.matmul(pB[i][:, :RB], lhsT=w3b[i][:, kc, :], rhs=XT[:, kc, :], start=(kc == 0), stop=(kc == KC - 1)), r=[("w3b", i), "XT"], w=[("pB", i)])
                    S.op("act", lambda e: e.activation(out=sl[:, :], in_=pA[i][:, :RB], func=AF.Silu), r=[("pA", i)], w=["sl"])
                    S.op("dve", lambda e: e.tensor_tensor(out=hT[:, dc, :], in0=sl[:, :], in1=pB[i][:, :RB], op=ALU.mult), r=["sl", ("pB", i)], w=["hT"])
                for cb in range(D // 512):
                    i = w2i % 2; w2i += 1
                    S.dma("sp" if cb % 2 == 0 else "act", w2s[:], w2[ex, :, cb * 512:(cb + 1) * 512].rearrange("(kc p) n -> p kc n", p=128), w=["w2s"])
                    if cb % 2 == 0:
                        S.op("act", lambda e: e.copy(out=w2b[i][:], in_=w2s[:]), r=["w2s"], w=[("w2b", i)])
                    else:
                        S.op("dve", lambda e: e.tensor_copy(out=w2b[i][:], in_=w2s[:]), r=["w2s"], w=[("w2b", i)])
                    for a in range(RT):
                        p = pY[yi % 2]; pk = ("pY", yi % 2); yi += 1
                        for kc in range(DC):
                            S.op("pe", lambda e: e.matmul(p[:, :], lhsT=hT[:, kc, a * 128:(a + 1) * 128], rhs=w2b[i][:, kc, :], start=(kc == 0), stop=(kc == DC - 1)), r=["hT", ("w2b", i)], w=[pk])
                        if ex == 0:
                            S.op("dve", lambda e: e.tensor_scalar(out=Ya[:, a, cb * 512:(cb + 1) * 512], in0=p[:, :], scalar1=g_t[:, a, ex:ex + 1], scalar2=None, op0=ALU.mult), r=[pk, "g"], w=[("Ya", a, cb)])
                        else:
                            S.op("dve", lambda e: e.scalar_tensor_tensor(out=Ya[:, a, cb * 512:(cb + 1) * 512], in0=p[:, :], scalar=g_t[:, a, ex:ex + 1], in1=Ya[:, a, cb * 512:(cb + 1) * 512], op0=ALU.mult, op1=ALU.add),
                                 r=[pk, "g", ("Ya", a, cb)], w=[("Ya", a, cb)])
            outs.append(S.dma("pool", Y[r0:r0 + RB, :].rearrange("(a p) d -> p a d", p=128), Ya[:], r=[("Ya", a, cb) for a in range(RT) for cb in range(D // 512)]))
        S.finish(outs, "pool")
        print("E ninst", S.ninst)
    return nc


def build_C(NT=1028, D=4096, CAP=192):
    nc = new_nc()
    di = lambda n, s, d=F32: nc.dram_tensor(n, s, d, kind="ExternalInput").ap()
    Yg = di("Yg", [8 * CAP, D]); idx = di("idx", [NT, 1], I32); h1 = di("h1", [NT, D]); lng = di("lng", [128, D]); lnb = di("lnb", [128, D])
    h2 = nc.dram_tensor("h2", [NT, D], F32, kind="ExternalOutput").ap()
    es = contextlib.ExitStack()
    with es:
        S = Sched(nc, es)
        sb = lambda n, s, d=F32: es.enter_context(nc.sbuf_tensor(n, s, d))
        gb = sb("gb", [128, D]); bb = sb("bb", [128, D]); ff = sb("ff", [128, D]); ht = sb("ht", [128, D]); sq = sb("sq", [128, D]); ot = sb("ot", [128, D])
        st = sb("st", [128, 4]); idx_t = sb("idx_t", [128, 1], I32)
        S.dma("sp", gb[:], lng, w=["lngb"]); S.dma("act", bb[:], lnb, w=["lngb"])
        outs = []
        for t0 in range(0, NT, 128):
            m = min(128, NT - t0)
            S.dma("sp", idx_t[:m, :], idx[t0:t0 + m, :], w=["idx_t"])
            S.dma("act", ht[:m, :], h1[t0:t0 + m, :], w=["ht"])
            S.dma("pool", None, None, r=["idx_t"], w=["ff"], fn=lambda e: e.indirect_dma_start(
                out=ff[:m, :], out_offset=None, in_=Yg[:, :], in_offset=bass.IndirectOffsetOnAxis(ap=idx_t[:m, 0:1], axis=0), bounds_check=8 * CAP - 1, oob_is_err=False))
            S.op("dve", lambda e: e.scalar_tensor_tensor(out=ff[:m, :], in0=ht[:m, :], scalar=ALPHA, in1=ff[:m, :], op0=ALU.mult, op1=ALU.add), r=["ht", "ff"], w=["ff"])
            ln_rows(S, ff, m, D, gb, bb, ot, sq, st, "ff", "ot")
            outs.append(S.dma("sp", h2[t0:t0 + m, :], ot[:m, :], r=["ot"]))
        S.finish(outs, "sp")
    return nc


def s3_consts():
    U = np.triu(np.ones((128, 128), np.float32), 1)
    return dict(cU=U, cOnes=np.ones((128, 128), np.float32), cIota=np.ascontiguousarray(np.broadcast_to(np.arange(8, dtype=np.float32)[None, :], (128, 8))),
                cI=np.eye(128, dtype=np.float32))

def bc128(a):
    return np.ascontiguousarray(np.broadcast_to(a[None, :], (128, a.shape[0])).astype(np.float32))


B_, SEQ_, D_, NMETA_ = 2, 4096, 4096, 16
T_ = SEQ_ + NMETA_
NT_ = T_ // 4
CAP_ = 192
_NC = {}

def _get(name, fn):
    if name not in _NC:
        t0 = time.time()
        _NC[name] = fn()
        print(f"[kernel] built {name} in {time.time() - t0:.1f}s", flush=True)
    return _NC[name]

def _run(name, nc, ims):
    t0 = time.time()
    res = run_bass_kernel_spmd(nc, ims, core_ids=list(range(8))).results
    print(f"[kernel] ran {name} in {time.time() - t0:.1f}s", flush=True)
    return res

def kernel(x, meta, w_in, rw_mu, rw_w2, rw_w0, rw_a2, rw_a0, rw_g2, rw_kk, rw_ka, rw_rk,
           rw_gn_g, rw_gn_b, at_qnorm_g, at_w_uq, at_w_iq, at_kidx_g, at_kidx_b, pool_w,
           pool_scale, w_out, ln1_g, ln1_b, router_g_w, router_g_b, router_e_w, router_e_b,
           exp_w1, exp_w3, exp_w2, ln2_g, ln2_b):
    A = lambda a: np.ascontiguousarray(np.asarray(a, dtype=np.float32))
    x = np.asarray(x, np.float32); meta = np.asarray(meta, np.float32)
    h = np.concatenate([np.broadcast_to(meta[None], (B_, NMETA_, D_)), x], axis=1).reshape(B_ * T_, D_)
    cs = s3_consts()
    for l in range(2):
        nc = _get("mm", lambda: build_mm(NT_, D_, 10088))
        wl = A(w_in[l])
        res = _run("proj", nc, [dict(xT=A(h[c * NT_:(c + 1) * NT_].T), w=wl) for c in range(8)])
        proj = np.concatenate([res[c]["out"] for c in range(8)]).reshape(B_, T_, 10088)
        del res, wl
        o = 0
        def take(n):
            nonlocal o
            s = slice(o, o + n); o += n
            return s
        s_r, s_k, s_v, s_dw, s_da, s_dg = take(1536), take(1536), take(1536), take(128), take(128), take(480)
        s_cq, s_ka, s_va, s_kidx, s_widx, s_pin = take(512), take(1536), take(1536), take(128), take(8), take(1024)
        mu = np.asarray(rw_mu[l], np.float32)
        nc = _get("rwkv", lambda: build_rwkv(T_, same=True))
        ims = []
        for c in range(8):
            b, hq = c // 4, c % 4
            ch = slice(hq * 384, (hq + 1) * 384)
            P = proj[b]
            sub = lambda s: P[:, s][:, ch]
            ims.append(rwkv_inmap(sub(s_r), sub(s_k), sub(s_v), P[:, s_dw], P[:, s_da], P[:, s_dg],
                                  mu[s_r][ch], mu[s_k][ch], mu[s_v][ch], mu[s_dw], mu[s_da], mu[s_dg],
                                  np.asarray(rw_w2[l])[:, ch], np.asarray(rw_w0[l])[ch], np.asarray(rw_a2[l])[:, ch], np.asarray(rw_a0[l])[ch],
                                  np.asarray(rw_g2[l])[:, ch], np.asarray(rw_kk[l])[ch], np.asarray(rw_ka[l])[ch], np.asarray(rw_rk[l]).reshape(-1)[ch],
                                  np.asarray(rw_gn_g[l])[ch], np.asarray(rw_gn_b[l])[ch]))
            ims[-1] = {k: A(v) for k, v in ims[-1].items()}
        res = _run("rwkv", nc, ims)
        mix = np.empty((B_, T_, D_), np.float32)
        for c in range(8):
            b, hq = c // 4, c % 4
            mix[b, :, hq * 384:(hq + 1) * 384] = res[c]["yT"].T
        del res, ims
        nc = _get("dsa", lambda: build_dsa(8, 12))
        ims = []
        for c in range(8):
            b, r = c // 4, c % 4
            P = proj[b]
            im = dsa_inmap(r, 8, P[:, s_cq], P[:, s_ka], P[:, s_va], P[:, s_kidx], P[:, s_widx], np.asarray(at_qnorm_g[l], np.float32),
                           np.asarray(at_w_uq[l], np.float32), np.asarray(at_w_iq[l], np.float32), np.asarray(at_kidx_g[l], np.float32), np.asarray(at_kidx_b[l], np.float32))
            ims.append({k: A(v) for k, v in im.items()})
        res = _run("dsa", nc, ims)
        for c in range(8):
            b, r = c // 4, c % 4
            mix[b, core_qpos(r, 8), 1536:3072] = res[c]["y"]
        del res, ims
        pool_ims = []
        for c in range(8):
            b, r = c // 4, c % 4
            im = pool_inmap(proj[b][:, s_pin], r * NT_, NT_, np.asarray(pool_w[l], np.float32), np.asarray(pool_scale[l], np.float32))
            pool_ims.append({k: A(v) for k, v in im.items()})
        del proj
        mix = mix.reshape(B_ * T_, D_)
        nc = _get("s3", lambda: build_s3(NT_, D_, CAP_, POOLF=True))
        wo = A(w_out[l]); g1 = bc128(np.asarray(ln1_g[l])); b1 = bc128(np.asarray(ln1_b[l]))
        wr = A(np.concatenate([np.asarray(router_g_w[l]), np.asarray(router_e_w[l])], axis=1))
        rbias = bc128(np.concatenate([np.asarray(router_g_b[l]), np.asarray(router_e_b[l])]))
        ims = [dict(mixT=A(mix[c * NT_:(c + 1) * NT_, :3072].T), **pool_ims[c], hrow=A(h[c * NT_:(c + 1) * NT_]), w_out=wo, lng=g1, lnb=b1, wr=wr, rbias=rbias, **cs) for c in range(8)]
        r3 = _run("s3", nc, ims)
        del ims, mix, wo
        nc = _get("E", lambda: build_E(8 * CAP_, D_, 512))
        ims = [dict(X=A(np.concatenate([r3[s]["xg"][g * CAP_:(g + 1) * CAP_] for s in range(8)])),
                    G=A(np.concatenate([r3[s]["gsc"][g * CAP_:(g + 1) * CAP_] for s in range(8)])),
                    w1=A(exp_w1[l][8 * g:8 * g + 8]), w3=A(exp_w3[l][8 * g:8 * g + 8]), w2=A(exp_w2[l][8 * g:8 * g + 8]), cI=cs["cI"]) for g in range(8)]
        rE = _run("E", nc, ims)
        del ims
        nc = _get("C", lambda: build_C(NT_, D_, CAP_))
        g2 = bc128(np.asarray(ln2_g[l])); b2 = bc128(np.asarray(ln2_b[l]))
        ims = [dict(Yg=A(np.concatenate([rE[g]["Y"][s * CAP_:(s + 1) * CAP_] for g in range(8)])), idx=np.ascontiguousarray(r3[s]["idx"]),
                    h1=A(r3[s]["h1"]), lng=g2, lnb=b2) for s in range(8)]
        rC = _run("C", nc, ims)
        h = np.concatenate([rC[c]["h2"] for c in range(8)])
        del ims, rE, r3, rC
    return np.ascontiguousarray(h.reshape(B_, T_, D_)[:, NMETA_:]).astype(np.float32)
```
